# Optimizing a Trainium2 kernel written in Bass

```python
import math
import jax, jax.numpy as jnp
from jax import lax
import numpy as np

D_MODEL = 1024
BATCH = 32
SEQ = 2048
DEPTH = 1

N_HEADS = 8
HEAD_DK = 64
HEAD_DV = 2 * HEAD_DK
Q_COLS = N_HEADS * 2 * HEAD_DK
ATTN_W = N_HEADS * HEAD_DV
Q_BLOCK = 128
N_BUCKETS = 32
MAX_DISTANCE = 128
POOL_WINDOWS = (2, 4, 8, 16)
N_POOL_GROUPS = 4
POOL_W = D_MODEL
POOL_GC = POOL_W // N_POOL_GROUPS
IN_COLS = 2 * Q_COLS + ATTN_W + POOL_W + 2 * D_MODEL
N_EXPERTS = 256
TOP_K = 8
N_EXPERT_GROUPS = 8
TOPK_GROUPS = 4
D_EXPERT = 256
D_SHARED = 256
ROUTED_SCALE = 2.5
MOE_BLOCK = 256
EPS = 1e-6

kernel_name = "hybrid_diffattn_pool_moe_encoder"


def rmsnorm(x, g):
    xf = x.astype(jnp.float32)
    y = xf * lax.rsqrt(jnp.mean(xf * xf, axis=-1, keepdims=True) + EPS) * g.astype(jnp.float32)
    return y.astype(x.dtype)


def t5_buckets(rel):
    nb = N_BUCKETS // 2
    max_exact = nb // 2
    n = jnp.abs(rel)
    large = max_exact + (jnp.log(jnp.maximum(n, 1).astype(jnp.float32) / max_exact)
                         / math.log(MAX_DISTANCE / max_exact) * (nb - max_exact)).astype(jnp.int32)
    large = jnp.minimum(large, nb - 1)
    return jnp.where(rel > 0, nb, 0) + jnp.where(n < max_exact, n, large)


def diff_attention(q, k, v, rel_table, lam):
    B, S = q.shape[0], q.shape[1]
    nqb = S // Q_BLOCK
    qb = q.reshape(B, nqb, Q_BLOCK, N_HEADS, 2, HEAD_DK).transpose(1, 0, 2, 3, 4, 5)
    kpos = jnp.arange(S)

    def block(args):
        qblk, q0 = args
        qpos = q0 + jnp.arange(Q_BLOCK)
        bias = rel_table[t5_buckets(kpos[None, :] - qpos[:, None])]
        bias = bias.transpose(2, 0, 1).astype(jnp.float32)
        s = jnp.einsum('bqhcd,bkhcd->bchqk', qblk, k).astype(jnp.float32) + bias
        p = jax.nn.softmax(s, axis=-1)
        a = (p[:, 0] - lam * p[:, 1]).astype(v.dtype)
        return jnp.einsum('bhqk,bkhd->bqhd', a, v)

    out = lax.map(block, (qb, jnp.arange(nqb) * Q_BLOCK))
    return out.transpose(1, 0, 2, 3, 4).reshape(B, S, N_HEADS, HEAD_DV)


def multiscale_pool(p, pool_w, pool_scale):
    B, S, _ = p.shape
    pg = p.astype(jnp.float32).reshape(B, S, N_POOL_GROUPS, POOL_GC)
    cs = jnp.concatenate([jnp.zeros_like(pg[:, :1]), jnp.cumsum(pg, axis=1)], axis=1)
    pos = jnp.arange(S)[:, None]
    half = jnp.array([w // 2 for w in POOL_WINDOWS], dtype=jnp.int32)[None, :]
    lo = jnp.clip(pos - half, 0, S - 1)
    hi = jnp.clip(pos + half - 1, 0, S - 1)
    grp = jnp.arange(N_POOL_GROUPS)[None, :]
    win_sum = cs[:, hi + 1, grp] - cs[:, lo, grp]
    cnt = (hi - lo + 1).astype(jnp.float32)[None, :, :, None]
    mixed = (win_sum / cnt - pg).astype(p.dtype)
    out = jnp.einsum('bsgc,gcd->bsgd', mixed, pool_w).reshape(B, S, POOL_W)
    return out * pool_scale


def moe_ffn(h, w_router, b_router, w_eg, w_eu, w_ed, w_sg, w_su, w_sd):
    T, D = h.shape
    scores = jax.nn.sigmoid((h @ w_router).astype(jnp.float32))
    biased = scores + b_router.astype(jnp.float32)
    per_grp = N_EXPERTS // N_EXPERT_GROUPS
    grp_score = lax.top_k(biased.reshape(T, N_EXPERT_GROUPS, per_grp), 2)[0].sum(-1)
    _, grp_idx = lax.top_k(grp_score, TOPK_GROUPS)
    grp_mask = jax.nn.one_hot(grp_idx, N_EXPERT_GROUPS, dtype=jnp.float32).sum(1) > 0
    exp_mask = jnp.repeat(grp_mask, per_grp, axis=1)
    _, eidx = lax.top_k(jnp.where(exp_mask, biased, -jnp.inf), TOP_K)
    wts = jnp.take_along_axis(scores, eidx, axis=-1)
    wts = wts / jnp.sum(wts, axis=-1, keepdims=True) * ROUTED_SCALE

    n_rows = T * TOP_K
    e_flat = eidx.reshape(n_rows)
    tok_flat = jnp.repeat(jnp.arange(T, dtype=jnp.int32), TOP_K)
    w_flat = wts.reshape(n_rows)
    order = jnp.argsort(e_flat)
    e_s, tok_s, w_s = e_flat[order], tok_flat[order], w_flat[order]
    counts = jnp.bincount(e_flat, length=N_EXPERTS)
    start = jnp.cumsum(counts) - counts
    nblk = (counts + MOE_BLOCK - 1) // MOE_BLOCK
    blk_end = jnp.cumsum(nblk)
    blk_start = blk_end - nblk
    dest = blk_start[e_s] * MOE_BLOCK + (jnp.arange(n_rows) - start[e_s])
    n_blocks = -(-n_rows // MOE_BLOCK) + N_EXPERTS
    tok_buf = jnp.zeros((n_blocks * MOE_BLOCK,), jnp.int32).at[dest].set(tok_s)
    w_buf = jnp.zeros((n_blocks * MOE_BLOCK,), jnp.float32).at[dest].set(w_s)
    blk_expert = jnp.clip(jnp.searchsorted(blk_end, jnp.arange(n_blocks), side='right'),
                          0, N_EXPERTS - 1)

    def body(acc, blk):
        tok, wt, e = blk
        xb = h[tok]
        yb = (jax.nn.silu(xb @ w_eg[e]) * (xb @ w_eu[e])) @ w_ed[e]
        return acc.at[tok].add(yb.astype(jnp.float32) * wt[:, None]), None

    acc, _ = lax.scan(body, jnp.zeros((T, D), jnp.float32),
                      (tok_buf.reshape(n_blocks, MOE_BLOCK), w_buf.reshape(n_blocks, MOE_BLOCK), blk_expert))
    shared = (jax.nn.silu(h @ w_sg) * (h @ w_su)) @ w_sd
    return acc.astype(h.dtype) + shared


def setup_inputs(seed: int = 0) -> dict:
    key = jax.random.key(seed)
    ks = jax.random.split(key, 32)
    f32 = jnp.float32
    D, L = D_MODEL, DEPTH

    def nrm(k, shape, scale):
        return jax.random.normal(k, shape, f32) * scale

    def gain(k, shape):
        return 1.0 + 0.1 * jax.random.normal(k, shape, f32)

    return {
        'x': nrm(ks[0], (BATCH, SEQ, D), 1.0),
        'c': nrm(ks[1], (BATCH, D), 1.0),
        'rel_bias_table': nrm(ks[2], (N_BUCKETS, N_HEADS), 0.5),
        'w_ada': nrm(ks[3], (L, D, 6 * D), 0.5 * D ** -0.5),
        'b_ada': nrm(ks[4], (L, 6 * D), 0.02),
        'norm1_g': gain(ks[5], (L, D)),
        'w_in': nrm(ks[6], (L, D, IN_COLS), D ** -0.5),
        'q_norm_g': gain(ks[7], (L, HEAD_DK)),
        'k_norm_g': gain(ks[8], (L, HEAD_DK)),
        'lambda_q1': nrm(ks[9], (L, HEAD_DK), 0.1),
        'lambda_k1': nrm(ks[10], (L, HEAD_DK), 0.1),
        'lambda_q2': nrm(ks[11], (L, HEAD_DK), 0.1),
        'lambda_k2': nrm(ks[12], (L, HEAD_DK), 0.1),
        'subln_g': gain(ks[13], (L, HEAD_DV)),
        'pool_w': nrm(ks[14], (L, N_POOL_GROUPS, POOL_GC, POOL_GC), POOL_GC ** -0.5),
        'pool_scale': gain(ks[15], (L, POOL_W)),
        'w_out': nrm(ks[16], (L, D, D), D ** -0.5),
        'norm2_g': gain(ks[17], (L, D)),
        'w_router': nrm(ks[18], (L, D, N_EXPERTS), D ** -0.5),
        'b_router': nrm(ks[19], (L, N_EXPERTS), 0.01),
        'w_exp_gate': nrm(ks[20], (L, N_EXPERTS, D, D_EXPERT), D ** -0.5),
        'w_exp_up': nrm(ks[21], (L, N_EXPERTS, D, D_EXPERT), D ** -0.5),
        'w_exp_down': nrm(ks[22], (L, N_EXPERTS, D_EXPERT, D), D_EXPERT ** -0.5),
        'w_sh_gate': nrm(ks[23], (L, D, D_SHARED), D ** -0.5),
        'w_sh_up': nrm(ks[24], (L, D, D_SHARED), D ** -0.5),
        'w_sh_down': nrm(ks[25], (L, D_SHARED, D), D_SHARED ** -0.5),
    }


def reference(x, c, rel_bias_table, w_ada, b_ada, norm1_g, w_in, q_norm_g, k_norm_g,
              lambda_q1, lambda_k1, lambda_q2, lambda_k2, subln_g, pool_w, pool_scale,
              w_out, norm2_g, w_router, b_router, w_exp_gate, w_exp_up, w_exp_down,
              w_sh_gate, w_sh_up, w_sh_down):
    B, S, D = x.shape
    split_pts = [Q_COLS, 2 * Q_COLS, 2 * Q_COLS + ATTN_W, 2 * Q_COLS + ATTN_W + POOL_W]
    for l in range(DEPTH):
        mod = jax.nn.silu(c) @ w_ada[l] + b_ada[l]
        sh1, sc1, g1, sh2, sc2, g2 = [m[:, None, :] for m in jnp.split(mod, 6, axis=-1)]

        h = rmsnorm(x, norm1_g[l]) * (1 + sc1) + sh1
        proj = h @ w_in[l]
        q, k, v, p_in, gts = jnp.split(proj, split_pts, axis=-1)
        q = rmsnorm(q.reshape(B, S, N_HEADS, 2, HEAD_DK), q_norm_g[l]) * (HEAD_DK ** -0.5)
        k = rmsnorm(k.reshape(B, S, N_HEADS, 2, HEAD_DK), k_norm_g[l])
        v = v.reshape(B, S, N_HEADS, HEAD_DV)
        lam_init = 0.8 - 0.6 * math.exp(-0.3 * l)
        lam = (jnp.exp(jnp.sum((lambda_q1[l] * lambda_k1[l]).astype(jnp.float32)))
               - jnp.exp(jnp.sum((lambda_q2[l] * lambda_k2[l]).astype(jnp.float32))) + lam_init)
        attn = diff_attention(q, k, v, rel_bias_table, lam)
        attn = (rmsnorm(attn, subln_g[l]) * (1 - lam_init)).reshape(B, S, ATTN_W)
        pool = multiscale_pool(p_in, pool_w[l], pool_scale[l])
        gate_a, gate_p = jnp.split(jax.nn.sigmoid(gts), 2, axis=-1)
        x = x + g1 * ((gate_a * attn + gate_p * pool) @ w_out[l])

        h2 = rmsnorm(x, norm2_g[l]) * (1 + sc2) + sh2
        y2 = moe_ffn(h2.reshape(B * S, D), w_router[l], b_router[l], w_exp_gate[l], w_exp_up[l],
                     w_exp_down[l], w_sh_gate[l], w_sh_up[l], w_sh_down[l])
        x = x + g2 * y2.reshape(B, S, D)
    return x
```

```python
import math
import numpy as np
from contextlib import ExitStack
import concourse.bass as bass
import concourse.mybir as mybir
from concourse.bass_utils import run_bass_kernel_spmd

F32 = mybir.dt.float32
BF16 = mybir.dt.bfloat16
AF = mybir.ActivationFunctionType
ALU = mybir.AluOpType
AX = mybir.AxisListType

D = 1024
S = 2048
NB = 32
NH = 8
NE = 256
EPS = 1e-6
LAM_INIT = 0.8 - 0.6 * math.exp(-0.3 * 0)
PADL = 16
LP = S + 2 * PADL
MW = 1280
FVW = MW + 127


class Res:
    __slots__ = ("w", "r")

    def __init__(self):
        self.w = None
        self.r = {}


class Sched:
    ENG = ("pe", "dve", "act", "pool", "sp")

    def __init__(self, nc, es):
        self.nc = nc
        self.es = es
        self.sem = {k: es.enter_context(nc.semaphore("s_" + k)) for k in self.ENG}
        self.cnt = {k: 0 for k in self.ENG}
        self.seen = {k: {} for k in self.ENG}
        self.prog = {k: [] for k in self.ENG}
        self.dsem = {}
        self.dcnt = {}

    def _wait(self, e, tok):
        if tok is None:
            return
        key, val = tok
        if key == e and e == "pe":
            return
        if self.seen[e].get(key, 0) >= val:
            return
        sem = self.sem[key] if key in self.sem else self.dsem[key]
        self.prog[e].append(("w", sem, val))
        self.seen[e][key] = val

    def _deps(self, e, reads, writes):
        for r in reads:
            self._wait(e, r.w)
        for w in writes:
            self._wait(e, w.w)
            for k, v in w.r.items():
                self._wait(e, (k, v))

    def _commit(self, tok, reads, writes):
        for r in reads:
            if r.r.get(tok[0], 0) < tok[1]:
                r.r[tok[0]] = tok[1]
        for w in writes:
            w.w = tok
            w.r = {}

    def op(self, e, fn, reads=(), writes=()):
        self._deps(e, reads, writes)
        self.cnt[e] += 1
        self.prog[e].append(("i", fn, self.sem[e], 1))
        tok = (e, self.cnt[e])
        self._commit(tok, reads, writes)
        return tok

    def dma(self, e, chan, out, in_, reads=(), writes=()):
        if chan not in self.dsem:
            self.dsem[chan] = self.es.enter_context(self.nc.semaphore("dm_" + chan))
            self.dcnt[chan] = 0
        self._deps(e, reads, writes)
        self.dcnt[chan] += 16
        self.prog[e].append(("i", lambda eng: eng.dma_start(out=out, in_=in_), self.dsem[chan], 16))
        tok = (chan, self.dcnt[chan])
        self._commit(tok, reads, writes)
        return tok

    def idma(self, chan, out, out_off, in_, in_off, reads=(), writes=(), bound=None):
        e = "pool"
        if chan not in self.dsem:
            self.dsem[chan] = self.es.enter_context(self.nc.semaphore("dm_" + chan))
            self.dcnt[chan] = 0
        self._deps(e, reads, writes)
        self.dcnt[chan] += 16
        oo = None if out_off is None else bass.IndirectOffsetOnAxis(ap=out_off, axis=0)
        io = None if in_off is None else bass.IndirectOffsetOnAxis(ap=in_off, axis=0)
        if bound is None:
            self.prog[e].append(("i", lambda eng: eng.indirect_dma_start(out=out, out_offset=oo, in_=in_, in_offset=io),
                                 self.dsem[chan], 16))
        else:
            self.bound_val = bound
            self.prog[e].append(("i", lambda eng: eng.indirect_dma_start(out=out, out_offset=oo, in_=in_, in_offset=io,
                                                                         bounds_check=self.bound_reg, oob_is_err=False),
                                 self.dsem[chan], 16))
        tok = (chan, self.dcnt[chan])
        self._commit(tok, reads, writes)
        return tok

    def wait(self, e, tok):
        self._wait(e, tok)

    def fence(self):
        toks = [(k, self.cnt[k]) for k in self.ENG if self.cnt[k] > 0]
        toks += [(k, v) for k, v in self.dcnt.items() if v > 0]
        for e in self.ENG:
            for t in toks:
                if t[0] == e:
                    continue
                self._wait(e, t)

    def emit(self):
        nc = self.nc
        with nc.Block() as block:
            def mk(e):
                def f(eng):
                    if e == "pool" and getattr(self, "bound_val", None) is not None:
                        self.bound_reg = eng.alloc_register("oob_bound")
                        eng.reg_mov(self.bound_reg, int(self.bound_val))
                    for it in self.prog[e]:
                        if it[0] == "w":
                            eng.wait_ge(it[1], it[2])
                        else:
                            it[1](eng).then_inc(it[2], it[3])
                return f
            block.tensor(mk("pe"))
            block.vector(mk("dve"))
            block.scalar(mk("act"))
            block.gpsimd(mk("pool"))
            block.sync(mk("sp"))


def build(NSEQ, n_exp=NE, dbg=False):
    nc = bass.Bass("TRN2", target_bir_lowering=False)

    def din(name, shape):
        return nc.dram_tensor(name, list(shape), F32, kind="ExternalInput").ap()

    xT_d = din("xT", [NSEQ, 128, 8, S])
    cT_d = din("cT", [128, 8, NSEQ])
    wada_d = din("w_ada", [48, 128, 8, 128])
    bada_d = din("b_adaT", [128, 48])
    n1g_d = din("n1g", [128, 8])
    n2g_d = din("n2g", [128, 8])
    win_d = din("w_in", [48, 128, 8, 128])
    qkg_d = din("qkg", [128, 2])
    lam_d = din("lam_in", [128, 4, 64])
    sg_d = din("subln_g", [128, 128])
    pw_d = din("pool_w", [4, 2, 128, 2, 128])
    psc_d = din("pool_scaleT", [128, 8])
    rc_d = din("pool_rc", [128, 4, 16])
    wo_d = din("w_out", [8, 128, 8, 128])
    wr_d = din("w_router", [128, 8, NE])
    br_d = din("b_router", [128, NE])
    wg_d = din("w_eg", [NE + 1, 128, 8, 256])
    wu_d = din("w_eu", [NE + 1, 128, 8, 256])
    wd_d = din("w_ed", [NE + 1, 128, 2, 1024])
    tab_d = din("rel_tab", [NB, NH])
    oh_d = din("bias_oh", [NB, FVW])
    idt_d = din("ident", [128, 128])
    aid_d = din("antiid", [128, 128])
    bones_d = din("blockones", [128, 128])
    out_d = nc.dram_tensor("outT", [NSEQ, 128, 8, S], F32, kind="ExternalOutput").ap()
    fv_d = nc.dram_tensor("fv_scr", [NH, FVW], F32, kind="Internal").ap()
    mst_d = nc.dram_tensor("mst_scr", [NH, 128, MW], F32, kind="Internal").ap()
    T = NSEQ * S
    NT = T // 128
    NBLK = T * 8 // 128 + NE
    NSLOT = NBLK * 128
    ls_d = din("lstrict", [128, 128])
    us_d = din("ustrict", [2, 128, NE])
    ui_d = din("uincl", [2, 128, NE])
    iot_d = din("iota_row", [128, 1024])
    pio_d = din("piota8", [128, 8])
    U32 = mybir.dt.uint32
    I32 = mybir.dt.int32
    h2_d = nc.dram_tensor("h2_scr", [T, D], BF16, kind="Internal").ap()
    pos_d = nc.dram_tensor("pos_scr", [NT, 128, NE], F32, kind="Internal").ap()
    wt_d = nc.dram_tensor("wt_scr", [NT, 128, NE], F32, kind="Internal").ap()
    xs_d = nc.dram_tensor("xs_scr", [NSEQ, 128, 8, S], F32, kind="ExternalOutput").ap() if dbg else out_d
    slot_d = nc.dram_tensor("slot_scr", [NSLOT, 2], F32, kind="Internal").ap()
    y_d = nc.dram_tensor("y_scr", [NSLOT, D], BF16, kind="Internal").ap()
    wg2 = wg_d.rearrange("e p a b -> (e p) (a b)")
    wu2 = wu_d.rearrange("e p a b -> (e p) (a b)")
    wd2 = wd_d.rearrange("e p a b -> (e p) (a b)")

    with ExitStack() as es:
        es.enter_context(nc.allow_low_precision("bf16 matmul operands, fp32 accumulation"))
        es.enter_context(nc.allow_non_contiguous_dma("overlapping-window bias load"))
        sc = Sched(nc, es)

        def sb(name, shape, dt=F32):
            return es.enter_context(nc.sbuf_tensor(name, list(shape), dt))

        CW = 256
        arA = sb("arA", [128, 16384])
        arB = sb("arB", [128, 8320])

        def cvA(off, n, dt=F32):
            return arA[:, off:off + n] if dt == F32 else arA[:, off:off + n].bitcast(dt)

        def cvB(off, n, dt=F32):
            return arB[:, off:off + n] if dt == F32 else arB[:, off:off + n].bitcast(dt)

        bigA = arA[:, :].rearrange("p (a b) -> p a b", a=8)
        mT = cvA(0, 8192, BF16).rearrange("p (a b) -> p a b", a=8)
        qz = [cvA(8192, 1024, BF16), cvB(0, 1024, BF16)]
        kT = cvA(9216, 1024, BF16)
        vA = cvA(10240, 1040, BF16).rearrange("p (a b) -> p a b", a=16)
        gaT = cvA(11280, 1024, BF16)
        mst = cvA(12304, 1280)
        O = [cvA(13584, 520).rearrange("p (a b) -> p a b", a=4), cvA(14104, 520).rearrange("p (a b) -> p a b", a=4)]
        sadd = [cvA(14624, 512), cvA(15136, 512)]
        hank = cvA(0, 1280)
        oh = arA[0:NB, 1280:1280 + FVW]
        fvs = arA[0:NH, 2688:2688 + FVW]
        wadaS = [cvA(4096, 1024).rearrange("p (a b) -> p a b", a=8), cvA(5120, 1024).rearrange("p (a b) -> p a b", a=8)]
        lam_in = cvA(6144, 256).rearrange("p (a b) -> p a b", a=4)
        lamt = cvA(6400, 256).rearrange("p (a b) -> p a b", a=4)
        sil = cvB(0, 1024).rearrange("p (a b) -> p a b", a=2)
        aT = cvB(1024, 512, BF16).rearrange("p (a b) -> p a b", a=2)
        scr = cvB(1536, 256)
        bia = cvB(1792, 256)
        msk = cvB(2048, 256)
        Wtok = [cvB(2304, 256), cvB(2560, 256)]
        selT2 = [cvB(2816, 256), cvB(4736, 256)]
        posS = [cvB(3072, 256), cvB(3328, 256)]
        m8 = cvB(3584, 64).rearrange("p (a b) -> p a b", a=8)
        gsc = cvB(3648, 8)
        gm8 = cvB(3656, 8)
        gmk = cvB(3664, 8)
        t8 = cvB(3672, 8)
        den = cvB(3680, 2)
        h2tm = [cvB(3712, 512, BF16), cvB(4224, 512, BF16)]
        pP = cvB(0, LP)
        pW = [cvB(2080, LP), cvB(4160, LP)]
        mixT = cvB(6240, 2048, BF16).rearrange("p (a b) -> p a b", a=2)
        nbf = cvB(0, 256)
        nbi = cvB(256, 256, I32)
        sbase = cvB(512, 256)
        nbT = [cvB(768, 128), cvB(896, 128)]
        bend = cvB(1024, 2)
        Cm = [cvB(1032, NBLK), cvB(1032 + NBLK, NBLK)]
        bef = cvB(1032 + 2 * NBLK, NBLK)
        o_b = 1032 + 3 * NBLK
        posL = [cvB(o_b, 256), cvB(o_b + 256, 256)]
        wtL = [cvB(o_b + 512, 256), cvB(o_b + 768, 256)]
        dp1 = cvB(o_b + 1024, 256)
        keyb = cvB(o_b + 1280, 256)
        junkb = cvB(o_b + 1536, 256)
        d8 = cvB(o_b + 1792, 8)
        rows = [cvB(o_b + 1800, 16).rearrange("p (a b) -> p a b", a=8), cvB(o_b + 1816, 16).rearrange("p (a b) -> p a b", a=8)]
        zslot = cvB(o_b + 1832, NSLOT * 2 // 128)
        assert o_b + 1832 + NSLOT * 2 // 128 <= 8320
        wgC = [cvA(3072 * i, 1024, BF16) for i in range(3)]
        wuC = [cvA(3072 * i + 1024, 1024, BF16) for i in range(3)]
        wdC = [cvA(3072 * i + 2048, 1024, BF16).rearrange("p (a b) -> p a b", a=2) for i in range(3)]
        xgC = [cvA(9216 + 512 * i, 512, BF16) for i in range(3)]
        xgT = [cvA(10752 + 512 * i, 512, BF16).rearrange("p (a b) -> p a b", a=8) for i in range(2)]
        silC = [cvA(11776, 256), cvA(12032, 256)]
        aTC = [cvA(12288, 128, BF16).rearrange("p (a b) -> p a b", a=2), cvA(12416, 128, BF16).rearrange("p (a b) -> p a b", a=2)]
        ysb = [cvA(12544, 512, BF16), cvA(13056, 512, BF16)]
        wA = cvA(13568, NBLK)
        tokuA = cvA(13568 + NBLK, NBLK, U32)
        sl6 = [cvA(13568 + 2 * NBLK, 256), cvA(13568 + 2 * NBLK + 256, 256)]
        sl6c = [cvA(13568 + 2 * NBLK + 512, 128), cvA(13568 + 2 * NBLK + 640, 128)]
        assert 13568 + 2 * NBLK + 768 <= 16384 and NBLK % 128 == 0
        yg = [cvA(4096 * i, 4096, BF16).rearrange("p (a b) -> p a b", a=8) for i in range(3)]
        accD = [cvA(12288, 1024), cvA(13312, 1024)]

        hT = sb("hT", [128, 8, S], BF16)
        tmpF = sb("tmpF", [128, 8, CW])
        xch = sb("xch", [128, 8, CW])
        sqc = [sb("sqc%d" % i, [128, CW]) for i in range(2)]
        rstd = sb("rstd", [128, CW])
        modT = sb("modT", [128, 48, NSEQ])
        bada = sb("bada", [128, 48])
        n1g = sb("n1g_s", [128, 8])
        n2g = sb("n2g_s", [128, 8])
        A1 = sb("A1", [128, 8])
        A2 = sb("A2", [128, 8])
        cT = sb("cT_s", [128, 8, NSEQ])
        scT = sb("scT", [128, 8, NSEQ])
        winS = [sb("win%d" % i, [128, 8, 128], BF16) for i in range(2)]
        qkg = sb("qkg_s", [128, 2])
        lamv = sb("lamv", [128, 4])
        nlam = sb("nlam", [128, 1])
        sg = sb("sg_s", [128, 128])
        pwS = sb("pw_s", [128, 2, 2, 128], BF16)
        psc = sb("psc_s", [128, 8])
        rc = sb("rc_s", [128, 4, 16])
        woS = [sb("wo%d" % i, [128, 8, 128], BF16) for i in range(2)]
        wr = sb("wr_s", [128, 8, NE])
        wsg = sb("wsg", [128, 8, 256], BF16)
        wsu = sb("wsu", [128, 8, 256], BF16)
        wsd = sb("wsd", [128, 2, 1024], BF16)
        base = sb("base", [128, NE])
        lsm = sb("lsm", [128, 128])
        idtb = sb("idtb", [128, 128], BF16)
        pio8 = sb("pio8", [128, 8])
        dstu = sb("dstu", [128, NT, 8], U32)
        idxW = sb("idxW", [128, NBLK], U32)
        brt = sb("br_s", [128, NE])
        tab = sb("tab_s", [NB, NH])
        idt = sb("idt", [128, 128])
        aid = sb("aid", [128, 128])
        bones = sb("bones", [128, 128])
        ones = sb("ones", [128, 128])
        sqh2 = [sb("sqh%d" % i, [128, 512]) for i in range(2)]
        rsh2 = [sb("rsh%d" % i, [128, 512]) for i in range(2)]
        ET = [sb("ET%d" % i, [128, 512], BF16) for i in range(3)]
        rr = sb("rr", [128, 8])
        att4 = sb("att4", [128, 4, 128])
        att4b = sb("att4b", [128, 4, 128])
        ssq4 = sb("ssq4", [128, 8])
        gpT = sb("gpT", [128, 512])
        ptmp = sb("ptmp", [128, 512])

        pb = [es.enter_context(nc.psum_tensor("pb%d" % i, [128, 512], F32)) for i in range(8)]
        rpb = [Res() for _ in range(8)]

        R = {}

        def res(name):
            if name not in R:
                R[name] = Res()
            return R[name]

        rbigA = [Res() for _ in range(4)]
        rhT = [Res() for _ in range(8)]
        rmT = [[Res() for _ in range(4)] for _ in range(8)]

        def load(dst, src, name, eng="sp"):
            sc.dma(eng, "ld_" + name, dst, src, writes=[res(name)])

        load(bada[:], bada_d, "bada")
        load(n1g[:], n1g_d, "n1g")
        load(n2g[:], n2g_d, "n2g")
        load(cT[:], cT_d, "cT")
        load(qkg[:], qkg_d, "qkg")
        load(lam_in[:], lam_d, "lam_in")
        load(sg[:], sg_d, "sg")
        load(psc[:], psc_d, "psc")
        load(rc[:], rc_d, "rc")
        load(wr[:], wr_d, "wr")
        load(brt[:], br_d, "brt")
        load(tab[:], tab_d, "tab")
        load(oh[:], oh_d, "oh")
        load(idt[:], idt_d, "idt")
        load(aid[:], aid_d, "aid")
        load(bones[:], bones_d, "bones")
        load(lsm[:], ls_d, "lsm")
        load(pio8[:], pio_d, "pio8")
        sc.dma("pool", "ld_wsg", wsg[:], wg_d[NE], writes=[res("wsg")])
        sc.dma("pool", "ld_wsu", wsu[:], wu_d[NE], writes=[res("wsu")])
        sc.dma("pool", "ld_wsd", wsd[:], wd_d[NE], writes=[res("wsd")])
        sc.op("dve", lambda e: e.memset(base[:], 0.0), writes=[res("base")])
        sc.op("dve", lambda e: e.tensor_copy(out=idtb[:], in_=idt[:]), reads=[res("idt")], writes=[res("idtb")])
        sc.op("dve", lambda e: e.memset(ones[:], 1.0), writes=[res("ones")])
        sc.op("dve", lambda e: e.tensor_scalar(sg[:], sg[:], float(1.0 - LAM_INIT), None, ALU.mult),
              reads=[res("sg")], writes=[res("sg")])
        sc.op("dve", lambda e: e.tensor_tensor(lamt[:, 0, :], lam_in[:, 0, :], lam_in[:, 1, :], ALU.mult),
              reads=[res("lam_in")], writes=[res("lamt")])
        sc.op("dve", lambda e: e.tensor_tensor(lamt[:, 1, :], lam_in[:, 2, :], lam_in[:, 3, :], ALU.mult),
              reads=[res("lam_in"), res("lamt")], writes=[res("lamt")])
        sc.op("dve", lambda e: e.tensor_reduce(lamv[:, 0:2], lamt[:, 0:2, :], AX.X, ALU.add),
              reads=[res("lamt")], writes=[res("lamv")])
        sc.op("act", lambda e: e.activation(lamv[:, 2:4], lamv[:, 0:2], AF.Exp),
              reads=[res("lamv")], writes=[res("lamv")])
        sc.op("dve", lambda e: e.scalar_tensor_tensor(nlam[:], lamv[:, 3:4], float(-LAM_INIT), lamv[:, 2:3],
                                                      ALU.add, ALU.subtract),
              reads=[res("lamv")], writes=[res("nlam")])

        sc.op("act", lambda e: e.activation(scT[:], cT[:], AF.Silu), reads=[res("cT")], writes=[res("scT")])
        for fc in range(48):
            wb = wadaS[fc % 2]
            rw = res("wada%d" % (fc % 2))
            sc.dma("sp", "ld_wada%d" % (fc % 2), wb[:], wada_d[fc], writes=[rw])
            for kc in range(8):
                sc.op("pe", lambda e, wb=wb, kc=kc: e.matmul(pb[7][:, 0:NSEQ], lhsT=wb[:, kc, :], rhs=scT[:, kc, :],
                                                              start=(kc == 0), stop=(kc == 7)),
                      reads=[rw, res("scT")], writes=[rpb[7]])
            sc.op("dve", lambda e, fc=fc: e.tensor_scalar(modT[:, fc, :], pb[7][:, 0:NSEQ], bada[:, fc:fc + 1], None,
                                                           ALU.add),
                  reads=[rpb[7], res("bada")], writes=[res("modT")])

        for c0 in range(0, FVW, 512):
            cw = min(512, FVW - c0)
            sc.op("pe", lambda e, c0=c0, cw=cw: e.matmul(pb[7][0:NH, 0:cw], lhsT=tab[:, :], rhs=oh[:, c0:c0 + cw],
                                                          start=True, stop=True),
                  reads=[res("tab"), res("oh")], writes=[rpb[7]])
            sc.op("dve", lambda e, c0=c0, cw=cw: e.tensor_copy(out=fvs[:, c0:c0 + cw], in_=pb[7][0:NH, 0:cw]),
                  reads=[rpb[7]], writes=[res("fvs")])
        sc.dma("sp", "st_fv", fv_d, fvs[:], reads=[res("fvs")], writes=[res("fv_d")])
        for h in range(NH):
            src = bass.AP(fv_d.tensor, h * FVW, [[1, 128], [1, MW]])
            sc.dma("sp", "ld_hank", hank[:], src, reads=[res("fv_d")], writes=[res("hank")])
            for c0 in range(0, MW, 512):
                cw = min(512, MW - c0)
                sc.op("pe", lambda e, c0=c0, cw=cw: e.matmul(pb[7][:, 0:cw], lhsT=aid[:], rhs=hank[:, c0:c0 + cw],
                                                              start=True, stop=True),
                      reads=[res("aid"), res("hank")], writes=[rpb[7]])
                sc.op("dve", lambda e, c0=c0, cw=cw: e.tensor_copy(out=mst[:, c0:c0 + cw], in_=pb[7][:, 0:cw]),
                      reads=[rpb[7]], writes=[res("mst")])
            sc.dma("sp", "st_mst", mst_d[h], mst[:], reads=[res("mst")], writes=[res("mst_d")])

        win_ctr = [0]

        def load_win(chunk):
            i = win_ctr[0] % 2
            win_ctr[0] += 1
            sc.dma("pool", "ld_win%d" % i, winS[i][:], win_d[chunk], writes=[res("win%d" % i)])
            return winS[i], res("win%d" % i)

        def rmsnorm_chunk(Acol, shcol0, s, ts, rdst, f32_out):
            for c in range(8):
                q = sqc[c % 2]
                rq = res("sqc%d" % (c % 2))
                sc.op("act", lambda e, c=c, q=q: e.activation(q[:], xch[:, c, :], AF.Square),
                      reads=[res("xch")], writes=[rq])
                sc.op("pe", lambda e, c=c, q=q: e.matmul(pb[7][:, 0:CW], lhsT=ones[:], rhs=q[:],
                                                         start=(c == 0), stop=(c == 7)),
                      reads=[res("ones"), rq], writes=[rpb[7]])
            sc.op("act", lambda e: e.activation(rstd[:], pb[7][:, 0:CW], AF.Sqrt, bias=float(EPS), scale=1.0 / D),
                  reads=[rpb[7]], writes=[res("rstd")])
            sc.op("dve", lambda e: e.reciprocal(rstd[:], rstd[:]), reads=[res("rstd")], writes=[res("rstd")])
            for c in range(8):
                sc.op("dve", lambda e, c=c: e.scalar_tensor_tensor(
                    tmpF[:, c, :], xch[:, c, :], Acol[:, c:c + 1], rstd[:], ALU.mult, ALU.mult),
                    reads=[res("xch"), res("rstd"), res("Acol")], writes=[res("tmpF%d" % c)])
                if not f32_out:
                    sc.op("act", lambda e, c=c: e.activation(
                        hT[:, c, ts], tmpF[:, c, :], AF.Identity, bias=modT[:, shcol0 + c, s:s + 1], scale=1.0),
                        reads=[res("tmpF%d" % c), res("modT")], writes=[rdst])
                else:
                    sc.op("act", lambda e, c=c: e.activation(
                        tmpF[:, c, :], tmpF[:, c, :], AF.Identity, bias=modT[:, shcol0 + c, s:s + 1], scale=1.0),
                        reads=[res("tmpF%d" % c), res("modT")], writes=[res("tmpF%d" % c)])
                    sc.op("dve", lambda e, c=c: e.tensor_copy(out=hT[:, c, ts], in_=tmpF[:, c, :]),
                          reads=[res("tmpF%d" % c)], writes=[rdst])

        rtmpF = [res("tmpF%d" % c) for c in range(8)]

        for s in range(NSEQ):
            sc.fence()
            sc.op("dve", lambda e: e.memset(vA[:], 1.0), writes=[res("vA")])
            sc.op("dve", lambda e: e.memset(qz[0][64:128, :], 0.0), writes=[res("qT")])
            sc.op("dve", lambda e: e.memset(qz[1][0:64, :], 0.0), writes=[res("qT")])
            sc.op("dve", lambda e, s=s: e.scalar_tensor_tensor(A1[:], modT[:, 8:16, s], 1.0, n1g[:], ALU.add, ALU.mult),
                  reads=[res("modT"), res("n1g")], writes=[res("Acol")])
            for j in range(8):
                ts = slice(j * CW, (j + 1) * CW)
                sc.dma("sp", "ld_x", xch[:], xT_d[s][:, :, ts], writes=[res("xch")])
                rmsnorm_chunk(A1, 0, s, ts, rhT[j], False)

            for h in range(NH):
                sc.dma("sp", "ld_mst", mst[:], mst_d[h], reads=[res("mst_d")], writes=[res("mst")])
                for which, dstT, rname in ((0, None, "qT"), (1, kT, "kT")):
                    wb, rw = load_win(which * 8 + h)
                    for j in range(4):
                        ts = slice(j * 512, (j + 1) * 512)
                        pa = 4 + 2 * (j % 2)
                        pn = pa + 1
                        sq_, rs_ = sqh2[j % 2], rsh2[j % 2]
                        rsq, rrs = res("sqh%d" % (j % 2)), res("rsh%d" % (j % 2))
                        for kc in range(8):
                            sc.op("pe", lambda e, wb=wb, kc=kc, ts=ts, pa=pa: e.matmul(
                                pb[pa][:], lhsT=wb[:, kc, :], rhs=hT[:, kc, ts], start=(kc == 0), stop=(kc == 7)),
                                reads=[rw, rhT[2 * j], rhT[2 * j + 1]], writes=[rpb[pa]])
                        sc.op("act", lambda e, pa=pa, sq_=sq_: e.activation(sq_[:], pb[pa][:], AF.Square),
                              reads=[rpb[pa]], writes=[rsq])
                        sc.op("pe", lambda e, pn=pn, sq_=sq_: e.matmul(pb[pn][:], lhsT=bones[:], rhs=sq_[:], start=True, stop=True),
                              reads=[res("bones"), rsq], writes=[rpb[pn]])
                        if which == 0:
                            sc.op("act", lambda e, pn=pn, rs_=rs_: e.activation(rs_[:], pb[pn][:], AF.Sqrt, bias=float(64 * EPS), scale=1.0),
                                  reads=[rpb[pn]], writes=[rrs])
                        else:
                            sc.op("act", lambda e, pn=pn, rs_=rs_: e.activation(rs_[:], pb[pn][:], AF.Sqrt, bias=float(EPS), scale=1.0 / 64),
                                  reads=[rpb[pn]], writes=[rrs])
                        sc.op("dve", lambda e, rs_=rs_: e.reciprocal(rs_[:], rs_[:]), reads=[rrs], writes=[rrs])
                        if which == 1:
                            sc.op("dve", lambda e, dstT=dstT, ts=ts, which=which, pa=pa, rs_=rs_: e.scalar_tensor_tensor(
                                dstT[:, ts], pb[pa][:], qkg[:, which:which + 1], rs_[:], ALU.mult, ALU.mult),
                                reads=[rpb[pa], rrs, res("qkg")], writes=[res(rname)])
                        else:
                            for comp in range(2):
                                pr = slice(comp * 64, (comp + 1) * 64)
                                sc.op("dve", lambda e, ts=ts, pa=pa, rs_=rs_, comp=comp, pr=pr: e.scalar_tensor_tensor(
                                    qz[comp][pr, ts], pb[pa][pr, :], qkg[pr, 0:1], rs_[pr, :], ALU.mult, ALU.mult),
                                    reads=[rpb[pa], rrs, res("qkg")], writes=[res(rname)])
                wb, rw = load_win(16 + h)
                for t in range(16):
                    tt = slice(t * 128, (t + 1) * 128)
                    vb = 4 + (t % 4)
                    for kc in range(8):
                        sc.op("pe", lambda e, wb=wb, kc=kc, tt=tt, vb=vb: e.matmul(
                            pb[vb][:, 0:128], lhsT=hT[:, kc, tt], rhs=wb[:, kc, :], start=(kc == 0), stop=(kc == 7)),
                            reads=[rw, rhT[t // 2]], writes=[rpb[vb]])
                    if t % 2 == 0:
                        sc.op("act", lambda e, t=t, vb=vb: e.activation(vA[:, t, 0:128], pb[vb][:, 0:128], AF.Copy),
                              reads=[rpb[vb]], writes=[res("vA")])
                    else:
                        sc.op("dve", lambda e, t=t, vb=vb: e.tensor_copy(out=vA[:, t, 0:128], in_=pb[vb][:, 0:128]),
                              reads=[rpb[vb]], writes=[res("vA")])
                wb, rw = load_win(32 + h)
                for j in range(4):
                    ts = slice(j * 512, (j + 1) * 512)
                    gb = 4 + j
                    for kc in range(8):
                        sc.op("pe", lambda e, wb=wb, kc=kc, ts=ts, gb=gb: e.matmul(
                            pb[gb][:], lhsT=wb[:, kc, :], rhs=hT[:, kc, ts], start=(kc == 0), stop=(kc == 7)),
                            reads=[rw, rhT[2 * j], rhT[2 * j + 1]], writes=[rpb[gb]])
                    sc.op("act", lambda e, ts=ts, gb=gb: e.activation(gaT[:, ts], pb[gb][:], AF.Sigmoid),
                          reads=[rpb[gb]], writes=[res("gaT")])
                tiles = [(j, comp, kb) for j in range(4) for comp in range(2) for kb in range(16)]

                def emit_st(n):
                    j, comp, kb = tiles[n]
                    bi = 4 + (n % 3)
                    ks = slice(kb * 128, (kb + 1) * 128)
                    qs = slice(j * 512, (j + 1) * 512)
                    sc.op("pe", lambda e, bi=bi, comp=comp, ks=ks, qs=qs: e.matmul(
                        pb[bi][:], lhsT=kT[:, ks], rhs=qz[comp][:, qs], start=True, stop=True),
                        reads=[res("qT"), res("kT")], writes=[rpb[bi]])

                emit_st(0)
                emit_st(1)
                for n, (j, comp, kb) in enumerate(tiles):
                    if n + 2 < len(tiles):
                        emit_st(n + 2)
                    bi = 4 + (n % 3)
                    ei = n % 3
                    o = kb - 4 * j
                    rET = res("ET%d" % ei)
                    if o <= -2 or o >= 5:
                        col = (MW - 1) if o <= -2 else 0
                        sc.op("act", lambda e, bi=bi, ei=ei, col=col: e.activation(
                            ET[ei][:], pb[bi][:], AF.Exp, bias=mst[:, col:col + 1], scale=1.0),
                            reads=[rpb[bi], res("mst")], writes=[rET])
                    else:
                        m0 = 640 - 128 * o
                        si = n % 2
                        rsa = res("sadd%d" % si)
                        sc.op("dve", lambda e, bi=bi, si=si, m0=m0: e.tensor_tensor(
                            sadd[si][:], pb[bi][:], mst[:, m0:m0 + 512], ALU.add),
                            reads=[rpb[bi], res("mst")], writes=[rsa])
                        sc.op("act", lambda e, ei=ei, si=si: e.activation(ET[ei][:], sadd[si][:], AF.Exp),
                              reads=[rsa], writes=[rET])
                    for sub in range(4):
                        sc.op("pe", lambda e, sub=sub, ei=ei, kb=kb: e.matmul(
                            pb[sub][:, 0:129], lhsT=ET[ei][:, sub * 128:(sub + 1) * 128], rhs=vA[:, kb, 0:129],
                            start=(kb == 0), stop=(kb == 15)),
                            reads=[rET, res("vA")], writes=[rpb[sub]])
                    if kb == 15:
                        for sub in range(4):
                            eng = "act" if sub % 2 == 0 else "dve"
                            if eng == "act":
                                sc.op("act", lambda e, sub=sub, comp=comp: e.activation(
                                    O[comp][:, sub, 0:129], pb[sub][:, 0:129], AF.Copy),
                                    reads=[rpb[sub]], writes=[res("O%d" % comp)])
                            else:
                                sc.op("dve", lambda e, sub=sub, comp=comp: e.tensor_copy(
                                    out=O[comp][:, sub, 0:129], in_=pb[sub][:, 0:129]),
                                    reads=[rpb[sub]], writes=[res("O%d" % comp)])
                    if kb == 15 and comp == 1:
                        ts = slice(j * 512, (j + 1) * 512)
                        sc.op("dve", lambda e: e.reciprocal(rr[:, 0:4], O[0][:, :, 128]),
                              reads=[res("O0")], writes=[res("rr")])
                        sc.op("dve", lambda e: e.reciprocal(rr[:, 4:8], O[1][:, :, 128]),
                              reads=[res("O1"), res("rr")], writes=[res("rr")])
                        sc.op("dve", lambda e: e.tensor_scalar(rr[:, 4:8], rr[:, 4:8], nlam[:, 0:1], None, ALU.mult),
                              reads=[res("rr"), res("nlam")], writes=[res("rr")])
                        sc.op("dve", lambda e: e.tensor_tensor(
                            att4[:, :, :], O[0][:, :, 0:128], rr[:, 0:4].unsqueeze(2).to_broadcast([128, 4, 128]), ALU.mult),
                            reads=[res("O0"), res("rr")], writes=[res("att4")])
                        sc.op("dve", lambda e: e.tensor_tensor(
                            att4b[:, :, :], O[1][:, :, 0:128], rr[:, 4:8].unsqueeze(2).to_broadcast([128, 4, 128]), ALU.mult),
                            reads=[res("O1"), res("rr")], writes=[res("att4b")])
                        sc.op("dve", lambda e: e.tensor_tensor(att4[:, :, :], att4[:, :, :], att4b[:, :, :], ALU.add),
                              reads=[res("att4"), res("att4b")], writes=[res("att4")])
                        sc.op("dve", lambda e: e.tensor_tensor(att4b[:, :, :], att4[:, :, :], att4[:, :, :], ALU.mult),
                              reads=[res("att4")], writes=[res("att4b")])
                        sc.op("dve", lambda e: e.tensor_reduce(ssq4[:, 0:4], att4b[:, :, :], AX.X, ALU.add),
                              reads=[res("att4b")], writes=[res("ssq4")])
                        sc.op("act", lambda e: e.activation(ssq4[:, 4:8], ssq4[:, 0:4], AF.Sqrt, bias=float(EPS), scale=1.0 / 128),
                              reads=[res("ssq4")], writes=[res("ssq4")])
                        sc.op("dve", lambda e: e.reciprocal(ssq4[:, 4:8], ssq4[:, 4:8]),
                              reads=[res("ssq4")], writes=[res("ssq4")])
                        sc.op("dve", lambda e: e.tensor_tensor(
                            att4[:, :, :], att4[:, :, :], ssq4[:, 4:8].unsqueeze(2).to_broadcast([128, 4, 128]), ALU.mult),
                            reads=[res("att4"), res("ssq4")], writes=[res("att4")])
                        sc.op("dve", lambda e: e.tensor_tensor(
                            att4[:, :, :], att4[:, :, :], sg[:, :].unsqueeze(1).to_broadcast([128, 4, 128]), ALU.mult),
                            reads=[res("att4"), res("sg")], writes=[res("att4")])
                        for sub in range(4):
                            sc.op("pe", lambda e, sub=sub: e.transpose(pb[7][:, sub * 128:(sub + 1) * 128], att4[:, sub, :], idt[:]),
                                  reads=[res("att4"), res("idt")], writes=[rpb[7]])
                        sc.op("dve", lambda e, ts=ts, h=h: e.tensor_tensor(mT[:, h, ts], pb[7][:, :], gaT[:, ts], ALU.mult),
                              reads=[rpb[7], res("gaT")], writes=[rmT[h][j]])

            sc.fence()
            sc.op("dve", lambda e: e.memset(pP[:], 0.0), writes=[res("pP")])
            for g in range(4):
                wnd = (2, 4, 8, 16)[g]
                for dc in range(2):
                    sc.dma("pool", "ld_pw", pwS[:, dc, :, :], pw_d[g, dc], writes=[res("pw")])
                for cc in range(2):
                    chunk = 2 * g + cc
                    wb, rw = load_win(24 + chunk)
                    for j in range(4):
                        ts = slice(j * 512, (j + 1) * 512)
                        for kc in range(8):
                            sc.op("pe", lambda e, wb=wb, kc=kc, ts=ts: e.matmul(
                                pb[6][:], lhsT=wb[:, kc, :], rhs=hT[:, kc, ts], start=(kc == 0), stop=(kc == 7)),
                                reads=[rw, rhT[2 * j], rhT[2 * j + 1]], writes=[rpb[6]])
                        sc.op("act", lambda e, j=j: e.activation(pP[:, PADL + j * 512:PADL + (j + 1) * 512], pb[6][:],
                                                                 AF.Copy),
                              reads=[rpb[6]], writes=[res("pP")])
                    L = LP
                    sc.op("dve", lambda e: e.tensor_tensor(pW[0][:, 1:L], pP[:, 0:L - 1], pP[:, 1:L], ALU.add),
                          reads=[res("pP")], writes=[res("pW0")])
                    cur = 0
                    for lvl, sh in ((4, 1), (8, 2), (16, 4)):
                        if wnd < lvl:
                            break
                        nxt = 1 - cur
                        sc.op("dve", lambda e, cur=cur, nxt=nxt, sh=sh: e.tensor_tensor(
                            pW[nxt][:, sh:L - sh], pW[cur][:, 0:L - 2 * sh], pW[cur][:, 2 * sh:L], ALU.add),
                            reads=[res("pW%d" % cur)], writes=[res("pW%d" % nxt)])
                        cur = nxt
                    Wc = pW[cur]
                    rWc = res("pW%d" % cur)
                    sc.op("dve", lambda e, Wc=Wc, cc=cc, wnd=wnd: e.scalar_tensor_tensor(
                        mixT[:, cc, :], Wc[:, PADL:PADL + S], 1.0 / wnd, pP[:, PADL:PADL + S], ALU.mult, ALU.subtract),
                        reads=[rWc, res("pP")], writes=[res("mixT")])
                    for (c0, r0) in ((0, 0), (S - 8, 8)):
                        sc.op("dve", lambda e, Wc=Wc, c0=c0, r0=r0, g=g: e.tensor_tensor(
                            ptmp[:, 0:8], Wc[:, PADL + c0:PADL + c0 + 8], rc[:, g, r0:r0 + 8], ALU.mult),
                            reads=[rWc, res("rc")], writes=[res("ptmp")])
                        sc.op("dve", lambda e, c0=c0, cc=cc: e.tensor_tensor(
                            mixT[:, cc, c0:c0 + 8], ptmp[:, 0:8], pP[:, PADL + c0:PADL + c0 + 8], ALU.subtract),
                            reads=[res("ptmp"), res("pP"), res("mixT")], writes=[res("mixT")])
                for dc in range(2):
                    chunk = 2 * g + dc
                    wb, rw = load_win(40 + chunk)
                    for j in range(4):
                        ts = slice(j * 512, (j + 1) * 512)
                        for kc in range(8):
                            sc.op("pe", lambda e, wb=wb, kc=kc, ts=ts: e.matmul(
                                pb[6][:], lhsT=wb[:, kc, :], rhs=hT[:, kc, ts], start=(kc == 0), stop=(kc == 7)),
                                reads=[rw, rhT[2 * j], rhT[2 * j + 1]], writes=[rpb[6]])
                        sc.op("act", lambda e: e.activation(gpT[:], pb[6][:], AF.Sigmoid),
                              reads=[rpb[6]], writes=[res("gpT")])
                        for cc in range(2):
                            sc.op("pe", lambda e, dc=dc, cc=cc, ts=ts: e.matmul(
                                pb[5][:], lhsT=pwS[:, dc, cc, :], rhs=mixT[:, cc, ts], start=(cc == 0), stop=(cc == 1)),
                                reads=[res("pw"), res("mixT")], writes=[rpb[5]])
                        sc.op("dve", lambda e, chunk=chunk: e.scalar_tensor_tensor(
                            ptmp[:], pb[5][:], psc[:, chunk:chunk + 1], gpT[:], ALU.mult, ALU.mult),
                            reads=[rpb[5], res("gpT"), res("psc")], writes=[res("ptmp")])
                        sc.op("dve", lambda e, chunk=chunk, ts=ts: e.tensor_tensor(
                            mT[:, chunk, ts], mT[:, chunk, ts], ptmp[:], ALU.add),
                            reads=[res("ptmp"), rmT[chunk][j]], writes=[rmT[chunk][j]])

            sc.fence()
            sc.op("dve", lambda e, s=s: e.scalar_tensor_tensor(A2[:], modT[:, 32:40, s], 1.0, n2g[:], ALU.add, ALU.mult),
                  reads=[res("modT"), res("n2g")], writes=[res("Acol")])

            def router(j, part):
                for sub in range(CW // 128):
                    gt = s * 16 + (CW // 128) * j + sub
                    Wt = Wtok[gt % 2]
                    rWt = res("Wtok%d" % (gt % 2))
                    pS = posS[gt % 2]
                    rpS = res("posS%d" % (gt % 2))
                    if part == "b":
                        sc.op("pe", lambda e, sub=sub: e.matmul(pb[5][:, 0:NE], lhsT=lsm[:], rhs=selT2[sub][:], start=True, stop=True),
                              reads=[res("lsm"), res("selT%d" % sub)], writes=[rpb[5]])
                        sc.op("dve", lambda e, pS=pS: e.tensor_tensor(pS[:], pb[5][:, 0:NE], base[:], ALU.add),
                              reads=[rpb[5], res("base")], writes=[rpS])
                        sc.op("pe", lambda e, sub=sub: e.matmul(pb[4][:, 0:NE], lhsT=ones[:], rhs=selT2[sub][:], start=True, stop=True),
                              reads=[res("ones"), res("selT%d" % sub)], writes=[rpb[4]])
                        sc.op("dve", lambda e: e.tensor_tensor(base[:], pb[4][:, 0:NE], base[:], ALU.add),
                              reads=[rpb[4], res("base")], writes=[res("base")])
                        sc.dma("sp", "st_pos", pos_d[gt], pS[:], reads=[rpS], writes=[res("pos_d")])
                        sc.dma("sp", "st_wt", wt_d[gt], Wt[:], reads=[rWt], writes=[res("wt_d")])
                        continue
                    if part == "h":
                        hb = h2tm[gt % 2]
                        rhb = res("h2tm%d" % (gt % 2))
                        tl = slice((CW * j) + sub * 128, (CW * j) + (sub + 1) * 128)
                        pbt = pb[7][:, :].bitcast(BF16)
                        for kc in range(8):
                            sc.op("pe", lambda e, kc=kc, tl=tl, pbt=pbt: e.transpose(pbt[:, kc * 128:(kc + 1) * 128], hT[:, kc, tl], idtb[:]),
                                  reads=[rhT[j], res("idtb")], writes=[rpb[7]])
                        sc.op("act", lambda e, hb=hb, pbt=pbt: e.activation(hb[:], pbt[:, :], AF.Copy),
                              reads=[rpb[7]], writes=[rhb])
                        sc.dma("sp", "st_h2", h2_d[gt * 128:(gt + 1) * 128, :], hb[:], reads=[rhb], writes=[res("h2_d")])
                        continue
                    for kc in range(8):
                        sc.op("pe", lambda e, kc=kc, sub=sub: e.matmul(
                            pb[6][:, 0:NE], lhsT=tmpF[:, kc, sub * 128:(sub + 1) * 128], rhs=wr[:, kc, :],
                            start=(kc == 0), stop=(kc == 7)),
                            reads=[rtmpF[kc], res("wr")], writes=[rpb[6]])
                    sc.op("act", lambda e: e.activation(scr[:], pb[6][:, 0:NE], AF.Sigmoid),
                          reads=[rpb[6]], writes=[res("scr")])
                    sc.op("dve", lambda e: e.tensor_tensor(bia[:], scr[:], brt[:], ALU.add),
                          reads=[res("scr"), res("brt")], writes=[res("bia")])
                    for gi in range(8):
                        sc.op("dve", lambda e, gi=gi: e.max(out=m8[:, gi, :], in_=bia[:, gi * 32:(gi + 1) * 32]),
                              reads=[res("bia")], writes=[res("m8")])
                    sc.op("dve", lambda e: e.tensor_tensor(gsc[:], m8[:, :, 0], m8[:, :, 1], ALU.add),
                          reads=[res("m8")], writes=[res("gsc")])
                    sc.op("dve", lambda e: e.max(out=gm8[:], in_=gsc[:]), reads=[res("gsc")], writes=[res("gm8")])
                    sc.op("dve", lambda e: e.tensor_scalar(gmk[:], gsc[:], gm8[:, 3:4], None, ALU.is_ge),
                          reads=[res("gsc"), res("gm8")], writes=[res("gmk")])
                    sc.op("dve", lambda e: e.tensor_scalar(msk[:], bia[:], 2.0, None, ALU.add),
                          reads=[res("bia")], writes=[res("msk")])
                    sc.op("dve", lambda e: e.tensor_tensor(
                        msk[:, :].rearrange("p (g k) -> p g k", g=8), msk[:, :].rearrange("p (g k) -> p g k", g=8),
                        gmk[:, :].unsqueeze(2).to_broadcast([128, 8, 32]), ALU.mult),
                        reads=[res("msk"), res("gmk")], writes=[res("msk")])
                    sc.op("dve", lambda e: e.max(out=t8[:], in_=msk[:]), reads=[res("msk")], writes=[res("t8")])
                    sc.op("dve", lambda e, Wt=Wt: e.scalar_tensor_tensor(
                        Wt[:], msk[:], t8[:, 7:8], scr[:], ALU.is_ge, ALU.mult),
                        reads=[res("msk"), res("t8"), res("scr")], writes=[rWt])
                    sc.op("dve", lambda e, Wt=Wt: e.tensor_reduce(den[:, 0:1], Wt[:], AX.X, ALU.add),
                          reads=[rWt], writes=[res("den")])
                    sc.op("dve", lambda e: e.reciprocal(den[:, 1:2], den[:, 0:1]),
                          reads=[res("den")], writes=[res("den")])
                    sc.op("dve", lambda e, Wt=Wt: e.tensor_scalar(Wt[:], Wt[:], den[:, 1:2], 2.5, ALU.mult, ALU.mult),
                          reads=[rWt, res("den")], writes=[rWt])
                    sc.op("dve", lambda e, Wt=Wt, sub=sub: e.tensor_scalar(selT2[sub][:], Wt[:], 0.0, None, ALU.is_gt),
                          reads=[rWt], writes=[res("selT%d" % sub)])

            for j in range(8):
                ts = slice(j * CW, (j + 1) * CW)
                sc.dma("sp", "ld_x", xch[:], xT_d[s][:, :, ts], writes=[res("xch")])
                for oc in range(8):
                    wb = woS[oc % 2]
                    rw = res("wo%d" % (oc % 2))
                    sc.dma("pool", "ld_wo%d" % (oc % 2), wb[:], wo_d[oc], writes=[rw])
                    for kc in range(8):
                        sc.op("pe", lambda e, wb=wb, kc=kc, ts=ts: e.matmul(
                            pb[4][:, 0:CW], lhsT=wb[:, kc, :], rhs=mT[:, kc, ts], start=(kc == 0), stop=(kc == 7)),
                            reads=[rw], writes=[rpb[4]])
                    sc.op("dve", lambda e, oc=oc, s=s: e.scalar_tensor_tensor(
                        xch[:, oc, :], pb[4][:, 0:CW], modT[:, 16 + oc, s:s + 1], xch[:, oc, :], ALU.mult, ALU.add),
                        reads=[rpb[4], res("modT"), res("xch")], writes=[res("xch")])
                rmsnorm_chunk(A2, 24, s, ts, rhT[j], True)
                router(j, "a")
                router(j, "h")
                for half in range(2):
                    hs = slice(half * 128, (half + 1) * 128)
                    for kc in range(8):
                        sc.op("pe", lambda e, kc=kc, hs=hs, ts=ts, half=half: e.matmul(
                            pb[half][:, 0:CW], lhsT=wsg[:, kc, hs], rhs=hT[:, kc, ts], start=(kc == 0), stop=(kc == 7)),
                            reads=[res("wsg"), rhT[j]], writes=[rpb[half]])
                    for kc in range(8):
                        sc.op("pe", lambda e, kc=kc, hs=hs, ts=ts, half=half: e.matmul(
                            pb[2 + half][:, 0:CW], lhsT=wsu[:, kc, hs], rhs=hT[:, kc, ts], start=(kc == 0), stop=(kc == 7)),
                            reads=[res("wsu"), rhT[j]], writes=[rpb[2 + half]])
                router(j, "b")
                for half in range(2):
                    sc.op("act", lambda e, half=half: e.activation(sil[:, half, 0:CW], pb[half][:, 0:CW], AF.Silu),
                          reads=[rpb[half]], writes=[res("sil%d" % half)])
                    sc.op("dve", lambda e, half=half: e.tensor_tensor(
                        aT[:, half, 0:CW], sil[:, half, 0:CW], pb[2 + half][:, 0:CW], ALU.mult),
                        reads=[res("sil%d" % half), rpb[2 + half]], writes=[res("aT")])
                for dcn in range(8):
                    bi = 5 + (dcn % 2)
                    for cc in range(2):
                        sc.op("pe", lambda e, cc=cc, dcn=dcn, bi=bi: e.matmul(
                            pb[bi][:, 0:CW], lhsT=wsd[:, cc, dcn * 128:(dcn + 1) * 128], rhs=aT[:, cc, 0:CW],
                            start=(cc == 0), stop=(cc == 1)),
                            reads=[res("wsd"), res("aT")], writes=[rpb[bi]])
                    sc.op("dve", lambda e, dcn=dcn, bi=bi, s=s: e.scalar_tensor_tensor(
                        xch[:, dcn, :], pb[bi][:, 0:CW], modT[:, 40 + dcn, s:s + 1], xch[:, dcn, :], ALU.mult, ALU.add),
                        reads=[rpb[bi], res("modT"), res("xch")], writes=[res("xch")])
                sc.dma("sp", "st_xs", xs_d[s][:, :, ts], xch[:], reads=[res("xch")], writes=[res("xs_d")])

        sc.fence()
        us = [cvA(0, 256), cvA(256, 256)]
        ui = [cvA(512, 256), cvA(768, 256)]
        iot = cvA(1024, 1024)
        for c in range(2):
            sc.dma("sp", "ld_us", us[c][:], us_d[c], writes=[res("us%d" % c)])
            sc.dma("sp", "ld_ui", ui[c][:], ui_d[c], writes=[res("ui%d" % c)])
        sc.dma("sp", "ld_iot", iot[:], iot_d, writes=[res("iot")])
        sc.op("dve", lambda e: e.tensor_scalar(nbf[:], base[:], 127.0, None, ALU.add), reads=[res("base")], writes=[res("nbf")])
        sc.op("dve", lambda e: e.tensor_copy(out=nbi[:], in_=nbf[:]), reads=[res("nbf")], writes=[res("nbi")])
        sc.op("dve", lambda e: e.tensor_single_scalar(nbi[:], nbi[:], 7, ALU.arith_shift_right),
              reads=[res("nbi")], writes=[res("nbi")])
        sc.op("dve", lambda e: e.tensor_copy(out=nbf[:], in_=nbi[:]), reads=[res("nbi")], writes=[res("nbf")])
        for c in range(2):
            sc.op("pe", lambda e, c=c: e.transpose(pb[c][:, 0:128], nbf[:, c * 128:(c + 1) * 128], idt[:]),
                  reads=[res("nbf"), res("idt")], writes=[rpb[c]])
            sc.op("dve", lambda e, c=c: e.tensor_copy(out=nbT[c][:], in_=pb[c][:, 0:128]), reads=[rpb[c]], writes=[res("nbT%d" % c)])
        for c in range(2):
            sc.op("pe", lambda e, c=c: e.matmul(pb[2][:, 0:NE], lhsT=nbT[c][:], rhs=us[c][:], start=(c == 0), stop=(c == 1)),
                  reads=[res("nbT%d" % c), res("us%d" % c)], writes=[rpb[2]])
        sc.op("dve", lambda e: e.tensor_scalar(sbase[:], pb[2][:, 0:NE], 128.0, None, ALU.mult), reads=[rpb[2]], writes=[res("sbase")])
        for c2 in range(2):
            for c in range(2):
                sc.op("pe", lambda e, c=c, c2=c2: e.matmul(pb[3][:, 0:2], lhsT=ui[c][:, c2 * 128:(c2 + 1) * 128], rhs=nbT[c][:, 0:2],
                                                           start=(c == 0), stop=(c == 1)),
                      reads=[res("nbT%d" % c), res("ui%d" % c)], writes=[rpb[3]])
            sc.op("dve", lambda e, c2=c2: e.tensor_copy(out=bend[:, c2:c2 + 1], in_=pb[3][:, 0:1]), reads=[rpb[3]], writes=[res("bend")])
        for c2 in range(2):
            sc.op("dve", lambda e, c2=c2: e.tensor_scalar(Cm[c2][:], iot[:, 0:NBLK], bend[:, c2:c2 + 1], None, ALU.is_ge),
                  reads=[res("iot"), res("bend")], writes=[res("Cm%d" % c2)])
        for c0 in range(0, NBLK, 512):
            cw = min(512, NBLK - c0)
            for c2 in range(2):
                sc.op("pe", lambda e, c0=c0, cw=cw, c2=c2: e.matmul(pb[4][:, 0:cw], lhsT=ones[:], rhs=Cm[c2][:, c0:c0 + cw],
                                                                    start=(c2 == 0), stop=(c2 == 1)),
                      reads=[res("ones"), res("Cm%d" % c2)], writes=[rpb[4]])
            sc.op("dve", lambda e, c0=c0, cw=cw: e.tensor_scalar(bef[:, c0:c0 + cw], pb[4][:, 0:cw], 128.0, None, ALU.mult),
                  reads=[rpb[4]], writes=[res("bef")])
        sc.op("dve", lambda e: e.tensor_scalar(idxW[:], bef[:], pio8[:, 0:1], None, ALU.add),
              reads=[res("bef"), res("pio8")], writes=[res("idxW")])
        sc.op("dve", lambda e: e.memset(zslot[:], 0.0), writes=[res("zslot")])
        sc.dma("sp", "st_z", slot_d.rearrange("(p a) b -> p (a b)", p=128), zslot[:], reads=[res("zslot")], writes=[res("slot_d")])
        for gt in range(NT):
            b2 = gt % 2
            sc.dma("sp", "ld_pos%d" % b2, posL[b2][:], pos_d[gt], reads=[res("pos_d")], writes=[res("posL%d" % b2)])
            sc.dma("sp", "ld_wt%d" % b2, wtL[b2][:], wt_d[gt], reads=[res("wt_d")], writes=[res("wtL%d" % b2)])
            sc.op("dve", lambda e, b2=b2: e.scalar_tensor_tensor(dp1[:], posL[b2][:], 1.0, sbase[:], ALU.add, ALU.add),
                  reads=[res("posL%d" % b2), res("sbase")], writes=[res("dp1")])
            sc.op("dve", lambda e, b2=b2: e.scalar_tensor_tensor(keyb[:], wtL[b2][:], 0.0, dp1[:], ALU.is_gt, ALU.mult),
                  reads=[res("wtL%d" % b2), res("dp1")], writes=[res("keyb")])
            sc.op("dve", lambda e: e.max(out=d8[:], in_=keyb[:]), reads=[res("keyb")], writes=[res("d8")])
            rrw = res("rows%d" % b2)
            sc.op("dve", lambda e, b2=b2, gt=gt: e.tensor_scalar(rows[b2][:, :, 0], pio8[:], float(gt * 128), None, ALU.add),
                  reads=[res("pio8")], writes=[rrw])
            for k in range(8):
                sc.op("dve", lambda e, b2=b2, k=k: e.scalar_tensor_tensor(
                    junkb[:], keyb[:], d8[:, k:k + 1], wtL[b2][:], ALU.is_equal, ALU.mult, accum_out=rows[b2][:, k, 1:2]),
                    reads=[res("keyb"), res("d8"), res("wtL%d" % b2)], writes=[res("junkb"), rrw])
            sc.op("dve", lambda e, gt=gt: e.tensor_scalar(dstu[:, gt, :], d8[:], -1.0, None, ALU.add),
                  reads=[res("d8")], writes=[res("dstu")])
            for k in range(8):
                sc.idma("sc_slot", slot_d, dstu[:, gt, k:k + 1], rows[b2][:, k, :], None,
                        reads=[rrw, res("dstu")], writes=[res("slot_d")])

        sc.fence()
        slotv = slot_d.rearrange("(i p) c -> i (p c)", p=128)
        for t6 in range(NBLK // 128):
            b2 = t6 % 2
            sc.dma("sp", "ld_srow%d" % b2, sl6[b2][:], slotv[t6 * 128:(t6 + 1) * 128, :], reads=[res("slot_d")],
                   writes=[res("sl6_%d" % b2)])
            v3 = sl6[b2][:, :].rearrange("i (p c) -> i p c", c=2)
            for c in range(2):
                sc.op("dve", lambda e, c=c, v3=v3: e.tensor_copy(out=sl6c[c][:], in_=v3[:, :, c]),
                      reads=[res("sl6_%d" % b2)], writes=[res("sl6c%d" % c)])
                sc.op("pe", lambda e, c=c: e.transpose(pb[c][:, 0:128], sl6c[c][:], idt[:]),
                      reads=[res("sl6c%d" % c), res("idt")], writes=[rpb[c]])
            sc.op("dve", lambda e, t6=t6: e.tensor_copy(out=tokuA[:, t6 * 128:(t6 + 1) * 128], in_=pb[0][:, 0:128]),
                  reads=[rpb[0]], writes=[res("tokuA")])
            sc.op("act", lambda e, t6=t6: e.activation(wA[:, t6 * 128:(t6 + 1) * 128], pb[1][:, 0:128], AF.Copy),
                  reads=[rpb[1]], writes=[res("srowA")])
        WB = NE * 128 - 1

        def gathers(i):
            b3 = i % 3
            sc.idma("g_x%d" % b3, xgC[b3][:], None, h2_d, tokuA[:, i:i + 1], reads=[res("tokuA"), res("h2_d")],
                    writes=[res("xg%d" % b3)])
            sc.idma("g_wg%d" % b3, wgC[b3][:], None, wg2, idxW[:, i:i + 1], reads=[res("idxW")], writes=[res("wgC%d" % b3)], bound=WB)
            sc.idma("g_wu%d" % b3, wuC[b3][:], None, wu2, idxW[:, i:i + 1], reads=[res("idxW")], writes=[res("wuC%d" % b3)], bound=WB)
            sc.idma("g_wd%d" % b3, wdC[b3][:, :, :].rearrange("p a b -> p (a b)"), None, wd2, idxW[:, i:i + 1],
                    reads=[res("idxW")], writes=[res("wdC%d" % b3)], bound=WB)

        def stage1(i):
            b2, b3 = i % 2, i % 3
            pT_, rT_ = pb[4 * b2], rpb[4 * b2]
            pbt = pT_[:, :].bitcast(BF16)
            for kc in range(8):
                sc.op("pe", lambda e, kc=kc, b3=b3, pbt=pbt: e.transpose(
                    pbt[:, kc * 128:(kc + 1) * 128], xgC[b3][:, kc * 128:(kc + 1) * 128], idtb[:]),
                    reads=[res("xg%d" % b3), res("idtb")], writes=[rT_])
            sc.op("act", lambda e, b2=b2, pbt=pbt: e.activation(xgT[b2][:, :, :].rearrange("p a b -> p (a b)"), pbt[:, :], AF.Copy),
                  reads=[rT_], writes=[res("xgT%d" % b2)])

        def stage2(i):
            b2, b3 = i % 2, i % 3
            pG_, rG_ = pb[4 * b2 + 1], rpb[4 * b2 + 1]
            for gu, wC, rn in ((0, wgC, "wgC%d"), (1, wuC, "wuC%d")):
                for half in range(2):
                    col = (gu * 2 + half) * 128
                    for kc in range(8):
                        sc.op("pe", lambda e, kc=kc, b2=b2, b3=b3, wC=wC, half=half, col=col, pG_=pG_: e.matmul(
                            pG_[:, col:col + 128], lhsT=wC[b3][:, kc * 256 + half * 128:kc * 256 + (half + 1) * 128],
                            rhs=xgT[b2][:, kc, :], start=(kc == 0), stop=(kc == 7)),
                            reads=[res(rn % b3), res("xgT%d" % b2)], writes=[rG_])
            sc.op("act", lambda e, b2=b2, pG_=pG_: e.activation(silC[b2][:], pG_[:, 0:256], AF.Silu),
                  reads=[rG_], writes=[res("silC%d" % b2)])
            sc.op("dve", lambda e, b2=b2, pG_=pG_: e.tensor_tensor(
                aTC[b2][:, :, :].rearrange("p a b -> p (a b)"), silC[b2][:], pG_[:, 256:512], ALU.mult),
                reads=[res("silC%d" % b2), rG_], writes=[res("aTC%d" % b2)])

        def stage3(i):
            b2, b3 = i % 2, i % 3
            pY0, pY1, rY0, rY1 = pb[4 * b2 + 2], pb[4 * b2 + 3], rpb[4 * b2 + 2], rpb[4 * b2 + 3]
            for dh, pY_, rY_ in ((0, pY0, rY0), (1, pY1, rY1)):
                for cc in range(2):
                    sc.op("pe", lambda e, cc=cc, b2=b2, b3=b3, dh=dh, pY_=pY_: e.matmul(
                        pY_[:], lhsT=aTC[b2][:, cc, :], rhs=wdC[b3][:, cc, dh * 512:(dh + 1) * 512],
                        start=(cc == 0), stop=(cc == 1)),
                        reads=[res("aTC%d" % b2), res("wdC%d" % b3)], writes=[rY_])
            sc.op("act", lambda e, b2=b2, pY0=pY0, i=i: e.activation(ysb[b2][:, 0:512], pY0[:], AF.Copy, scale=wA[:, i:i + 1]),
                  reads=[rY0, res("srowA")], writes=[res("ysb%d" % b2)])
            sc.op("dve", lambda e, b2=b2, pY1=pY1, i=i: e.tensor_scalar(ysb[b2][:, 512:1024], pY1[:], wA[:, i:i + 1], None, ALU.mult),
                  reads=[rY1, res("srowA")], writes=[res("ysb%d" % b2)])
            sc.dma("sp", "st_y", y_d[i * 128:(i + 1) * 128, :], ysb[b2][:], reads=[res("ysb%d" % b2)], writes=[res("y_d")])

        gathers(0)
        gathers(1)
        stage1(0)
        for i in range(NBLK):
            if i + 2 < NBLK:
                gathers(i + 2)
            stage2(i)
            if i + 1 < NBLK:
                stage1(i + 1)
            stage3(i)

        sc.fence()
        xstL = [cvB(0, 1024).rearrange("p (a b) -> p a b", a=8), cvB(2048, 1024).rearrange("p (a b) -> p a b", a=8)]
        otlL = [cvB(1024, 1024).rearrange("p (a b) -> p a b", a=8), cvB(3072, 1024).rearrange("p (a b) -> p a b", a=8)]

        def gathD(gt):
            b3 = gt % 3
            for k in range(8):
                sc.idma("g_y%d" % b3, yg[b3][:, k, :], None, y_d, dstu[:, gt, k:k + 1], reads=[res("dstu"), res("y_d")],
                        writes=[res("yg%d" % b3)])

        gathD(0)
        if NT > 1:
            gathD(1)
        for gt in range(NT):
            b2, b3 = gt % 2, gt % 3
            if gt + 2 < NT:
                gathD(gt + 2)
            s_, t_ = gt // 16, gt % 16
            tt = slice(t_ * 128, (t_ + 1) * 128)
            ryg = res("yg%d" % b3)
            acc_, racc = accD[b2], res("accD%d" % b2)
            xst_, rxst = xstL[b2], res("xst%d" % b2)
            otl_, rotl = otlL[b2], res("otl%d" % b2)
            sc.dma("sp", "ld_xs%d" % b2, xst_[:, :, :], xs_d[s_][:, :, tt], reads=[res("xs_d")], writes=[rxst])
            sc.op("dve", lambda e, b3=b3, acc_=acc_: e.tensor_tensor(acc_[:], yg[b3][:, 0, :], yg[b3][:, 1, :], ALU.add),
                  reads=[ryg], writes=[racc])
            for k in range(2, 8):
                sc.op("dve", lambda e, k=k, b3=b3, acc_=acc_: e.tensor_tensor(acc_[:], acc_[:], yg[b3][:, k, :], ALU.add),
                      reads=[ryg, racc], writes=[racc])
            for c in range(8):
                bi = 4 * b2 + c // 4
                sc.op("pe", lambda e, c=c, bi=bi, acc_=acc_: e.transpose(pb[bi][:, (c % 4) * 128:(c % 4 + 1) * 128],
                                                                          acc_[:, c * 128:(c + 1) * 128], idt[:]),
                      reads=[racc, res("idt")], writes=[rpb[bi]])
            for c in range(8):
                bi = 4 * b2 + c // 4
                sc.op("dve", lambda e, c=c, bi=bi, s_=s_, otl_=otl_, xst_=xst_: e.scalar_tensor_tensor(
                    otl_[:, c, :], pb[bi][:, (c % 4) * 128:(c % 4 + 1) * 128], modT[:, 40 + c, s_:s_ + 1], xst_[:, c, :],
                    ALU.mult, ALU.add),
                    reads=[rpb[bi], res("modT"), rxst], writes=[rotl])
            tok = sc.dma("sp", "st_out", out_d[s_][:, :, tt], otl_[:, :, :], reads=[rotl], writes=[res("out_d")])
        sc.wait("sp", tok)
        sc.emit()
    return nc


def _t5_bucket_np(rel):
    import jax
    import jax.numpy as jnp
    with jax.default_device(jax.devices("cpu")[0]):
        nb = NB // 2
        max_exact = nb // 2
        rel = jnp.asarray(np.asarray(rel, dtype=np.int32))
        n = jnp.abs(rel)
        large = max_exact + (jnp.log(jnp.maximum(n, 1).astype(jnp.float32) / max_exact)
                             / math.log(128 / max_exact) * (nb - max_exact)).astype(jnp.int32)
        large = jnp.minimum(large, nb - 1)
        return np.asarray(jnp.where(rel > 0, nb, 0) + jnp.where(n < max_exact, n, large))


def _chunk_w(w, ncol_chunk=128):
    K, N = w.shape
    return np.ascontiguousarray(w.reshape(K // 128, 128, N // ncol_chunk, ncol_chunk).transpose(2, 1, 0, 3))


def _prep_shared(inp):
    f = np.float32
    g = {}
    g["w_ada"] = _chunk_w(inp["w_ada"][0])
    g["b_adaT"] = np.ascontiguousarray(inp["b_ada"][0].reshape(48, 128).T)
    g["n1g"] = np.ascontiguousarray(inp["norm1_g"][0].reshape(8, 128).T)
    g["n2g"] = np.ascontiguousarray(inp["norm2_g"][0].reshape(8, 128).T)
    g["w_in"] = _chunk_w(inp["w_in"][0])
    g["qkg"] = np.ascontiguousarray(np.stack([np.tile(inp["q_norm_g"][0], 2), np.tile(inp["k_norm_g"][0], 2)], axis=1))
    lam = np.stack([inp["lambda_q1"][0], inp["lambda_k1"][0], inp["lambda_q2"][0], inp["lambda_k2"][0]], axis=0)
    g["lam_in"] = np.ascontiguousarray(np.broadcast_to(lam[None], (128, 4, 64)))
    g["subln_g"] = np.ascontiguousarray(np.broadcast_to(inp["subln_g"][0][None], (128, 128)))
    pw = inp["pool_w"][0]
    g["pool_w"] = np.ascontiguousarray(pw.reshape(4, 2, 128, 2, 128).transpose(0, 3, 2, 1, 4))
    g["pool_scaleT"] = np.ascontiguousarray(inp["pool_scale"][0].reshape(8, 128).T)
    rc = np.zeros((4, 16), f)
    for gi, w in enumerate((2, 4, 8, 16)):
        for k, pos in enumerate(list(range(8)) + list(range(S - 8, S))):
            lo = min(max(pos - w // 2, 0), S - 1)
            hi = min(max(pos + w // 2 - 1, 0), S - 1)
            rc[gi, k] = 1.0 / float(hi - lo + 1)
    g["pool_rc"] = np.ascontiguousarray(np.broadcast_to(rc[None], (128, 4, 16)))
    g["w_out"] = _chunk_w(inp["w_out"][0])
    g["w_router"] = np.ascontiguousarray(inp["w_router"][0].reshape(8, 128, NE).transpose(1, 0, 2))
    g["b_router"] = np.ascontiguousarray(np.broadcast_to(inp["b_router"][0][None], (128, NE)))
    wg = np.concatenate([inp["w_exp_gate"][0], inp["w_sh_gate"]], axis=0)
    wu = np.concatenate([inp["w_exp_up"][0], inp["w_sh_up"]], axis=0)
    wd = np.concatenate([inp["w_exp_down"][0], inp["w_sh_down"]], axis=0)
    g["w_eg"] = np.ascontiguousarray(wg.reshape(NE + 1, 8, 128, 256).transpose(0, 2, 1, 3))
    g["w_eu"] = np.ascontiguousarray(wu.reshape(NE + 1, 8, 128, 256).transpose(0, 2, 1, 3))
    g["w_ed"] = np.ascontiguousarray(wd.reshape(NE + 1, 2, 128, 1024).transpose(0, 2, 1, 3))
    g["rel_tab"] = np.ascontiguousarray(inp["rel_bias_table"])
    jj = np.arange(FVW)
    bk = _t5_bucket_np(767 - jj)
    oh = np.zeros((NB, FVW), f)
    oh[bk, jj] = 1.0
    g["bias_oh"] = oh
    g["ident"] = np.eye(128, dtype=f)
    g["antiid"] = np.ascontiguousarray(np.eye(128, dtype=f)[::-1])
    bo = np.zeros((128, 128), f)
    bo[:64, :64] = 1.0
    bo[64:, 64:] = 1.0
    g["blockones"] = bo
    ar = np.arange(128)
    g["lstrict"] = (ar[:, None] < ar[None, :]).astype(f)
    ee = np.arange(NE)
    g["ustrict"] = np.stack([((c * 128 + ar)[:, None] < ee[None, :]).astype(f) for c in range(2)])
    g["uincl"] = np.stack([((c * 128 + ar)[:, None] <= ee[None, :]).astype(f) for c in range(2)])
    g["iota_row"] = np.ascontiguousarray(np.broadcast_to(np.arange(1024, dtype=f)[None], (128, 1024)))
    g["piota8"] = np.ascontiguousarray(np.broadcast_to(ar.astype(f)[:, None], (128, 8)))
    return {k: np.asarray(v, dtype=f) for k, v in g.items()}


def _core_inputs(shared, x, c, b0, nseq):
    m = dict(shared)
    xs = x[b0:b0 + nseq]
    m["xT"] = np.ascontiguousarray(xs.reshape(nseq, S, 8, 128).transpose(0, 3, 2, 1))
    m["cT"] = np.ascontiguousarray(c[b0:b0 + nseq].reshape(nseq, 8, 128).transpose(2, 1, 0))
    return m


def _unpack(outT):
    return np.ascontiguousarray(outT.transpose(0, 3, 2, 1).reshape(outT.shape[0], S, D))


def kernel(**inputs):
    inp = {k: np.asarray(v, dtype=np.float32) for k, v in inputs.items()}
    ncores = 8
    nseq = inp["x"].shape[0] // ncores
    shared = _prep_shared(inp)
    nc = build(nseq)
    in_maps = [_core_inputs(shared, inp["x"], inp["c"], i * nseq, nseq) for i in range(ncores)]
    res = run_bass_kernel_spmd(nc, in_maps, core_ids=list(range(ncores)))
    return np.concatenate([_unpack(r["outT"]) for r in res.results], axis=0)
```

```python
import math
import numpy as np
from contextlib import ExitStack
import concourse.bass as bass
import concourse.mybir as mybir
from concourse.bass_utils import run_bass_kernel_spmd

F32 = mybir.dt.float32
BF16 = mybir.dt.bfloat16
AF = mybir.ActivationFunctionType
ALU = mybir.AluOpType
AX = mybir.AxisListType

D = 1024
S = 2048
NB = 32
NH = 8
NE = 256
EPS = 1e-6
LAM_INIT = 0.8 - 0.6 * math.exp(-0.3 * 0)
PADL = 16
LP = S + 2 * PADL
MW = 1280
FVW = MW + 127


class Res:
    __slots__ = ("w", "r")

    def __init__(self):
        self.w = None
        self.r = {}


class Sched:
    ENG = ("pe", "dve", "act", "pool", "sp")

    def __init__(self, nc, es):
        self.nc = nc
        self.es = es
        self.sem = {k: es.enter_context(nc.semaphore("s_" + k)) for k in self.ENG}
        self.cnt = {k: 0 for k in self.ENG}
        self.seen = {k: {} for k in self.ENG}
        self.prog = {k: [] for k in self.ENG}
        self.dsem = {}
        self.dcnt = {}

    def _wait(self, e, tok):
        if tok is None:
            return
        key, val = tok
        if key == e and e == "pe":
            return
        if self.seen[e].get(key, 0) >= val:
            return
        sem = self.sem[key] if key in self.sem else self.dsem[key]
        self.prog[e].append(("w", sem, val))
        self.seen[e][key] = val

    def _deps(self, e, reads, writes):
        for r in reads:
            self._wait(e, r.w)
        for w in writes:
            self._wait(e, w.w)
            for k, v in w.r.items():
                self._wait(e, (k, v))

    def _commit(self, tok, reads, writes):
        for r in reads:
            if r.r.get(tok[0], 0) < tok[1]:
                r.r[tok[0]] = tok[1]
        for w in writes:
            w.w = tok
            w.r = {}

    def op(self, e, fn, reads=(), writes=()):
        self._deps(e, reads, writes)
        self.cnt[e] += 1
        self.prog[e].append(("i", fn, self.sem[e], 1))
        tok = (e, self.cnt[e])
        self._commit(tok, reads, writes)
        return tok

    def dma(self, e, chan, out, in_, reads=(), writes=()):
        if chan not in self.dsem:
            self.dsem[chan] = self.es.enter_context(self.nc.semaphore("dm_" + chan))
            self.dcnt[chan] = 0
        self._deps(e, reads, writes)
        self.dcnt[chan] += 16
        self.prog[e].append(("i", lambda eng: eng.dma_start(out=out, in_=in_), self.dsem[chan], 16))
        tok = (chan, self.dcnt[chan])
        self._commit(tok, reads, writes)
        return tok

    def idma(self, chan, out, out_off, in_, in_off, reads=(), writes=(), bound=None):
        e = "pool"
        if chan not in self.dsem:
            self.dsem[chan] = self.es.enter_context(self.nc.semaphore("dm_" + chan))
            self.dcnt[chan] = 0
        self._deps(e, reads, writes)
        self.dcnt[chan] += 16
        oo = None if out_off is None else bass.IndirectOffsetOnAxis(ap=out_off, axis=0)
        io = None if in_off is None else bass.IndirectOffsetOnAxis(ap=in_off, axis=0)
        if bound is None:
            self.prog[e].append(("i", lambda eng: eng.indirect_dma_start(out=out, out_offset=oo, in_=in_, in_offset=io),
                                 self.dsem[chan], 16))
        else:
            self.bound_val = bound
            self.prog[e].append(("i", lambda eng: eng.indirect_dma_start(out=out, out_offset=oo, in_=in_, in_offset=io,
                                                                         bounds_check=self.bound_reg, oob_is_err=False),
                                 self.dsem[chan], 16))
        tok = (chan, self.dcnt[chan])
        self._commit(tok, reads, writes)
        return tok

    def wait(self, e, tok):
        self._wait(e, tok)

    def fence(self):
        toks = [(k, self.cnt[k]) for k in self.ENG if self.cnt[k] > 0]
        toks += [(k, v) for k, v in self.dcnt.items() if v > 0]
        for e in self.ENG:
            for t in toks:
                if t[0] == e:
                    continue
                self._wait(e, t)

    def emit(self):
        nc = self.nc
        with nc.Block() as block:
            def mk(e):
                def f(eng):
                    if e == "pool" and getattr(self, "bound_val", None) is not None:
                        self.bound_reg = eng.alloc_register("oob_bound")
                        eng.reg_mov(self.bound_reg, int(self.bound_val))
                    for it in self.prog[e]:
                        if it[0] == "w":
                            eng.wait_ge(it[1], it[2])
                        else:
                            it[1](eng).then_inc(it[2], it[3])
                return f
            block.tensor(mk("pe"))
            block.vector(mk("dve"))
            block.scalar(mk("act"))
            block.gpsimd(mk("pool"))
            block.sync(mk("sp"))


def build(NSEQ, n_exp=NE, dbg=False):
    nc = bass.Bass("TRN2", target_bir_lowering=False)

    def din(name, shape):
        return nc.dram_tensor(name, list(shape), F32, kind="ExternalInput").ap()

    xT_d = din("xT", [NSEQ, 128, 8, S])
    cT_d = din("cT", [128, 8, NSEQ])
    wada_d = din("w_ada", [48, 128, 8, 128])
    bada_d = din("b_adaT", [128, 48])
    n1g_d = din("n1g", [128, 8])
    n2g_d = din("n2g", [128, 8])
    win_d = din("w_in", [48, 128, 8, 128])
    qkg_d = din("qkg", [128, 2])
    lam_d = din("lam_in", [128, 4, 64])
    sg_d = din("subln_g", [128, 128])
    pw_d = din("pool_w", [4, 2, 128, 2, 128])
    psc_d = din("pool_scaleT", [128, 8])
    rc_d = din("pool_rc", [128, 4, 16])
    wo_d = din("w_out", [8, 128, 8, 128])
    wr_d = din("w_router", [128, 8, NE])
    br_d = din("b_router", [128, NE])
    wg_d = din("w_eg", [NE + 1, 128, 8, 256])
    wu_d = din("w_eu", [NE + 1, 128, 8, 256])
    wd_d = din("w_ed", [NE + 1, 128, 2, 1024])
    tab_d = din("rel_tab", [NB, NH])
    oh_d = din("bias_oh", [NB, FVW])
    idt_d = din("ident", [128, 128])
    aid_d = din("antiid", [128, 128])
    bones_d = din("blockones", [128, 128])
    out_d = nc.dram_tensor("outT", [NSEQ, 128, 8, S], F32, kind="ExternalOutput").ap()
    fv_d = nc.dram_tensor("fv_scr", [NH, FVW], F32, kind="Internal").ap()
    mst_d = nc.dram_tensor("mst_scr", [NH, 128, MW], F32, kind="Internal").ap()
    T = NSEQ * S
    NT = T // 128
    NBLK = T * 8 // 128 + NE
    NSLOT = NBLK * 128
    ls_d = din("lstrict", [128, 128])
    us_d = din("ustrict", [2, 128, NE])
    ui_d = din("uincl", [2, 128, NE])
    iot_d = din("iota_row", [128, 1024])
    pio_d = din("piota8", [128, 8])
    U32 = mybir.dt.uint32
    I32 = mybir.dt.int32
    h2_d = nc.dram_tensor("h2_scr", [T, D], BF16, kind="Internal").ap()
    pos_d = nc.dram_tensor("pos_scr", [NT, 128, NE], F32, kind="Internal").ap()
    wt_d = nc.dram_tensor("wt_scr", [NT, 128, NE], F32, kind="Internal").ap()
    xs_d = nc.dram_tensor("xs_scr", [NSEQ, 128, 8, S], F32, kind="ExternalOutput").ap() if dbg else out_d
    slot_d = nc.dram_tensor("slot_scr", [NSLOT, 2], F32, kind="Internal").ap()
    y_d = nc.dram_tensor("y_scr", [NSLOT, D], BF16, kind="Internal").ap()
    wg2 = wg_d.rearrange("e p a b -> (e p) (a b)")
    wu2 = wu_d.rearrange("e p a b -> (e p) (a b)")
    wd2 = wd_d.rearrange("e p a b -> (e p) (a b)")

    with ExitStack() as es:
        es.enter_context(nc.allow_low_precision("bf16 matmul operands, fp32 accumulation"))
        es.enter_context(nc.allow_non_contiguous_dma("overlapping-window bias load"))
        sc = Sched(nc, es)

        def sb(name, shape, dt=F32):
            return es.enter_context(nc.sbuf_tensor(name, list(shape), dt))

        CW = 256
        arA = sb("arA", [128, 16384])
        arB = sb("arB", [128, 8320])

        def cvA(off, n, dt=F32):
            return arA[:, off:off + n] if dt == F32 else arA[:, off:off + n].bitcast(dt)

        def cvB(off, n, dt=F32):
            return arB[:, off:off + n] if dt == F32 else arB[:, off:off + n].bitcast(dt)

        bigA = arA[:, :].rearrange("p (a b) -> p a b", a=8)
        mT = cvA(0, 8192, BF16).rearrange("p (a b) -> p a b", a=8)
        qz = [cvA(8192, 1024, BF16), cvB(0, 1024, BF16)]
        kT = cvA(9216, 1024, BF16)
        vA = cvA(10240, 1040, BF16).rearrange("p (a b) -> p a b", a=16)
        gaT = cvA(11280, 1024, BF16)
        mst = cvA(12304, 1280)
        O = [cvA(13584, 520).rearrange("p (a b) -> p a b", a=4), cvA(14104, 520).rearrange("p (a b) -> p a b", a=4)]
        sadd = [cvA(14624, 512), cvA(15136, 512)]
        hank = cvA(0, 1280)
        oh = arA[0:NB, 1280:1280 + FVW]
        fvs = arA[0:NH, 2688:2688 + FVW]
        wadaS = [cvA(4096, 1024).rearrange("p (a b) -> p a b", a=8), cvA(5120, 1024).rearrange("p (a b) -> p a b", a=8)]
        lam_in = cvA(6144, 256).rearrange("p (a b) -> p a b", a=4)
        lamt = cvA(6400, 256).rearrange("p (a b) -> p a b", a=4)
        sil = cvB(0, 1024).rearrange("p (a b) -> p a b", a=2)
        aT = cvB(1024, 512, BF16).rearrange("p (a b) -> p a b", a=2)
        scr = cvB(1536, 256)
        bia = cvB(1792, 256)
        msk = cvB(2048, 256)
        Wtok = [cvB(2304, 256), cvB(2560, 256)]
        selT2 = [cvB(2816, 256), cvB(4736, 256)]
        posS = [cvB(3072, 256), cvB(3328, 256)]
        m8 = cvB(3584, 64).rearrange("p (a b) -> p a b", a=8)
        gsc = cvB(3648, 8)
        gm8 = cvB(3656, 8)
        gmk = cvB(3664, 8)
        t8 = cvB(3672, 8)
        den = cvB(3680, 2)
        h2tm = [cvB(3712, 512, BF16), cvB(4224, 512, BF16)]
        pP = cvB(0, LP)
        pW = [cvB(2080, LP), cvB(4160, LP)]
        mixT = cvB(6240, 2048, BF16).rearrange("p (a b) -> p a b", a=2)
        nbf = cvB(0, 256)
        nbi = cvB(256, 256, I32)
        sbase = cvB(512, 256)
        nbT = [cvB(768, 128), cvB(896, 128)]
        bend = cvB(1024, 2)
        Cm = [cvB(1032, NBLK), cvB(1032 + NBLK, NBLK)]
        bef = cvB(1032 + 2 * NBLK, NBLK)
        o_b = 1032 + 3 * NBLK
        posL = [cvB(o_b, 256), cvB(o_b + 256, 256)]
        wtL = [cvB(o_b + 512, 256), cvB(o_b + 768, 256)]
        dp1 = cvB(o_b + 1024, 256)
        keyb = cvB(o_b + 1280, 256)
        junkb = cvB(o_b + 1536, 256)
        d8 = cvB(o_b + 1792, 8)
        rows = [cvB(o_b + 1800, 16).rearrange("p (a b) -> p a b", a=8), cvB(o_b + 1816, 16).rearrange("p (a b) -> p a b", a=8)]
        zslot = cvB(o_b + 1832, NSLOT * 2 // 128)
        assert o_b + 1832 + NSLOT * 2 // 128 <= 8320
        wgC = [cvA(3072 * i, 1024, BF16) for i in range(3)]
        wuC = [cvA(3072 * i + 1024, 1024, BF16) for i in range(3)]
        wdC = [cvA(3072 * i + 2048, 1024, BF16).rearrange("p (a b) -> p a b", a=2) for i in range(3)]
        xgC = [cvA(9216 + 512 * i, 512, BF16) for i in range(3)]
        xgT = [cvA(10752 + 512 * i, 512, BF16).rearrange("p (a b) -> p a b", a=8) for i in range(2)]
        silC = [cvA(11776, 256), cvA(12032, 256)]
        aTC = [cvA(12288, 128, BF16).rearrange("p (a b) -> p a b", a=2), cvA(12416, 128, BF16).rearrange("p (a b) -> p a b", a=2)]
        ysb = [cvA(12544, 512, BF16), cvA(13056, 512, BF16)]
        wA = cvA(13568, NBLK)
        tokuA = cvA(13568 + NBLK, NBLK, U32)
        sl6 = [cvA(13568 + 2 * NBLK, 256), cvA(13568 + 2 * NBLK + 256, 256)]
        sl6c = [cvA(13568 + 2 * NBLK + 512, 128), cvA(13568 + 2 * NBLK + 640, 128)]
        assert 13568 + 2 * NBLK + 768 <= 16384 and NBLK % 128 == 0
        yg = [cvA(4096 * i, 4096, BF16).rearrange("p (a b) -> p a b", a=8) for i in range(3)]
        accD = [cvA(12288, 1024), cvA(13312, 1024)]

        hT = sb("hT", [128, 8, S], BF16)
        tmpF = sb("tmpF", [128, 8, CW])
        xch = sb("xch", [128, 8, CW])
        sqc = [sb("sqc%d" % i, [128, CW]) for i in range(2)]
        rstd = sb("rstd", [128, CW])
        modT = sb("modT", [128, 48, NSEQ])
        bada = sb("bada", [128, 48])
        n1g = sb("n1g_s", [128, 8])
        n2g = sb("n2g_s", [128, 8])
        A1 = sb("A1", [128, 8])
        A2 = sb("A2", [128, 8])
        cT = sb("cT_s", [128, 8, NSEQ])
        scT = sb("scT", [128, 8, NSEQ])
        winS = [sb("win%d" % i, [128, 8, 128], BF16) for i in range(2)]
        qkg = sb("qkg_s", [128, 2])
        lamv = sb("lamv", [128, 4])
        nlam = sb("nlam", [128, 1])
        sg = sb("sg_s", [128, 128])
        pwS = sb("pw_s", [128, 2, 2, 128], BF16)
        psc = sb("psc_s", [128, 8])
        rc = sb("rc_s", [128, 4, 16])
        woS = [sb("wo%d" % i, [128, 8, 128], BF16) for i in range(2)]
        wr = sb("wr_s", [128, 8, NE])
        wsg = sb("wsg", [128, 8, 256], BF16)
        wsu = sb("wsu", [128, 8, 256], BF16)
        wsd = sb("wsd", [128, 2, 1024], BF16)
        base = sb("base", [128, NE])
        lsm = sb("lsm", [128, 128])
        idtb = sb("idtb", [128, 128], BF16)
        pio8 = sb("pio8", [128, 8])
        dstu = sb("dstu", [128, NT, 8], U32)
        idxW = sb("idxW", [128, NBLK], U32)
        brt = sb("br_s", [128, NE])
        tab = sb("tab_s", [NB, NH])
        idt = sb("idt", [128, 128])
        aid = sb("aid", [128, 128])
        bones = sb("bones", [128, 128])
        ones = sb("ones", [128, 128])
        sqh2 = [sb("sqh%d" % i, [128, 512]) for i in range(2)]
        rsh2 = [sb("rsh%d" % i, [128, 512]) for i in range(2)]
        ET = [sb("ET%d" % i, [128, 512], BF16) for i in range(3)]
        rr = sb("rr", [128, 8])
        att4 = sb("att4", [128, 4, 128])
        att4b = sb("att4b", [128, 4, 128])
        ssq4 = sb("ssq4", [128, 8])
        epsc = sb("epsc", [128, 2])
        gpT = sb("gpT", [128, 512])
        ptmp = sb("ptmp", [128, 512])

        pb = [es.enter_context(nc.psum_tensor("pb%d" % i, [128, 512], F32)) for i in range(8)]
        rpb = [Res() for _ in range(8)]

        R = {}

        def res(name):
            if name not in R:
                R[name] = Res()
            return R[name]

        rbigA = [Res() for _ in range(4)]
        rhT = [Res() for _ in range(8)]
        rmT = [[Res() for _ in range(4)] for _ in range(8)]

        def load(dst, src, name, eng="sp"):
            sc.dma(eng, "ld_" + name, dst, src, writes=[res(name)])

        load(bada[:], bada_d, "bada")
        load(n1g[:], n1g_d, "n1g")
        load(n2g[:], n2g_d, "n2g")
        load(cT[:], cT_d, "cT")
        load(qkg[:], qkg_d, "qkg")
        load(lam_in[:], lam_d, "lam_in")
        load(sg[:], sg_d, "sg")
        load(psc[:], psc_d, "psc")
        load(rc[:], rc_d, "rc")
        load(wr[:], wr_d, "wr")
        load(brt[:], br_d, "brt")
        load(tab[:], tab_d, "tab")
        load(oh[:], oh_d, "oh")
        load(idt[:], idt_d, "idt")
        load(aid[:], aid_d, "aid")
        load(bones[:], bones_d, "bones")
        load(lsm[:], ls_d, "lsm")
        load(pio8[:], pio_d, "pio8")
        sc.dma("pool", "ld_wsg", wsg[:], wg_d[NE], writes=[res("wsg")])
        sc.dma("pool", "ld_wsu", wsu[:], wu_d[NE], writes=[res("wsu")])
        sc.dma("pool", "ld_wsd", wsd[:], wd_d[NE], writes=[res("wsd")])
        sc.op("dve", lambda e: e.memset(base[:], 0.0), writes=[res("base")])
        sc.op("dve", lambda e: e.tensor_copy(out=idtb[:], in_=idt[:]), reads=[res("idt")], writes=[res("idtb")])
        sc.op("dve", lambda e: e.memset(ones[:], 1.0), writes=[res("ones")])
        sc.op("dve", lambda e: e.memset(epsc[:, 0:1], float(EPS)), writes=[res("epsc")])
        sc.op("dve", lambda e: e.memset(epsc[:, 1:2], float(64 * EPS)), reads=[res("epsc")], writes=[res("epsc")])
        sc.op("dve", lambda e: e.tensor_scalar(sg[:], sg[:], float(1.0 - LAM_INIT), None, ALU.mult),
              reads=[res("sg")], writes=[res("sg")])
        sc.op("dve", lambda e: e.tensor_tensor(lamt[:, 0, :], lam_in[:, 0, :], lam_in[:, 1, :], ALU.mult),
              reads=[res("lam_in")], writes=[res("lamt")])
        sc.op("dve", lambda e: e.tensor_tensor(lamt[:, 1, :], lam_in[:, 2, :], lam_in[:, 3, :], ALU.mult),
              reads=[res("lam_in"), res("lamt")], writes=[res("lamt")])
        sc.op("dve", lambda e: e.tensor_reduce(lamv[:, 0:2], lamt[:, 0:2, :], AX.X, ALU.add),
              reads=[res("lamt")], writes=[res("lamv")])
        sc.op("act", lambda e: e.activation(lamv[:, 2:4], lamv[:, 0:2], AF.Exp),
              reads=[res("lamv")], writes=[res("lamv")])
        sc.op("dve", lambda e: e.scalar_tensor_tensor(nlam[:], lamv[:, 3:4], float(-LAM_INIT), lamv[:, 2:3],
                                                      ALU.add, ALU.subtract),
              reads=[res("lamv")], writes=[res("nlam")])

        sc.op("act", lambda e: e.activation(scT[:], cT[:], AF.Silu), reads=[res("cT")], writes=[res("scT")])
        for fc in range(48):
            wb = wadaS[fc % 2]
            rw = res("wada%d" % (fc % 2))
            sc.dma("sp", "ld_wada%d" % (fc % 2), wb[:], wada_d[fc], writes=[rw])
            for kc in range(8):
                sc.op("pe", lambda e, wb=wb, kc=kc: e.matmul(pb[7][:, 0:NSEQ], lhsT=wb[:, kc, :], rhs=scT[:, kc, :],
                                                              start=(kc == 0), stop=(kc == 7)),
                      reads=[rw, res("scT")], writes=[rpb[7]])
            sc.op("dve", lambda e, fc=fc: e.tensor_scalar(modT[:, fc, :], pb[7][:, 0:NSEQ], bada[:, fc:fc + 1], None,
                                                           ALU.add),
                  reads=[rpb[7], res("bada")], writes=[res("modT")])

        for c0 in range(0, FVW, 512):
            cw = min(512, FVW - c0)
            sc.op("pe", lambda e, c0=c0, cw=cw: e.matmul(pb[7][0:NH, 0:cw], lhsT=tab[:, :], rhs=oh[:, c0:c0 + cw],
                                                          start=True, stop=True),
                  reads=[res("tab"), res("oh")], writes=[rpb[7]])
            sc.op("dve", lambda e, c0=c0, cw=cw: e.tensor_copy(out=fvs[:, c0:c0 + cw], in_=pb[7][0:NH, 0:cw]),
                  reads=[rpb[7]], writes=[res("fvs")])
        sc.dma("sp", "st_fv", fv_d, fvs[:], reads=[res("fvs")], writes=[res("fv_d")])
        for h in range(NH):
            src = bass.AP(fv_d.tensor, h * FVW, [[1, 128], [1, MW]])
            sc.dma("sp", "ld_hank", hank[:], src, reads=[res("fv_d")], writes=[res("hank")])
            for c0 in range(0, MW, 512):
                cw = min(512, MW - c0)
                sc.op("pe", lambda e, c0=c0, cw=cw: e.matmul(pb[7][:, 0:cw], lhsT=aid[:], rhs=hank[:, c0:c0 + cw],
                                                              start=True, stop=True),
                      reads=[res("aid"), res("hank")], writes=[rpb[7]])
                sc.op("dve", lambda e, c0=c0, cw=cw: e.tensor_copy(out=mst[:, c0:c0 + cw], in_=pb[7][:, 0:cw]),
                      reads=[rpb[7]], writes=[res("mst")])
            sc.dma("sp", "st_mst", mst_d[h], mst[:], reads=[res("mst")], writes=[res("mst_d")])

        win_ctr = [0]

        def load_win(chunk):
            i = win_ctr[0] % 2
            win_ctr[0] += 1
            sc.dma("pool", "ld_win%d" % i, winS[i][:], win_d[chunk], writes=[res("win%d" % i)])
            return winS[i], res("win%d" % i)

        def rmsnorm_chunk(Acol, shcol0, s, ts, rdst, f32_out):
            for c in range(8):
                q = sqc[c % 2]
                rq = res("sqc%d" % (c % 2))
                sc.op("act", lambda e, c=c, q=q: e.activation(q[:], xch[:, c, :], AF.Square),
                      reads=[res("xch")], writes=[rq])
                sc.op("pe", lambda e, c=c, q=q: e.matmul(pb[7][:, 0:CW], lhsT=ones[:], rhs=q[:],
                                                         start=(c == 0), stop=(c == 7)),
                      reads=[res("ones"), rq], writes=[rpb[7]])
            sc.op("act", lambda e: e.activation(rstd[:], pb[7][:, 0:CW], AF.Sqrt, bias=float(EPS), scale=1.0 / D),
                  reads=[rpb[7]], writes=[res("rstd")])
            sc.op("dve", lambda e: e.reciprocal(rstd[:], rstd[:]), reads=[res("rstd")], writes=[res("rstd")])
            for c in range(8):
                sc.op("dve", lambda e, c=c: e.scalar_tensor_tensor(
                    tmpF[:, c, :], xch[:, c, :], Acol[:, c:c + 1], rstd[:], ALU.mult, ALU.mult),
                    reads=[res("xch"), res("rstd"), res("Acol")], writes=[res("tmpF%d" % c)])
                if not f32_out:
                    sc.op("act", lambda e, c=c: e.activation(
                        hT[:, c, ts], tmpF[:, c, :], AF.Identity, bias=modT[:, shcol0 + c, s:s + 1], scale=1.0),
                        reads=[res("tmpF%d" % c), res("modT")], writes=[rdst])
                else:
                    sc.op("act", lambda e, c=c: e.activation(
                        tmpF[:, c, :], tmpF[:, c, :], AF.Identity, bias=modT[:, shcol0 + c, s:s + 1], scale=1.0),
                        reads=[res("tmpF%d" % c), res("modT")], writes=[res("tmpF%d" % c)])
                    sc.op("dve", lambda e, c=c: e.tensor_copy(out=hT[:, c, ts], in_=tmpF[:, c, :]),
                          reads=[res("tmpF%d" % c)], writes=[rdst])

        rtmpF = [res("tmpF%d" % c) for c in range(8)]

        for s in range(NSEQ):
            sc.fence()
            sc.op("dve", lambda e: e.memset(vA[:], 1.0), writes=[res("vA")])
            sc.op("dve", lambda e: e.memset(qz[0][64:128, :], 0.0), writes=[res("qT")])
            sc.op("dve", lambda e: e.memset(qz[1][0:64, :], 0.0), writes=[res("qT")])
            sc.op("dve", lambda e, s=s: e.scalar_tensor_tensor(A1[:], modT[:, 8:16, s], 1.0, n1g[:], ALU.add, ALU.mult),
                  reads=[res("modT"), res("n1g")], writes=[res("Acol")])
            for j in range(8):
                ts = slice(j * CW, (j + 1) * CW)
                sc.dma("sp", "ld_x", xch[:], xT_d[s][:, :, ts], writes=[res("xch")])
                rmsnorm_chunk(A1, 0, s, ts, rhT[j], False)

            for h in range(NH):
                sc.dma("sp", "ld_mst", mst[:], mst_d[h], reads=[res("mst_d")], writes=[res("mst")])
                for which, dstT, rname in ((0, None, "qT"), (1, kT, "kT")):
                    wb, rw = load_win(which * 8 + h)
                    for j in range(4):
                        ts = slice(j * 512, (j + 1) * 512)
                        pa = 4 + 2 * (j % 2)
                        pn = pa + 1
                        sq_, rs_ = sqh2[j % 2], rsh2[j % 2]
                        rsq, rrs = res("sqh%d" % (j % 2)), res("rsh%d" % (j % 2))
                        for kc in range(8):
                            sc.op("pe", lambda e, wb=wb, kc=kc, ts=ts, pa=pa: e.matmul(
                                pb[pa][:], lhsT=wb[:, kc, :], rhs=hT[:, kc, ts], start=(kc == 0), stop=(kc == 7)),
                                reads=[rw, rhT[2 * j], rhT[2 * j + 1]], writes=[rpb[pa]])
                        sc.op("act", lambda e, pa=pa, sq_=sq_: e.activation(sq_[:], pb[pa][:], AF.Square),
                              reads=[rpb[pa]], writes=[rsq])
                        sc.op("pe", lambda e, pn=pn, sq_=sq_: e.matmul(pb[pn][:], lhsT=bones[:], rhs=sq_[:], start=True, stop=True),
                              reads=[res("bones"), rsq], writes=[rpb[pn]])
                        if which == 0:
                            sc.op("act", lambda e, pn=pn, rs_=rs_: e.activation(rs_[:], pb[pn][:], AF.Ln, bias=epsc[:, 1:2], scale=1.0),
                                  reads=[rpb[pn], res("epsc")], writes=[rrs])
                        else:
                            sc.op("act", lambda e, pn=pn, rs_=rs_: e.activation(rs_[:], pb[pn][:], AF.Ln, bias=epsc[:, 0:1], scale=1.0 / 64),
                                  reads=[rpb[pn], res("epsc")], writes=[rrs])
                        sc.op("act", lambda e, rs_=rs_: e.activation(rs_[:], rs_[:], AF.Exp, scale=-0.5), reads=[rrs], writes=[rrs])
                        if which == 1:
                            sc.op("dve", lambda e, dstT=dstT, ts=ts, which=which, pa=pa, rs_=rs_: e.scalar_tensor_tensor(
                                dstT[:, ts], pb[pa][:], qkg[:, which:which + 1], rs_[:], ALU.mult, ALU.mult),
                                reads=[rpb[pa], rrs, res("qkg")], writes=[res(rname)])
                        else:
                            for comp in range(2):
                                pr = slice(comp * 64, (comp + 1) * 64)
                                sc.op("dve", lambda e, ts=ts, pa=pa, rs_=rs_, comp=comp, pr=pr: e.scalar_tensor_tensor(
                                    qz[comp][pr, ts], pb[pa][pr, :], qkg[pr, 0:1], rs_[pr, :], ALU.mult, ALU.mult),
                                    reads=[rpb[pa], rrs, res("qkg")], writes=[res(rname)])
                wb, rw = load_win(16 + h)
                for t in range(16):
                    tt = slice(t * 128, (t + 1) * 128)
                    vb = 4 + (t % 4)
                    for kc in range(8):
                        sc.op("pe", lambda e, wb=wb, kc=kc, tt=tt, vb=vb: e.matmul(
                            pb[vb][:, 0:128], lhsT=hT[:, kc, tt], rhs=wb[:, kc, :], start=(kc == 0), stop=(kc == 7)),
                            reads=[rw, rhT[t // 2]], writes=[rpb[vb]])
                    if t % 2 == 0:
                        sc.op("act", lambda e, t=t, vb=vb: e.activation(vA[:, t, 0:128], pb[vb][:, 0:128], AF.Copy),
                              reads=[rpb[vb]], writes=[res("vA")])
                    else:
                        sc.op("dve", lambda e, t=t, vb=vb: e.tensor_copy(out=vA[:, t, 0:128], in_=pb[vb][:, 0:128]),
                              reads=[rpb[vb]], writes=[res("vA")])
                wb, rw = load_win(32 + h)
                for j in range(4):
                    ts = slice(j * 512, (j + 1) * 512)
                    gb = 4 + j
                    for kc in range(8):
                        sc.op("pe", lambda e, wb=wb, kc=kc, ts=ts, gb=gb: e.matmul(
                            pb[gb][:], lhsT=wb[:, kc, :], rhs=hT[:, kc, ts], start=(kc == 0), stop=(kc == 7)),
                            reads=[rw, rhT[2 * j], rhT[2 * j + 1]], writes=[rpb[gb]])
                    sc.op("act", lambda e, ts=ts, gb=gb: e.activation(gaT[:, ts], pb[gb][:], AF.Sigmoid),
                          reads=[rpb[gb]], writes=[res("gaT")])
                tiles = [(j, comp, kb) for j in range(4) for comp in range(2) for kb in range(16)]

                def emit_st(n):
                    j, comp, kb = tiles[n]
                    bi = 4 + (n % 3)
                    ks = slice(kb * 128, (kb + 1) * 128)
                    qs = slice(j * 512, (j + 1) * 512)
                    sc.op("pe", lambda e, bi=bi, comp=comp, ks=ks, qs=qs: e.matmul(
                        pb[bi][:], lhsT=kT[:, ks], rhs=qz[comp][:, qs], start=True, stop=True),
                        reads=[res("qT"), res("kT")], writes=[rpb[bi]])

                emit_st(0)
                emit_st(1)
                for n, (j, comp, kb) in enumerate(tiles):
                    if n + 2 < len(tiles):
                        emit_st(n + 2)
                    bi = 4 + (n % 3)
                    ei = n % 3
                    o = kb - 4 * j
                    rET = res("ET%d" % ei)
                    if o <= -2 or o >= 5:
                        col = (MW - 1) if o <= -2 else 0
                        sc.op("act", lambda e, bi=bi, ei=ei, col=col: e.activation(
                            ET[ei][:], pb[bi][:], AF.Exp, bias=mst[:, col:col + 1], scale=1.0),
                            reads=[rpb[bi], res("mst")], writes=[rET])
                    else:
                        m0 = 640 - 128 * o
                        si = n % 2
                        rsa = res("sadd%d" % si)
                        sc.op("dve", lambda e, bi=bi, si=si, m0=m0: e.tensor_tensor(
                            sadd[si][:], pb[bi][:], mst[:, m0:m0 + 512], ALU.add),
                            reads=[rpb[bi], res("mst")], writes=[rsa])
                        sc.op("act", lambda e, ei=ei, si=si: e.activation(ET[ei][:], sadd[si][:], AF.Exp),
                              reads=[rsa], writes=[rET])
                    for sub in range(4):
                        sc.op("pe", lambda e, sub=sub, ei=ei, kb=kb: e.matmul(
                            pb[sub][:, 0:129], lhsT=ET[ei][:, sub * 128:(sub + 1) * 128], rhs=vA[:, kb, 0:129],
                            start=(kb == 0), stop=(kb == 15)),
                            reads=[rET, res("vA")], writes=[rpb[sub]])
                    if kb == 15:
                        for sub in range(4):
                            eng = "act" if sub % 2 == 0 else "dve"
                            if eng == "act":
                                sc.op("act", lambda e, sub=sub, comp=comp: e.activation(
                                    O[comp][:, sub, 0:129], pb[sub][:, 0:129], AF.Copy),
                                    reads=[rpb[sub]], writes=[res("O%d" % comp)])
                            else:
                                sc.op("dve", lambda e, sub=sub, comp=comp: e.tensor_copy(
                                    out=O[comp][:, sub, 0:129], in_=pb[sub][:, 0:129]),
                                    reads=[rpb[sub]], writes=[res("O%d" % comp)])
                    if kb == 15 and comp == 1:
                        ts = slice(j * 512, (j + 1) * 512)
                        sc.op("dve", lambda e: e.reciprocal(rr[:, 0:4], O[0][:, :, 128]),
                              reads=[res("O0")], writes=[res("rr")])
                        sc.op("dve", lambda e: e.reciprocal(rr[:, 4:8], O[1][:, :, 128]),
                              reads=[res("O1"), res("rr")], writes=[res("rr")])
                        sc.op("dve", lambda e: e.tensor_scalar(rr[:, 4:8], rr[:, 4:8], nlam[:, 0:1], None, ALU.mult),
                              reads=[res("rr"), res("nlam")], writes=[res("rr")])
                        sc.op("dve", lambda e: e.tensor_tensor(
                            att4[:, :, :], O[0][:, :, 0:128], rr[:, 0:4].unsqueeze(2).to_broadcast([128, 4, 128]), ALU.mult),
                            reads=[res("O0"), res("rr")], writes=[res("att4")])
                        sc.op("dve", lambda e: e.tensor_tensor(
                            att4b[:, :, :], O[1][:, :, 0:128], rr[:, 4:8].unsqueeze(2).to_broadcast([128, 4, 128]), ALU.mult),
                            reads=[res("O1"), res("rr")], writes=[res("att4b")])
                        sc.op("dve", lambda e: e.tensor_tensor(att4[:, :, :], att4[:, :, :], att4b[:, :, :], ALU.add),
                              reads=[res("att4"), res("att4b")], writes=[res("att4")])
                        sc.op("dve", lambda e: e.tensor_tensor(att4b[:, :, :], att4[:, :, :], att4[:, :, :], ALU.mult),
                              reads=[res("att4")], writes=[res("att4b")])
                        sc.op("dve", lambda e: e.tensor_reduce(ssq4[:, 0:4], att4b[:, :, :], AX.X, ALU.add),
                              reads=[res("att4b")], writes=[res("ssq4")])
                        sc.op("act", lambda e: e.activation(ssq4[:, 4:8], ssq4[:, 0:4], AF.Ln, bias=epsc[:, 0:1], scale=1.0 / 128),
                              reads=[res("ssq4"), res("epsc")], writes=[res("ssq4")])
                        sc.op("act", lambda e: e.activation(ssq4[:, 4:8], ssq4[:, 4:8], AF.Exp, scale=-0.5),
                              reads=[res("ssq4")], writes=[res("ssq4")])
                        sc.op("dve", lambda e: e.tensor_tensor(
                            att4[:, :, :], att4[:, :, :], ssq4[:, 4:8].unsqueeze(2).to_broadcast([128, 4, 128]), ALU.mult),
                            reads=[res("att4"), res("ssq4")], writes=[res("att4")])
                        sc.op("dve", lambda e: e.tensor_tensor(
                            att4[:, :, :], att4[:, :, :], sg[:, :].unsqueeze(1).to_broadcast([128, 4, 128]), ALU.mult),
                            reads=[res("att4"), res("sg")], writes=[res("att4")])
                        for sub in range(4):
                            sc.op("pe", lambda e, sub=sub: e.transpose(pb[7][:, sub * 128:(sub + 1) * 128], att4[:, sub, :], idt[:]),
                                  reads=[res("att4"), res("idt")], writes=[rpb[7]])
                        sc.op("dve", lambda e, ts=ts, h=h: e.tensor_tensor(mT[:, h, ts], pb[7][:, :], gaT[:, ts], ALU.mult),
                              reads=[rpb[7], res("gaT")], writes=[rmT[h][j]])

            sc.fence()
            sc.op("dve", lambda e: e.memset(pP[:], 0.0), writes=[res("pP")])
            for g in range(4):
                wnd = (2, 4, 8, 16)[g]
                for dc in range(2):
                    sc.dma("pool", "ld_pw", pwS[:, dc, :, :], pw_d[g, dc], writes=[res("pw")])
                for cc in range(2):
                    chunk = 2 * g + cc
                    wb, rw = load_win(24 + chunk)
                    for j in range(4):
                        ts = slice(j * 512, (j + 1) * 512)
                        for kc in range(8):
                            sc.op("pe", lambda e, wb=wb, kc=kc, ts=ts: e.matmul(
                                pb[6][:], lhsT=wb[:, kc, :], rhs=hT[:, kc, ts], start=(kc == 0), stop=(kc == 7)),
                                reads=[rw, rhT[2 * j], rhT[2 * j + 1]], writes=[rpb[6]])
                        sc.op("act", lambda e, j=j: e.activation(pP[:, PADL + j * 512:PADL + (j + 1) * 512], pb[6][:],
                                                                 AF.Copy),
                              reads=[rpb[6]], writes=[res("pP")])
                    L = LP
                    sc.op("dve", lambda e: e.tensor_tensor(pW[0][:, 1:L], pP[:, 0:L - 1], pP[:, 1:L], ALU.add),
                          reads=[res("pP")], writes=[res("pW0")])
                    cur = 0
                    for lvl, sh in ((4, 1), (8, 2), (16, 4)):
                        if wnd < lvl:
                            break
                        nxt = 1 - cur
                        sc.op("dve", lambda e, cur=cur, nxt=nxt, sh=sh: e.tensor_tensor(
                            pW[nxt][:, sh:L - sh], pW[cur][:, 0:L - 2 * sh], pW[cur][:, 2 * sh:L], ALU.add),
                            reads=[res("pW%d" % cur)], writes=[res("pW%d" % nxt)])
                        cur = nxt
                    Wc = pW[cur]
                    rWc = res("pW%d" % cur)
                    sc.op("dve", lambda e, Wc=Wc, cc=cc, wnd=wnd: e.scalar_tensor_tensor(
                        mixT[:, cc, :], Wc[:, PADL:PADL + S], 1.0 / wnd, pP[:, PADL:PADL + S], ALU.mult, ALU.subtract),
                        reads=[rWc, res("pP")], writes=[res("mixT")])
                    for (c0, r0) in ((0, 0), (S - 8, 8)):
                        sc.op("dve", lambda e, Wc=Wc, c0=c0, r0=r0, g=g: e.tensor_tensor(
                            ptmp[:, 0:8], Wc[:, PADL + c0:PADL + c0 + 8], rc[:, g, r0:r0 + 8], ALU.mult),
                            reads=[rWc, res("rc")], writes=[res("ptmp")])
                        sc.op("dve", lambda e, c0=c0, cc=cc: e.tensor_tensor(
                            mixT[:, cc, c0:c0 + 8], ptmp[:, 0:8], pP[:, PADL + c0:PADL + c0 + 8], ALU.subtract),
                            reads=[res("ptmp"), res("pP"), res("mixT")], writes=[res("mixT")])
                for dc in range(2):
                    chunk = 2 * g + dc
                    wb, rw = load_win(40 + chunk)
                    for j in range(4):
                        ts = slice(j * 512, (j + 1) * 512)
                        for kc in range(8):
                            sc.op("pe", lambda e, wb=wb, kc=kc, ts=ts: e.matmul(
                                pb[6][:], lhsT=wb[:, kc, :], rhs=hT[:, kc, ts], start=(kc == 0), stop=(kc == 7)),
                                reads=[rw, rhT[2 * j], rhT[2 * j + 1]], writes=[rpb[6]])
                        sc.op("act", lambda e: e.activation(gpT[:], pb[6][:], AF.Sigmoid),
                              reads=[rpb[6]], writes=[res("gpT")])
                        for cc in range(2):
                            sc.op("pe", lambda e, dc=dc, cc=cc, ts=ts: e.matmul(
                                pb[5][:], lhsT=pwS[:, dc, cc, :], rhs=mixT[:, cc, ts], start=(cc == 0), stop=(cc == 1)),
                                reads=[res("pw"), res("mixT")], writes=[rpb[5]])
                        sc.op("dve", lambda e, chunk=chunk: e.scalar_tensor_tensor(
                            ptmp[:], pb[5][:], psc[:, chunk:chunk + 1], gpT[:], ALU.mult, ALU.mult),
                            reads=[rpb[5], res("gpT"), res("psc")], writes=[res("ptmp")])
                        sc.op("dve", lambda e, chunk=chunk, ts=ts: e.tensor_tensor(
                            mT[:, chunk, ts], mT[:, chunk, ts], ptmp[:], ALU.add),
                            reads=[res("ptmp"), rmT[chunk][j]], writes=[rmT[chunk][j]])

            sc.fence()
            sc.op("dve", lambda e, s=s: e.scalar_tensor_tensor(A2[:], modT[:, 32:40, s], 1.0, n2g[:], ALU.add, ALU.mult),
                  reads=[res("modT"), res("n2g")], writes=[res("Acol")])

            def router(j, part):
                for sub in range(CW // 128):
                    gt = s * 16 + (CW // 128) * j + sub
                    Wt = Wtok[gt % 2]
                    rWt = res("Wtok%d" % (gt % 2))
                    pS = posS[gt % 2]
                    rpS = res("posS%d" % (gt % 2))
                    if part == "b":
                        sc.op("pe", lambda e, sub=sub: e.matmul(pb[5][:, 0:NE], lhsT=lsm[:], rhs=selT2[sub][:], start=True, stop=True),
                              reads=[res("lsm"), res("selT%d" % sub)], writes=[rpb[5]])
                        sc.op("dve", lambda e, pS=pS: e.tensor_tensor(pS[:], pb[5][:, 0:NE], base[:], ALU.add),
                              reads=[rpb[5], res("base")], writes=[rpS])
                        sc.op("pe", lambda e, sub=sub: e.matmul(pb[4][:, 0:NE], lhsT=ones[:], rhs=selT2[sub][:], start=True, stop=True),
                              reads=[res("ones"), res("selT%d" % sub)], writes=[rpb[4]])
                        sc.op("dve", lambda e: e.tensor_tensor(base[:], pb[4][:, 0:NE], base[:], ALU.add),
                              reads=[rpb[4], res("base")], writes=[res("base")])
                        sc.dma("sp", "st_pos", pos_d[gt], pS[:], reads=[rpS], writes=[res("pos_d")])
                        sc.dma("sp", "st_wt", wt_d[gt], Wt[:], reads=[rWt], writes=[res("wt_d")])
                        continue
                    if part == "h":
                        hb = h2tm[gt % 2]
                        rhb = res("h2tm%d" % (gt % 2))
                        tl = slice((CW * j) + sub * 128, (CW * j) + (sub + 1) * 128)
                        pbt = pb[7][:, :].bitcast(BF16)
                        for kc in range(8):
                            sc.op("pe", lambda e, kc=kc, tl=tl, pbt=pbt: e.transpose(pbt[:, kc * 128:(kc + 1) * 128], hT[:, kc, tl], idtb[:]),
                                  reads=[rhT[j], res("idtb")], writes=[rpb[7]])
                        sc.op("act", lambda e, hb=hb, pbt=pbt: e.activation(hb[:], pbt[:, :], AF.Copy),
                              reads=[rpb[7]], writes=[rhb])
                        sc.dma("sp", "st_h2", h2_d[gt * 128:(gt + 1) * 128, :], hb[:], reads=[rhb], writes=[res("h2_d")])
                        continue
                    for kc in range(8):
                        sc.op("pe", lambda e, kc=kc, sub=sub: e.matmul(
                            pb[6][:, 0:NE], lhsT=tmpF[:, kc, sub * 128:(sub + 1) * 128], rhs=wr[:, kc, :],
                            start=(kc == 0), stop=(kc == 7)),
                            reads=[rtmpF[kc], res("wr")], writes=[rpb[6]])
                    sc.op("act", lambda e: e.activation(scr[:], pb[6][:, 0:NE], AF.Sigmoid),
                          reads=[rpb[6]], writes=[res("scr")])
                    sc.op("dve", lambda e: e.tensor_tensor(bia[:], scr[:], brt[:], ALU.add),
                          reads=[res("scr"), res("brt")], writes=[res("bia")])
                    for gi in range(8):
                        sc.op("dve", lambda e, gi=gi: e.max(out=m8[:, gi, :], in_=bia[:, gi * 32:(gi + 1) * 32]),
                              reads=[res("bia")], writes=[res("m8")])
                    sc.op("dve", lambda e: e.tensor_tensor(gsc[:], m8[:, :, 0], m8[:, :, 1], ALU.add),
                          reads=[res("m8")], writes=[res("gsc")])
                    sc.op("dve", lambda e: e.max(out=gm8[:], in_=gsc[:]), reads=[res("gsc")], writes=[res("gm8")])
                    sc.op("dve", lambda e: e.tensor_scalar(gmk[:], gsc[:], gm8[:, 3:4], None, ALU.is_ge),
                          reads=[res("gsc"), res("gm8")], writes=[res("gmk")])
                    sc.op("dve", lambda e: e.tensor_scalar(msk[:], bia[:], 2.0, None, ALU.add),
                          reads=[res("bia")], writes=[res("msk")])
                    sc.op("dve", lambda e: e.tensor_tensor(
                        msk[:, :].rearrange("p (g k) -> p g k", g=8), msk[:, :].rearrange("p (g k) -> p g k", g=8),
                        gmk[:, :].unsqueeze(2).to_broadcast([128, 8, 32]), ALU.mult),
                        reads=[res("msk"), res("gmk")], writes=[res("msk")])
                    sc.op("dve", lambda e: e.max(out=t8[:], in_=msk[:]), reads=[res("msk")], writes=[res("t8")])
                    sc.op("dve", lambda e, Wt=Wt: e.scalar_tensor_tensor(
                        Wt[:], msk[:], t8[:, 7:8], scr[:], ALU.is_ge, ALU.mult),
                        reads=[res("msk"), res("t8"), res("scr")], writes=[rWt])
                    sc.op("dve", lambda e, Wt=Wt: e.tensor_reduce(den[:, 0:1], Wt[:], AX.X, ALU.add),
                          reads=[rWt], writes=[res("den")])
                    sc.op("dve", lambda e: e.reciprocal(den[:, 1:2], den[:, 0:1]),
                          reads=[res("den")], writes=[res("den")])
                    sc.op("dve", lambda e, Wt=Wt: e.tensor_scalar(Wt[:], Wt[:], den[:, 1:2], 2.5, ALU.mult, ALU.mult),
                          reads=[rWt, res("den")], writes=[rWt])
                    sc.op("dve", lambda e, Wt=Wt, sub=sub: e.tensor_scalar(selT2[sub][:], Wt[:], 0.0, None, ALU.is_gt),
                          reads=[rWt], writes=[res("selT%d" % sub)])

            for j in range(8):
                ts = slice(j * CW, (j + 1) * CW)
                sc.dma("sp", "ld_x", xch[:], xT_d[s][:, :, ts], writes=[res("xch")])
                for oc in range(8):
                    wb = woS[oc % 2]
                    rw = res("wo%d" % (oc % 2))
                    sc.dma("pool", "ld_wo%d" % (oc % 2), wb[:], wo_d[oc], writes=[rw])
                    for kc in range(8):
                        sc.op("pe", lambda e, wb=wb, kc=kc, ts=ts: e.matmul(
                            pb[4][:, 0:CW], lhsT=wb[:, kc, :], rhs=mT[:, kc, ts], start=(kc == 0), stop=(kc == 7)),
                            reads=[rw], writes=[rpb[4]])
                    sc.op("dve", lambda e, oc=oc, s=s: e.scalar_tensor_tensor(
                        xch[:, oc, :], pb[4][:, 0:CW], modT[:, 16 + oc, s:s + 1], xch[:, oc, :], ALU.mult, ALU.add),
                        reads=[rpb[4], res("modT"), res("xch")], writes=[res("xch")])
                rmsnorm_chunk(A2, 24, s, ts, rhT[j], True)
                router(j, "a")
                router(j, "h")
                for half in range(2):
                    hs = slice(half * 128, (half + 1) * 128)
                    for kc in range(8):
                        sc.op("pe", lambda e, kc=kc, hs=hs, ts=ts, half=half: e.matmul(
                            pb[half][:, 0:CW], lhsT=wsg[:, kc, hs], rhs=hT[:, kc, ts], start=(kc == 0), stop=(kc == 7)),
                            reads=[res("wsg"), rhT[j]], writes=[rpb[half]])
                    for kc in range(8):
                        sc.op("pe", lambda e, kc=kc, hs=hs, ts=ts, half=half: e.matmul(
                            pb[2 + half][:, 0:CW], lhsT=wsu[:, kc, hs], rhs=hT[:, kc, ts], start=(kc == 0), stop=(kc == 7)),
                            reads=[res("wsu"), rhT[j]], writes=[rpb[2 + half]])
                router(j, "b")
                for half in range(2):
                    sc.op("act", lambda e, half=half: e.activation(sil[:, half, 0:CW], pb[half][:, 0:CW], AF.Silu),
                          reads=[rpb[half]], writes=[res("sil%d" % half)])
                    sc.op("dve", lambda e, half=half: e.tensor_tensor(
                        aT[:, half, 0:CW], sil[:, half, 0:CW], pb[2 + half][:, 0:CW], ALU.mult),
                        reads=[res("sil%d" % half), rpb[2 + half]], writes=[res("aT")])
                for dcn in range(8):
                    bi = 5 + (dcn % 2)
                    for cc in range(2):
                        sc.op("pe", lambda e, cc=cc, dcn=dcn, bi=bi: e.matmul(
                            pb[bi][:, 0:CW], lhsT=wsd[:, cc, dcn * 128:(dcn + 1) * 128], rhs=aT[:, cc, 0:CW],
                            start=(cc == 0), stop=(cc == 1)),
                            reads=[res("wsd"), res("aT")], writes=[rpb[bi]])
                    sc.op("dve", lambda e, dcn=dcn, bi=bi, s=s: e.scalar_tensor_tensor(
                        xch[:, dcn, :], pb[bi][:, 0:CW], modT[:, 40 + dcn, s:s + 1], xch[:, dcn, :], ALU.mult, ALU.add),
                        reads=[rpb[bi], res("modT"), res("xch")], writes=[res("xch")])
                sc.dma("sp", "st_xs", xs_d[s][:, :, ts], xch[:], reads=[res("xch")], writes=[res("xs_d")])

        sc.fence()
        us = [cvA(0, 256), cvA(256, 256)]
        ui = [cvA(512, 256), cvA(768, 256)]
        iot = cvA(1024, 1024)
        for c in range(2):
            sc.dma("sp", "ld_us", us[c][:], us_d[c], writes=[res("us%d" % c)])
            sc.dma("sp", "ld_ui", ui[c][:], ui_d[c], writes=[res("ui%d" % c)])
        sc.dma("sp", "ld_iot", iot[:], iot_d, writes=[res("iot")])
        sc.op("dve", lambda e: e.tensor_scalar(nbf[:], base[:], 127.0, None, ALU.add), reads=[res("base")], writes=[res("nbf")])
        sc.op("dve", lambda e: e.tensor_copy(out=nbi[:], in_=nbf[:]), reads=[res("nbf")], writes=[res("nbi")])
        sc.op("dve", lambda e: e.tensor_single_scalar(nbi[:], nbi[:], 7, ALU.arith_shift_right),
              reads=[res("nbi")], writes=[res("nbi")])
        sc.op("dve", lambda e: e.tensor_copy(out=nbf[:], in_=nbi[:]), reads=[res("nbi")], writes=[res("nbf")])
        for c in range(2):
            sc.op("pe", lambda e, c=c: e.transpose(pb[c][:, 0:128], nbf[:, c * 128:(c + 1) * 128], idt[:]),
                  reads=[res("nbf"), res("idt")], writes=[rpb[c]])
            sc.op("dve", lambda e, c=c: e.tensor_copy(out=nbT[c][:], in_=pb[c][:, 0:128]), reads=[rpb[c]], writes=[res("nbT%d" % c)])
        for c in range(2):
            sc.op("pe", lambda e, c=c: e.matmul(pb[2][:, 0:NE], lhsT=nbT[c][:], rhs=us[c][:], start=(c == 0), stop=(c == 1)),
                  reads=[res("nbT%d" % c), res("us%d" % c)], writes=[rpb[2]])
        sc.op("dve", lambda e: e.tensor_scalar(sbase[:], pb[2][:, 0:NE], 128.0, None, ALU.mult), reads=[rpb[2]], writes=[res("sbase")])
        for c2 in range(2):
            for c in range(2):
                sc.op("pe", lambda e, c=c, c2=c2: e.matmul(pb[3][:, 0:2], lhsT=ui[c][:, c2 * 128:(c2 + 1) * 128], rhs=nbT[c][:, 0:2],
                                                           start=(c == 0), stop=(c == 1)),
                      reads=[res("nbT%d" % c), res("ui%d" % c)], writes=[rpb[3]])
            sc.op("dve", lambda e, c2=c2: e.tensor_copy(out=bend[:, c2:c2 + 1], in_=pb[3][:, 0:1]), reads=[rpb[3]], writes=[res("bend")])
        for c2 in range(2):
            sc.op("dve", lambda e, c2=c2: e.tensor_scalar(Cm[c2][:], iot[:, 0:NBLK], bend[:, c2:c2 + 1], None, ALU.is_ge),
                  reads=[res("iot"), res("bend")], writes=[res("Cm%d" % c2)])
        for c0 in range(0, NBLK, 512):
            cw = min(512, NBLK - c0)
            for c2 in range(2):
                sc.op("pe", lambda e, c0=c0, cw=cw, c2=c2: e.matmul(pb[4][:, 0:cw], lhsT=ones[:], rhs=Cm[c2][:, c0:c0 + cw],
                                                                    start=(c2 == 0), stop=(c2 == 1)),
                      reads=[res("ones"), res("Cm%d" % c2)], writes=[rpb[4]])
            sc.op("dve", lambda e, c0=c0, cw=cw: e.tensor_scalar(bef[:, c0:c0 + cw], pb[4][:, 0:cw], 128.0, None, ALU.mult),
                  reads=[rpb[4]], writes=[res("bef")])
        sc.op("dve", lambda e: e.tensor_scalar(idxW[:], bef[:], pio8[:, 0:1], None, ALU.add),
              reads=[res("bef"), res("pio8")], writes=[res("idxW")])
        sc.op("dve", lambda e: e.memset(zslot[:], 0.0), writes=[res("zslot")])
        sc.dma("sp", "st_z", slot_d.rearrange("(p a) b -> p (a b)", p=128), zslot[:], reads=[res("zslot")], writes=[res("slot_d")])
        for gt in range(NT):
            b2 = gt % 2
            sc.dma("sp", "ld_pos%d" % b2, posL[b2][:], pos_d[gt], reads=[res("pos_d")], writes=[res("posL%d" % b2)])
            sc.dma("sp", "ld_wt%d" % b2, wtL[b2][:], wt_d[gt], reads=[res("wt_d")], writes=[res("wtL%d" % b2)])
            sc.op("dve", lambda e, b2=b2: e.scalar_tensor_tensor(dp1[:], posL[b2][:], 1.0, sbase[:], ALU.add, ALU.add),
                  reads=[res("posL%d" % b2), res("sbase")], writes=[res("dp1")])
            sc.op("dve", lambda e, b2=b2: e.scalar_tensor_tensor(keyb[:], wtL[b2][:], 0.0, dp1[:], ALU.is_gt, ALU.mult),
                  reads=[res("wtL%d" % b2), res("dp1")], writes=[res("keyb")])
            sc.op("dve", lambda e: e.max(out=d8[:], in_=keyb[:]), reads=[res("keyb")], writes=[res("d8")])
            rrw = res("rows%d" % b2)
            sc.op("dve", lambda e, b2=b2, gt=gt: e.tensor_scalar(rows[b2][:, :, 0], pio8[:], float(gt * 128), None, ALU.add),
                  reads=[res("pio8")], writes=[rrw])
            for k in range(8):
                sc.op("dve", lambda e, b2=b2, k=k: e.scalar_tensor_tensor(
                    junkb[:], keyb[:], d8[:, k:k + 1], wtL[b2][:], ALU.is_equal, ALU.mult, accum_out=rows[b2][:, k, 1:2]),
                    reads=[res("keyb"), res("d8"), res("wtL%d" % b2)], writes=[res("junkb"), rrw])
            sc.op("dve", lambda e, gt=gt: e.tensor_scalar(dstu[:, gt, :], d8[:], -1.0, None, ALU.add),
                  reads=[res("d8")], writes=[res("dstu%d" % gt)])
            for k in range(8):
                sc.idma("sc_slot", slot_d, dstu[:, gt, k:k + 1], rows[b2][:, k, :], None,
                        reads=[rrw, res("dstu%d" % gt)], writes=[res("slot_d")])

        sc.fence()
        slotv = slot_d.rearrange("(i p) c -> i (p c)", p=128)
        for t6 in range(NBLK // 128):
            b2 = t6 % 2
            sc.dma("sp", "ld_srow%d" % b2, sl6[b2][:], slotv[t6 * 128:(t6 + 1) * 128, :], reads=[res("slot_d")],
                   writes=[res("sl6_%d" % b2)])
            v3 = sl6[b2][:, :].rearrange("i (p c) -> i p c", c=2)
            for c in range(2):
                sc.op("dve", lambda e, c=c, v3=v3: e.tensor_copy(out=sl6c[c][:], in_=v3[:, :, c]),
                      reads=[res("sl6_%d" % b2)], writes=[res("sl6c%d" % c)])
                sc.op("pe", lambda e, c=c: e.transpose(pb[c][:, 0:128], sl6c[c][:], idt[:]),
                      reads=[res("sl6c%d" % c), res("idt")], writes=[rpb[c]])
            sc.op("dve", lambda e, t6=t6: e.tensor_copy(out=tokuA[:, t6 * 128:(t6 + 1) * 128], in_=pb[0][:, 0:128]),
                  reads=[rpb[0]], writes=[res("tokuA")])
            sc.op("act", lambda e, t6=t6: e.activation(wA[:, t6 * 128:(t6 + 1) * 128], pb[1][:, 0:128], AF.Copy),
                  reads=[rpb[1]], writes=[res("srowA")])
        WB = NE * 128 - 1

        def gathers(i):
            b3 = i % 3
            sc.idma("g_x%d" % b3, xgC[b3][:], None, h2_d, tokuA[:, i:i + 1], reads=[res("tokuA"), res("h2_d")],
                    writes=[res("xg%d" % b3)])
            sc.idma("g_wg%d" % b3, wgC[b3][:], None, wg2, idxW[:, i:i + 1], reads=[res("idxW")], writes=[res("wgC%d" % b3)], bound=WB)
            sc.idma("g_wu%d" % b3, wuC[b3][:], None, wu2, idxW[:, i:i + 1], reads=[res("idxW")], writes=[res("wuC%d" % b3)], bound=WB)
            sc.idma("g_wd%d" % b3, wdC[b3][:, :, :].rearrange("p a b -> p (a b)"), None, wd2, idxW[:, i:i + 1],
                    reads=[res("idxW")], writes=[res("wdC%d" % b3)], bound=WB)

        def stage1(i):
            b2, b3 = i % 2, i % 3
            pT_, rT_ = pb[4 * b2], rpb[4 * b2]
            pbt = pT_[:, :].bitcast(BF16)
            for kc in range(8):
                sc.op("pe", lambda e, kc=kc, b3=b3, pbt=pbt: e.transpose(
                    pbt[:, kc * 128:(kc + 1) * 128], xgC[b3][:, kc * 128:(kc + 1) * 128], idtb[:]),
                    reads=[res("xg%d" % b3), res("idtb")], writes=[rT_])
            sc.op("act", lambda e, b2=b2, pbt=pbt: e.activation(xgT[b2][:, :, :].rearrange("p a b -> p (a b)"), pbt[:, :], AF.Copy),
                  reads=[rT_], writes=[res("xgT%d" % b2)])

        def stage2(i):
            b2, b3 = i % 2, i % 3
            pG_, rG_ = pb[4 * b2 + 1], rpb[4 * b2 + 1]
            for gu, wC, rn in ((0, wgC, "wgC%d"), (1, wuC, "wuC%d")):
                for half in range(2):
                    col = (gu * 2 + half) * 128
                    for kc in range(8):
                        sc.op("pe", lambda e, kc=kc, b2=b2, b3=b3, wC=wC, half=half, col=col, pG_=pG_: e.matmul(
                            pG_[:, col:col + 128], lhsT=wC[b3][:, kc * 256 + half * 128:kc * 256 + (half + 1) * 128],
                            rhs=xgT[b2][:, kc, :], start=(kc == 0), stop=(kc == 7)),
                            reads=[res(rn % b3), res("xgT%d" % b2)], writes=[rG_])
            sc.op("act", lambda e, b2=b2, pG_=pG_: e.activation(silC[b2][:], pG_[:, 0:256], AF.Silu),
                  reads=[rG_], writes=[res("silC%d" % b2)])
            sc.op("dve", lambda e, b2=b2, pG_=pG_: e.tensor_tensor(
                aTC[b2][:, :, :].rearrange("p a b -> p (a b)"), silC[b2][:], pG_[:, 256:512], ALU.mult),
                reads=[res("silC%d" % b2), rG_], writes=[res("aTC%d" % b2)])

        def stage3(i):
            b2, b3 = i % 2, i % 3
            pY0, pY1, rY0, rY1 = pb[4 * b2 + 2], pb[4 * b2 + 3], rpb[4 * b2 + 2], rpb[4 * b2 + 3]
            for dh, pY_, rY_ in ((0, pY0, rY0), (1, pY1, rY1)):
                for cc in range(2):
                    sc.op("pe", lambda e, cc=cc, b2=b2, b3=b3, dh=dh, pY_=pY_: e.matmul(
                        pY_[:], lhsT=aTC[b2][:, cc, :], rhs=wdC[b3][:, cc, dh * 512:(dh + 1) * 512],
                        start=(cc == 0), stop=(cc == 1)),
                        reads=[res("aTC%d" % b2), res("wdC%d" % b3)], writes=[rY_])
            sc.op("act", lambda e, b2=b2, pY0=pY0, i=i: e.activation(ysb[b2][:, 0:512], pY0[:], AF.Copy, scale=wA[:, i:i + 1]),
                  reads=[rY0, res("srowA")], writes=[res("ysb%d" % b2)])
            sc.op("dve", lambda e, b2=b2, pY1=pY1, i=i: e.tensor_scalar(ysb[b2][:, 512:1024], pY1[:], wA[:, i:i + 1], None, ALU.mult),
                  reads=[rY1, res("srowA")], writes=[res("ysb%d" % b2)])
            sc.dma("sp", "st_y", y_d[i * 128:(i + 1) * 128, :], ysb[b2][:], reads=[res("ysb%d" % b2)], writes=[res("y_d")])

        gathers(0)
        gathers(1)
        stage1(0)
        for i in range(NBLK):
            if i + 2 < NBLK:
                gathers(i + 2)
            stage2(i)
            if i + 1 < NBLK:
                stage1(i + 1)
            stage3(i)

        sc.fence()
        xstL = [cvB(0, 1024).rearrange("p (a b) -> p a b", a=8), cvB(2048, 1024).rearrange("p (a b) -> p a b", a=8)]
        otlL = [cvB(1024, 1024).rearrange("p (a b) -> p a b", a=8), cvB(3072, 1024).rearrange("p (a b) -> p a b", a=8)]

        def gathD(gt):
            b3 = gt % 3
            for k in range(8):
                sc.idma("g_y%d" % b3, yg[b3][:, k, :], None, y_d, dstu[:, gt, k:k + 1], reads=[res("dstu"), res("y_d")],
                        writes=[res("yg%d" % b3)])

        gathD(0)
        if NT > 1:
            gathD(1)
        for gt in range(NT):
            b2, b3 = gt % 2, gt % 3
            if gt + 2 < NT:
                gathD(gt + 2)
            s_, t_ = gt // 16, gt % 16
            tt = slice(t_ * 128, (t_ + 1) * 128)
            ryg = res("yg%d" % b3)
            acc_, racc = accD[b2], res("accD%d" % b2)
            xst_, rxst = xstL[b2], res("xst%d" % b2)
            otl_, rotl = otlL[b2], res("otl%d" % b2)
            if gt == 0:
                sc.dma("sp", "ld_xs%d" % b2, xst_[:, :, :], xs_d[s_][:, :, tt], reads=[res("xs_d")], writes=[rxst])
            if gt + 1 < NT:
                g1 = gt + 1
                s1, t1 = g1 // 16, g1 % 16
                sc.dma("sp", "ld_xs%d" % (g1 % 2), xstL[g1 % 2][:, :, :], xs_d[s1][:, :, t1 * 128:(t1 + 1) * 128],
                       reads=[res("xs_d")], writes=[res("xst%d" % (g1 % 2))])
            sc.op("dve", lambda e, b3=b3, acc_=acc_: e.tensor_tensor(acc_[:], yg[b3][:, 0, :], yg[b3][:, 1, :], ALU.add),
                  reads=[ryg], writes=[racc])
            for k in range(2, 8):
                sc.op("dve", lambda e, k=k, b3=b3, acc_=acc_: e.tensor_tensor(acc_[:], acc_[:], yg[b3][:, k, :], ALU.add),
                      reads=[ryg, racc], writes=[racc])
            for c in range(8):
                bi = 4 * b2 + c // 4
                sc.op("pe", lambda e, c=c, bi=bi, acc_=acc_: e.transpose(pb[bi][:, (c % 4) * 128:(c % 4 + 1) * 128],
                                                                          acc_[:, c * 128:(c + 1) * 128], idt[:]),
                      reads=[racc, res("idt")], writes=[rpb[bi]])
            for c in range(8):
                bi = 4 * b2 + c // 4
                sc.op("dve", lambda e, c=c, bi=bi, s_=s_, otl_=otl_, xst_=xst_: e.scalar_tensor_tensor(
                    otl_[:, c, :], pb[bi][:, (c % 4) * 128:(c % 4 + 1) * 128], modT[:, 40 + c, s_:s_ + 1], xst_[:, c, :],
                    ALU.mult, ALU.add),
                    reads=[rpb[bi], res("modT"), rxst], writes=[rotl])
            tok = sc.dma("sp", "st_out", out_d[s_][:, :, tt], otl_[:, :, :], reads=[rotl], writes=[res("out_d")])
        sc.wait("sp", tok)
        sc.emit()
    return nc


def _t5_bucket_np(rel):
    import jax
    import jax.numpy as jnp
    with jax.default_device(jax.devices("cpu")[0]):
        nb = NB // 2
        max_exact = nb // 2
        rel = jnp.asarray(np.asarray(rel, dtype=np.int32))
        n = jnp.abs(rel)
        large = max_exact + (jnp.log(jnp.maximum(n, 1).astype(jnp.float32) / max_exact)
                             / math.log(128 / max_exact) * (nb - max_exact)).astype(jnp.int32)
        large = jnp.minimum(large, nb - 1)
        return np.asarray(jnp.where(rel > 0, nb, 0) + jnp.where(n < max_exact, n, large))


def _chunk_w(w, ncol_chunk=128):
    K, N = w.shape
    return np.ascontiguousarray(w.reshape(K // 128, 128, N // ncol_chunk, ncol_chunk).transpose(2, 1, 0, 3))


def _prep_shared(inp):
    f = np.float32
    g = {}
    g["w_ada"] = _chunk_w(inp["w_ada"][0])
    g["b_adaT"] = np.ascontiguousarray(inp["b_ada"][0].reshape(48, 128).T)
    g["n1g"] = np.ascontiguousarray(inp["norm1_g"][0].reshape(8, 128).T)
    g["n2g"] = np.ascontiguousarray(inp["norm2_g"][0].reshape(8, 128).T)
    g["w_in"] = _chunk_w(inp["w_in"][0])
    g["qkg"] = np.ascontiguousarray(np.stack([np.tile(inp["q_norm_g"][0], 2), np.tile(inp["k_norm_g"][0], 2)], axis=1))
    lam = np.stack([inp["lambda_q1"][0], inp["lambda_k1"][0], inp["lambda_q2"][0], inp["lambda_k2"][0]], axis=0)
    g["lam_in"] = np.ascontiguousarray(np.broadcast_to(lam[None], (128, 4, 64)))
    g["subln_g"] = np.ascontiguousarray(np.broadcast_to(inp["subln_g"][0][None], (128, 128)))
    pw = inp["pool_w"][0]
    g["pool_w"] = np.ascontiguousarray(pw.reshape(4, 2, 128, 2, 128).transpose(0, 3, 2, 1, 4))
    g["pool_scaleT"] = np.ascontiguousarray(inp["pool_scale"][0].reshape(8, 128).T)
    rc = np.zeros((4, 16), f)
    for gi, w in enumerate((2, 4, 8, 16)):
        for k, pos in enumerate(list(range(8)) + list(range(S - 8, S))):
            lo = min(max(pos - w // 2, 0), S - 1)
            hi = min(max(pos + w // 2 - 1, 0), S - 1)
            rc[gi, k] = 1.0 / float(hi - lo + 1)
    g["pool_rc"] = np.ascontiguousarray(np.broadcast_to(rc[None], (128, 4, 16)))
    g["w_out"] = _chunk_w(inp["w_out"][0])
    g["w_router"] = np.ascontiguousarray(inp["w_router"][0].reshape(8, 128, NE).transpose(1, 0, 2))
    g["b_router"] = np.ascontiguousarray(np.broadcast_to(inp["b_router"][0][None], (128, NE)))
    wg = np.concatenate([inp["w_exp_gate"][0], inp["w_sh_gate"]], axis=0)
    wu = np.concatenate([inp["w_exp_up"][0], inp["w_sh_up"]], axis=0)
    wd = np.concatenate([inp["w_exp_down"][0], inp["w_sh_down"]], axis=0)
    g["w_eg"] = np.ascontiguousarray(wg.reshape(NE + 1, 8, 128, 256).transpose(0, 2, 1, 3))
    g["w_eu"] = np.ascontiguousarray(wu.reshape(NE + 1, 8, 128, 256).transpose(0, 2, 1, 3))
    g["w_ed"] = np.ascontiguousarray(wd.reshape(NE + 1, 2, 128, 1024).transpose(0, 2, 1, 3))
    g["rel_tab"] = np.ascontiguousarray(inp["rel_bias_table"])
    jj = np.arange(FVW)
    bk = _t5_bucket_np(767 - jj)
    oh = np.zeros((NB, FVW), f)
    oh[bk, jj] = 1.0
    g["bias_oh"] = oh
    g["ident"] = np.eye(128, dtype=f)
    g["antiid"] = np.ascontiguousarray(np.eye(128, dtype=f)[::-1])
    bo = np.zeros((128, 128), f)
    bo[:64, :64] = 1.0
    bo[64:, 64:] = 1.0
    g["blockones"] = bo
    ar = np.arange(128)
    g["lstrict"] = (ar[:, None] < ar[None, :]).astype(f)
    ee = np.arange(NE)
    g["ustrict"] = np.stack([((c * 128 + ar)[:, None] < ee[None, :]).astype(f) for c in range(2)])
    g["uincl"] = np.stack([((c * 128 + ar)[:, None] <= ee[None, :]).astype(f) for c in range(2)])
    g["iota_row"] = np.ascontiguousarray(np.broadcast_to(np.arange(1024, dtype=f)[None], (128, 1024)))
    g["piota8"] = np.ascontiguousarray(np.broadcast_to(ar.astype(f)[:, None], (128, 8)))
    return {k: np.asarray(v, dtype=f) for k, v in g.items()}


def _core_inputs(shared, x, c, b0, nseq):
    m = dict(shared)
    xs = x[b0:b0 + nseq]
    m["xT"] = np.ascontiguousarray(xs.reshape(nseq, S, 8, 128).transpose(0, 3, 2, 1))
    m["cT"] = np.ascontiguousarray(c[b0:b0 + nseq].reshape(nseq, 8, 128).transpose(2, 1, 0))
    return m


def _unpack(outT):
    return np.ascontiguousarray(outT.transpose(0, 3, 2, 1).reshape(outT.shape[0], S, D))


def kernel(**inputs):
    inp = {k: np.asarray(v, dtype=np.float32) for k, v in inputs.items()}
    ncores = 8
    nseq = inp["x"].shape[0] // ncores
    shared = _prep_shared(inp)
    nc = build(nseq)
    in_maps = [_core_inputs(shared, inp["x"], inp["c"], i * nseq, nseq) for i in range(ncores)]
    res = run_bass_kernel_spmd(nc, in_maps, core_ids=list(range(ncores)))
    return np.concatenate([_unpack(r["outT"]) for r in res.results], axis=0)
```

```python
import math
import numpy as np
from contextlib import ExitStack
import concourse.bass as bass
import concourse.mybir as mybir
from concourse.bass_utils import run_bass_kernel_spmd

F32 = mybir.dt.float32
BF16 = mybir.dt.bfloat16
AF = mybir.ActivationFunctionType
ALU = mybir.AluOpType
AX = mybir.AxisListType

D = 1024
S = 2048
NB = 32
NH = 8
NE = 256
EPS = 1e-6
LAM_INIT = 0.8 - 0.6 * math.exp(-0.3 * 0)
PADL = 16
LP = S + 2 * PADL
MW = 1280
FVW = MW + 127


class Res:
    __slots__ = ("w", "r")

    def __init__(self):
        self.w = None
        self.r = {}


class Sched:
    ENG = ("pe", "dve", "act", "pool", "sp")

    def __init__(self, nc, es):
        self.nc = nc
        self.es = es
        self.sem = {k: es.enter_context(nc.semaphore("s_" + k)) for k in self.ENG}
        self.cnt = {k: 0 for k in self.ENG}
        self.seen = {k: {} for k in self.ENG}
        self.prog = {k: [] for k in self.ENG}
        self.dsem = {}
        self.dcnt = {}

    def _wait(self, e, tok):
        if tok is None:
            return
        key, val = tok
        if key == e and e == "pe":
            return
        if self.seen[e].get(key, 0) >= val:
            return
        sem = self.sem[key] if key in self.sem else self.dsem[key]
        self.prog[e].append(("w", sem, val))
        self.seen[e][key] = val

    def _deps(self, e, reads, writes):
        for r in reads:
            self._wait(e, r.w)
        for w in writes:
            self._wait(e, w.w)
            for k, v in w.r.items():
                self._wait(e, (k, v))

    def _commit(self, tok, reads, writes):
        for r in reads:
            if r.r.get(tok[0], 0) < tok[1]:
                r.r[tok[0]] = tok[1]
        for w in writes:
            w.w = tok
            w.r = {}

    def op(self, e, fn, reads=(), writes=()):
        self._deps(e, reads, writes)
        self.cnt[e] += 1
        self.prog[e].append(("i", fn, self.sem[e], 1))
        tok = (e, self.cnt[e])
        self._commit(tok, reads, writes)
        return tok

    def dma(self, e, chan, out, in_, reads=(), writes=()):
        if chan not in self.dsem:
            self.dsem[chan] = self.es.enter_context(self.nc.semaphore("dm_" + chan))
            self.dcnt[chan] = 0
        self._deps(e, reads, writes)
        self.dcnt[chan] += 16
        self.prog[e].append(("i", lambda eng: eng.dma_start(out=out, in_=in_), self.dsem[chan], 16))
        tok = (chan, self.dcnt[chan])
        self._commit(tok, reads, writes)
        return tok

    def idma(self, chan, out, out_off, in_, in_off, reads=(), writes=(), bound=None):
        e = "pool"
        if chan not in self.dsem:
            self.dsem[chan] = self.es.enter_context(self.nc.semaphore("dm_" + chan))
            self.dcnt[chan] = 0
        self._deps(e, reads, writes)
        self.dcnt[chan] += 16
        oo = None if out_off is None else bass.IndirectOffsetOnAxis(ap=out_off, axis=0)
        io = None if in_off is None else bass.IndirectOffsetOnAxis(ap=in_off, axis=0)
        if bound is None:
            self.prog[e].append(("i", lambda eng: eng.indirect_dma_start(out=out, out_offset=oo, in_=in_, in_offset=io),
                                 self.dsem[chan], 16))
        else:
            self.bound_val = bound
            self.prog[e].append(("i", lambda eng: eng.indirect_dma_start(out=out, out_offset=oo, in_=in_, in_offset=io,
                                                                         bounds_check=self.bound_reg, oob_is_err=False),
                                 self.dsem[chan], 16))
        tok = (chan, self.dcnt[chan])
        self._commit(tok, reads, writes)
        return tok

    def wait(self, e, tok):
        self._wait(e, tok)

    def fence(self):
        toks = [(k, self.cnt[k]) for k in self.ENG if self.cnt[k] > 0]
        toks += [(k, v) for k, v in self.dcnt.items() if v > 0]
        for e in self.ENG:
            for t in toks:
                if t[0] == e:
                    continue
                self._wait(e, t)

    def emit(self):
        nc = self.nc
        with nc.Block() as block:
            def mk(e):
                def f(eng):
                    if e == "pool" and getattr(self, "bound_val", None) is not None:
                        self.bound_reg = eng.alloc_register("oob_bound")
                        eng.reg_mov(self.bound_reg, int(self.bound_val))
                    for it in self.prog[e]:
                        if it[0] == "w":
                            eng.wait_ge(it[1], it[2])
                        else:
                            it[1](eng).then_inc(it[2], it[3])
                return f
            block.tensor(mk("pe"))
            block.vector(mk("dve"))
            block.scalar(mk("act"))
            block.gpsimd(mk("pool"))
            block.sync(mk("sp"))


def build(NSEQ, n_exp=NE, dbg=False):
    nc = bass.Bass("TRN2", target_bir_lowering=False)

    def din(name, shape):
        return nc.dram_tensor(name, list(shape), F32, kind="ExternalInput").ap()

    xT_d = din("xT", [NSEQ, 128, 8, S])
    cT_d = din("cT", [128, 8, NSEQ])
    wada_d = din("w_ada", [48, 128, 8, 128])
    bada_d = din("b_adaT", [128, 48])
    n1g_d = din("n1g", [128, 8])
    n2g_d = din("n2g", [128, 8])
    win_d = din("w_in", [48, 128, 8, 128])
    qkg_d = din("qkg", [128, 2])
    lam_d = din("lam_in", [128, 4, 64])
    sg_d = din("subln_g", [128, 128])
    pw_d = din("pool_w", [4, 2, 128, 2, 128])
    psc_d = din("pool_scaleT", [128, 8])
    rc_d = din("pool_rc", [128, 4, 16])
    wo_d = din("w_out", [8, 128, 8, 128])
    wr_d = din("w_router", [128, 8, NE])
    br_d = din("b_router", [128, NE])
    wg_d = din("w_eg", [NE + 1, 128, 8, 256])
    wu_d = din("w_eu", [NE + 1, 128, 8, 256])
    wd_d = din("w_ed", [NE + 1, 128, 2, 1024])
    tab_d = din("rel_tab", [NB, NH])
    oh_d = din("bias_oh", [NB, FVW])
    idt_d = din("ident", [128, 128])
    aid_d = din("antiid", [128, 128])
    bones_d = din("blockones", [128, 128])
    out_d = nc.dram_tensor("outT", [NSEQ, 128, 8, S], F32, kind="ExternalOutput").ap()
    fv_d = nc.dram_tensor("fv_scr", [NH, FVW], F32, kind="Internal").ap()
    mst_d = nc.dram_tensor("mst_scr", [NH, 128, MW], F32, kind="Internal").ap()
    T = NSEQ * S
    NT = T // 128
    NBLK = T * 8 // 128 + NE
    NSLOT = NBLK * 128
    ls_d = din("lstrict", [128, 128])
    us_d = din("ustrict", [2, 128, NE])
    ui_d = din("uincl", [2, 128, NE])
    iot_d = din("iota_row", [128, 1024])
    pio_d = din("piota8", [128, 8])
    U32 = mybir.dt.uint32
    I32 = mybir.dt.int32
    h2_d = nc.dram_tensor("h2_scr", [T, D], BF16, kind="Internal").ap()
    pos_d = nc.dram_tensor("pos_scr", [NT, 128, NE], F32, kind="Internal").ap()
    wt_d = nc.dram_tensor("wt_scr", [NT, 128, NE], F32, kind="Internal").ap()
    xs_d = nc.dram_tensor("xs_scr", [NSEQ, 128, 8, S], F32, kind="ExternalOutput").ap() if dbg else out_d
    slot_d = nc.dram_tensor("slot_scr", [NSLOT, 2], F32, kind="Internal").ap()
    y_d = nc.dram_tensor("y_scr", [NSLOT, D], BF16, kind="Internal").ap()
    wg2 = wg_d.rearrange("e p a b -> (e p) (a b)")
    wu2 = wu_d.rearrange("e p a b -> (e p) (a b)")
    wd2 = wd_d.rearrange("e p a b -> (e p) (a b)")

    with ExitStack() as es:
        es.enter_context(nc.allow_low_precision("bf16 matmul operands, fp32 accumulation"))
        es.enter_context(nc.allow_non_contiguous_dma("overlapping-window bias load"))
        sc = Sched(nc, es)

        def sb(name, shape, dt=F32):
            return es.enter_context(nc.sbuf_tensor(name, list(shape), dt))

        CW = 256
        arA = sb("arA", [128, 16384])
        arB = sb("arB", [128, 8320])

        def cvA(off, n, dt=F32):
            return arA[:, off:off + n] if dt == F32 else arA[:, off:off + n].bitcast(dt)

        def cvB(off, n, dt=F32):
            return arB[:, off:off + n] if dt == F32 else arB[:, off:off + n].bitcast(dt)

        bigA = arA[:, :].rearrange("p (a b) -> p a b", a=8)
        mT = cvA(0, 8192, BF16).rearrange("p (a b) -> p a b", a=8)
        qz = [cvA(8192, 1024, BF16), cvB(0, 1024, BF16)]
        kT = cvA(9216, 1024, BF16)
        vA = cvA(10240, 1040, BF16).rearrange("p (a b) -> p a b", a=16)
        gaT = cvA(11280, 1024, BF16)
        mst = cvA(12304, 1280)
        O = [cvA(13584, 520).rearrange("p (a b) -> p a b", a=4), cvA(14104, 520).rearrange("p (a b) -> p a b", a=4)]
        sadd = [cvA(14624, 512), cvA(15136, 512)]
        hank = cvA(0, 1280)
        oh = arA[0:NB, 1280:1280 + FVW]
        fvs = arA[0:NH, 2688:2688 + FVW]
        wadaS = [cvA(4096, 1024).rearrange("p (a b) -> p a b", a=8), cvA(5120, 1024).rearrange("p (a b) -> p a b", a=8)]
        lam_in = cvA(6144, 256).rearrange("p (a b) -> p a b", a=4)
        lamt = cvA(6400, 256).rearrange("p (a b) -> p a b", a=4)
        sil = cvB(0, 1024).rearrange("p (a b) -> p a b", a=2)
        aT = cvB(1024, 512, BF16).rearrange("p (a b) -> p a b", a=2)
        scr = cvB(1536, 256)
        bia = cvB(1792, 256)
        msk = cvB(2048, 256)
        Wtok = [cvB(2304, 256), cvB(2560, 256)]
        selT2 = [cvB(2816, 256), cvB(4736, 256)]
        posS = [cvB(3072, 256), cvB(3328, 256)]
        m8 = cvB(3584, 64).rearrange("p (a b) -> p a b", a=8)
        gsc = cvB(3648, 8)
        gm8 = cvB(3656, 8)
        gmk = cvB(3664, 8)
        t8 = cvB(3672, 8)
        den = cvB(3680, 2)
        h2tm = [cvB(3712, 512, BF16), cvB(4224, 512, BF16)]
        pP = cvB(0, LP)
        pW = [cvB(2080, LP), cvB(4160, LP)]
        mixT = cvB(6240, 2048, BF16).rearrange("p (a b) -> p a b", a=2)
        nbf = cvB(0, 256)
        nbi = cvB(256, 256, I32)
        sbase = cvB(512, 256)
        nbT = [cvB(768, 128), cvB(896, 128)]
        bend = cvB(1024, 2)
        Cm = [cvB(1032, NBLK), cvB(1032 + NBLK, NBLK)]
        bef = cvB(1032 + 2 * NBLK, NBLK)
        o_b = 1032 + 3 * NBLK
        NPB = 4
        NRB = 8
        posL = [cvB(o_b + 256 * i, 256) for i in range(NPB)]
        wtL = [cvB(o_b + 256 * NPB + 256 * i, 256) for i in range(NPB)]
        o_c = o_b + 512 * NPB
        dp1 = cvB(o_c, 256)
        keyb = cvB(o_c + 256, 256)
        junkb = cvB(o_c + 512, 256)
        d8 = cvB(o_c + 768, 8)
        rows = [cvB(o_c + 776 + 16 * i, 16).rearrange("p (a b) -> p a b", a=8) for i in range(NRB)]
        zslot = cvB(o_c + 776 + 16 * NRB, NSLOT * 2 // 128)
        assert o_c + 776 + 16 * NRB + NSLOT * 2 // 128 <= 8320
        wgC = [cvA(3072 * i, 1024, BF16) for i in range(3)]
        wuC = [cvA(3072 * i + 1024, 1024, BF16) for i in range(3)]
        wdC = [cvA(3072 * i + 2048, 1024, BF16).rearrange("p (a b) -> p a b", a=2) for i in range(3)]
        xgC = [cvA(9216 + 512 * i, 512, BF16) for i in range(3)]
        xgT = [cvA(10752 + 512 * i, 512, BF16).rearrange("p (a b) -> p a b", a=8) for i in range(2)]
        silC = [cvA(11776, 256), cvA(12032, 256)]
        aTC = [cvA(12288, 128, BF16).rearrange("p (a b) -> p a b", a=2), cvA(12416, 128, BF16).rearrange("p (a b) -> p a b", a=2)]
        ysb = [cvA(12544, 512, BF16), cvA(13056, 512, BF16)]
        wA = cvA(13568, NBLK)
        tokuA = cvA(13568 + NBLK, NBLK, U32)
        sl6 = [cvA(13568 + 2 * NBLK, 256), cvA(13568 + 2 * NBLK + 256, 256)]
        sl6c = [cvA(13568 + 2 * NBLK + 512, 128), cvA(13568 + 2 * NBLK + 640, 128)]
        assert 13568 + 2 * NBLK + 768 <= 16384 and NBLK % 128 == 0
        NYG = 4
        yg = [cvA(4096 * i, 4096, BF16).rearrange("p (a b) -> p a b", a=8) for i in range(NYG)]
        accD = [cvB(4096, 1024), cvB(5120, 1024)]

        hT = sb("hT", [128, 8, S], BF16)
        tmpF = sb("tmpF", [128, 8, CW])
        xch = sb("xch", [128, 8, CW])
        sqc = [sb("sqc%d" % i, [128, CW]) for i in range(2)]
        rstd = sb("rstd", [128, CW])
        modT = sb("modT", [128, 48, NSEQ])
        bada = sb("bada", [128, 48])
        n1g = sb("n1g_s", [128, 8])
        n2g = sb("n2g_s", [128, 8])
        A1 = sb("A1", [128, 8])
        A2 = sb("A2", [128, 8])
        cT = sb("cT_s", [128, 8, NSEQ])
        scT = sb("scT", [128, 8, NSEQ])
        winS = [sb("win%d" % i, [128, 8, 128], BF16) for i in range(2)]
        qkg = sb("qkg_s", [128, 2])
        lamv = sb("lamv", [128, 4])
        nlam = sb("nlam", [128, 1])
        sg = sb("sg_s", [128, 128])
        pwS = sb("pw_s", [128, 2, 2, 128], BF16)
        psc = sb("psc_s", [128, 8])
        rc = sb("rc_s", [128, 4, 16])
        woS = [sb("wo%d" % i, [128, 8, 128], BF16) for i in range(2)]
        wr = sb("wr_s", [128, 8, NE])
        wsg = sb("wsg", [128, 8, 256], BF16)
        wsu = sb("wsu", [128, 8, 256], BF16)
        wsd = sb("wsd", [128, 2, 1024], BF16)
        base = sb("base", [128, NE])
        lsm = sb("lsm", [128, 128])
        idtb = sb("idtb", [128, 128], BF16)
        pio8 = sb("pio8", [128, 8])
        dstu = sb("dstu", [128, NT, 8], U32)
        idxW = sb("idxW", [128, NBLK], U32)
        brt = sb("br_s", [128, NE])
        tab = sb("tab_s", [NB, NH])
        idt = sb("idt", [128, 128])
        aid = sb("aid", [128, 128])
        bones = sb("bones", [128, 128])
        ones = sb("ones", [128, 128])
        sqh2 = [sb("sqh%d" % i, [128, 512]) for i in range(2)]
        rsh2 = [sb("rsh%d" % i, [128, 512]) for i in range(2)]
        ET = [sb("ET%d" % i, [128, 512], BF16) for i in range(3)]
        rr = sb("rr", [128, 8])
        att4 = sb("att4", [128, 4, 128])
        att4b = sb("att4b", [128, 4, 128])
        ssq4 = sb("ssq4", [128, 8])
        epsc = sb("epsc", [128, 2])
        gpT = sb("gpT", [128, 512])
        ptmp = sb("ptmp", [128, 512])

        pb = [es.enter_context(nc.psum_tensor("pb%d" % i, [128, 512], F32)) for i in range(8)]
        rpb = [Res() for _ in range(8)]

        R = {}

        def res(name):
            if name not in R:
                R[name] = Res()
            return R[name]

        rbigA = [Res() for _ in range(4)]
        rhT = [Res() for _ in range(8)]
        rmT = [[Res() for _ in range(4)] for _ in range(8)]

        def load(dst, src, name, eng="sp"):
            sc.dma(eng, "ld_" + name, dst, src, writes=[res(name)])

        load(bada[:], bada_d, "bada")
        load(n1g[:], n1g_d, "n1g")
        load(n2g[:], n2g_d, "n2g")
        load(cT[:], cT_d, "cT")
        load(qkg[:], qkg_d, "qkg")
        load(lam_in[:], lam_d, "lam_in")
        load(sg[:], sg_d, "sg")
        load(psc[:], psc_d, "psc")
        load(rc[:], rc_d, "rc")
        load(wr[:], wr_d, "wr")
        load(brt[:], br_d, "brt")
        load(tab[:], tab_d, "tab")
        load(oh[:], oh_d, "oh")
        load(idt[:], idt_d, "idt")
        load(aid[:], aid_d, "aid")
        load(bones[:], bones_d, "bones")
        load(lsm[:], ls_d, "lsm")
        load(pio8[:], pio_d, "pio8")
        sc.dma("pool", "ld_wsg", wsg[:], wg_d[NE], writes=[res("wsg")])
        sc.dma("pool", "ld_wsu", wsu[:], wu_d[NE], writes=[res("wsu")])
        sc.dma("pool", "ld_wsd", wsd[:], wd_d[NE], writes=[res("wsd")])
        sc.op("dve", lambda e: e.memset(base[:], 0.0), writes=[res("base")])
        sc.op("dve", lambda e: e.tensor_copy(out=idtb[:], in_=idt[:]), reads=[res("idt")], writes=[res("idtb")])
        sc.op("dve", lambda e: e.memset(ones[:], 1.0), writes=[res("ones")])
        sc.op("dve", lambda e: e.memset(epsc[:, 0:1], float(EPS)), writes=[res("epsc")])
        sc.op("dve", lambda e: e.memset(epsc[:, 1:2], float(64 * EPS)), reads=[res("epsc")], writes=[res("epsc")])
        sc.op("dve", lambda e: e.tensor_scalar(sg[:], sg[:], float(1.0 - LAM_INIT), None, ALU.mult),
              reads=[res("sg")], writes=[res("sg")])
        sc.op("dve", lambda e: e.tensor_tensor(lamt[:, 0, :], lam_in[:, 0, :], lam_in[:, 1, :], ALU.mult),
              reads=[res("lam_in")], writes=[res("lamt")])
        sc.op("dve", lambda e: e.tensor_tensor(lamt[:, 1, :], lam_in[:, 2, :], lam_in[:, 3, :], ALU.mult),
              reads=[res("lam_in"), res("lamt")], writes=[res("lamt")])
        sc.op("dve", lambda e: e.tensor_reduce(lamv[:, 0:2], lamt[:, 0:2, :], AX.X, ALU.add),
              reads=[res("lamt")], writes=[res("lamv")])
        sc.op("act", lambda e: e.activation(lamv[:, 2:4], lamv[:, 0:2], AF.Exp),
              reads=[res("lamv")], writes=[res("lamv")])
        sc.op("dve", lambda e: e.scalar_tensor_tensor(nlam[:], lamv[:, 3:4], float(-LAM_INIT), lamv[:, 2:3],
                                                      ALU.add, ALU.subtract),
              reads=[res("lamv")], writes=[res("nlam")])

        sc.op("act", lambda e: e.activation(scT[:], cT[:], AF.Silu), reads=[res("cT")], writes=[res("scT")])
        for fc in range(48):
            wb = wadaS[fc % 2]
            rw = res("wada%d" % (fc % 2))
            sc.dma("sp", "ld_wada%d" % (fc % 2), wb[:], wada_d[fc], writes=[rw])
            for kc in range(8):
                sc.op("pe", lambda e, wb=wb, kc=kc: e.matmul(pb[7][:, 0:NSEQ], lhsT=wb[:, kc, :], rhs=scT[:, kc, :],
                                                              start=(kc == 0), stop=(kc == 7)),
                      reads=[rw, res("scT")], writes=[rpb[7]])
            sc.op("dve", lambda e, fc=fc: e.tensor_scalar(modT[:, fc, :], pb[7][:, 0:NSEQ], bada[:, fc:fc + 1], None,
                                                           ALU.add),
                  reads=[rpb[7], res("bada")], writes=[res("modT")])

        for c0 in range(0, FVW, 512):
            cw = min(512, FVW - c0)
            sc.op("pe", lambda e, c0=c0, cw=cw: e.matmul(pb[7][0:NH, 0:cw], lhsT=tab[:, :], rhs=oh[:, c0:c0 + cw],
                                                          start=True, stop=True),
                  reads=[res("tab"), res("oh")], writes=[rpb[7]])
            sc.op("dve", lambda e, c0=c0, cw=cw: e.tensor_copy(out=fvs[:, c0:c0 + cw], in_=pb[7][0:NH, 0:cw]),
                  reads=[rpb[7]], writes=[res("fvs")])
        sc.dma("sp", "st_fv", fv_d, fvs[:], reads=[res("fvs")], writes=[res("fv_d")])
        for h in range(NH):
            src = bass.AP(fv_d.tensor, h * FVW, [[1, 128], [1, MW]])
            sc.dma("sp", "ld_hank", hank[:], src, reads=[res("fv_d")], writes=[res("hank")])
            for c0 in range(0, MW, 512):
                cw = min(512, MW - c0)
                sc.op("pe", lambda e, c0=c0, cw=cw: e.matmul(pb[7][:, 0:cw], lhsT=aid[:], rhs=hank[:, c0:c0 + cw],
                                                              start=True, stop=True),
                      reads=[res("aid"), res("hank")], writes=[rpb[7]])
                sc.op("dve", lambda e, c0=c0, cw=cw: e.tensor_copy(out=mst[:, c0:c0 + cw], in_=pb[7][:, 0:cw]),
                      reads=[rpb[7]], writes=[res("mst")])
            sc.dma("sp", "st_mst", mst_d[h], mst[:], reads=[res("mst")], writes=[res("mst_d")])

        win_ctr = [0]

        def load_win(chunk):
            i = win_ctr[0] % 2
            win_ctr[0] += 1
            sc.dma("pool", "ld_win%d" % i, winS[i][:], win_d[chunk], writes=[res("win%d" % i)])
            return winS[i], res("win%d" % i)

        def rmsnorm_chunk(Acol, shcol0, s, ts, rdst, f32_out):
            for c in range(8):
                q = sqc[c % 2]
                rq = res("sqc%d" % (c % 2))
                sc.op("act", lambda e, c=c, q=q: e.activation(q[:], xch[:, c, :], AF.Square),
                      reads=[res("xch")], writes=[rq])
                sc.op("pe", lambda e, c=c, q=q: e.matmul(pb[7][:, 0:CW], lhsT=ones[:], rhs=q[:],
                                                         start=(c == 0), stop=(c == 7)),
                      reads=[res("ones"), rq], writes=[rpb[7]])
            sc.op("act", lambda e: e.activation(rstd[:], pb[7][:, 0:CW], AF.Sqrt, bias=float(EPS), scale=1.0 / D),
                  reads=[rpb[7]], writes=[res("rstd")])
            sc.op("dve", lambda e: e.reciprocal(rstd[:], rstd[:]), reads=[res("rstd")], writes=[res("rstd")])
            for c in range(8):
                sc.op("dve", lambda e, c=c: e.scalar_tensor_tensor(
                    tmpF[:, c, :], xch[:, c, :], Acol[:, c:c + 1], rstd[:], ALU.mult, ALU.mult),
                    reads=[res("xch"), res("rstd"), res("Acol")], writes=[res("tmpF%d" % c)])
                if not f32_out:
                    sc.op("act", lambda e, c=c: e.activation(
                        hT[:, c, ts], tmpF[:, c, :], AF.Identity, bias=modT[:, shcol0 + c, s:s + 1], scale=1.0),
                        reads=[res("tmpF%d" % c), res("modT")], writes=[rdst])
                else:
                    sc.op("act", lambda e, c=c: e.activation(
                        tmpF[:, c, :], tmpF[:, c, :], AF.Identity, bias=modT[:, shcol0 + c, s:s + 1], scale=1.0),
                        reads=[res("tmpF%d" % c), res("modT")], writes=[res("tmpF%d" % c)])
                    sc.op("dve", lambda e, c=c: e.tensor_copy(out=hT[:, c, ts], in_=tmpF[:, c, :]),
                          reads=[res("tmpF%d" % c)], writes=[rdst])

        rtmpF = [res("tmpF%d" % c) for c in range(8)]

        for s in range(NSEQ):
            sc.fence()
            sc.op("dve", lambda e: e.memset(vA[:], 1.0), writes=[res("vA")])
            sc.op("dve", lambda e: e.memset(qz[0][64:128, :], 0.0), writes=[res("qT")])
            sc.op("dve", lambda e: e.memset(qz[1][0:64, :], 0.0), writes=[res("qT")])
            sc.op("dve", lambda e, s=s: e.scalar_tensor_tensor(A1[:], modT[:, 8:16, s], 1.0, n1g[:], ALU.add, ALU.mult),
                  reads=[res("modT"), res("n1g")], writes=[res("Acol")])
            for j in range(8):
                ts = slice(j * CW, (j + 1) * CW)
                sc.dma("sp", "ld_x", xch[:], xT_d[s][:, :, ts], writes=[res("xch")])
                rmsnorm_chunk(A1, 0, s, ts, rhT[j], False)

            for h in range(NH):
                sc.dma("sp", "ld_mst", mst[:], mst_d[h], reads=[res("mst_d")], writes=[res("mst")])
                for which, dstT, rname in ((0, None, "qT"), (1, kT, "kT")):
                    wb, rw = load_win(which * 8 + h)
                    for j in range(4):
                        ts = slice(j * 512, (j + 1) * 512)
                        pa = 4 + 2 * (j % 2)
                        pn = pa + 1
                        sq_, rs_ = sqh2[j % 2], rsh2[j % 2]
                        rsq, rrs = res("sqh%d" % (j % 2)), res("rsh%d" % (j % 2))
                        for kc in range(8):
                            sc.op("pe", lambda e, wb=wb, kc=kc, ts=ts, pa=pa: e.matmul(
                                pb[pa][:], lhsT=wb[:, kc, :], rhs=hT[:, kc, ts], start=(kc == 0), stop=(kc == 7)),
                                reads=[rw, rhT[2 * j], rhT[2 * j + 1]], writes=[rpb[pa]])
                        sc.op("act", lambda e, pa=pa, sq_=sq_: e.activation(sq_[:], pb[pa][:], AF.Square),
                              reads=[rpb[pa]], writes=[rsq])
                        sc.op("pe", lambda e, pn=pn, sq_=sq_: e.matmul(pb[pn][:], lhsT=bones[:], rhs=sq_[:], start=True, stop=True),
                              reads=[res("bones"), rsq], writes=[rpb[pn]])
                        if which == 0:
                            sc.op("act", lambda e, pn=pn, rs_=rs_: e.activation(rs_[:], pb[pn][:], AF.Ln, bias=epsc[:, 1:2], scale=1.0),
                                  reads=[rpb[pn], res("epsc")], writes=[rrs])
                        else:
                            sc.op("act", lambda e, pn=pn, rs_=rs_: e.activation(rs_[:], pb[pn][:], AF.Ln, bias=epsc[:, 0:1], scale=1.0 / 64),
                                  reads=[rpb[pn], res("epsc")], writes=[rrs])
                        sc.op("act", lambda e, rs_=rs_: e.activation(rs_[:], rs_[:], AF.Exp, scale=-0.5), reads=[rrs], writes=[rrs])
                        if which == 1:
                            sc.op("dve", lambda e, dstT=dstT, ts=ts, which=which, pa=pa, rs_=rs_: e.scalar_tensor_tensor(
                                dstT[:, ts], pb[pa][:], qkg[:, which:which + 1], rs_[:], ALU.mult, ALU.mult),
                                reads=[rpb[pa], rrs, res("qkg")], writes=[res(rname)])
                        else:
                            for comp in range(2):
                                pr = slice(comp * 64, (comp + 1) * 64)
                                sc.op("dve", lambda e, ts=ts, pa=pa, rs_=rs_, comp=comp, pr=pr: e.scalar_tensor_tensor(
                                    qz[comp][pr, ts], pb[pa][pr, :], qkg[pr, 0:1], rs_[pr, :], ALU.mult, ALU.mult),
                                    reads=[rpb[pa], rrs, res("qkg")], writes=[res(rname)])
                wb, rw = load_win(16 + h)
                for t in range(16):
                    tt = slice(t * 128, (t + 1) * 128)
                    vb = 4 + (t % 4)
                    for kc in range(8):
                        sc.op("pe", lambda e, wb=wb, kc=kc, tt=tt, vb=vb: e.matmul(
                            pb[vb][:, 0:128], lhsT=hT[:, kc, tt], rhs=wb[:, kc, :], start=(kc == 0), stop=(kc == 7)),
                            reads=[rw, rhT[t // 2]], writes=[rpb[vb]])
                    if t % 2 == 0:
                        sc.op("act", lambda e, t=t, vb=vb: e.activation(vA[:, t, 0:128], pb[vb][:, 0:128], AF.Copy),
                              reads=[rpb[vb]], writes=[res("vA")])
                    else:
                        sc.op("dve", lambda e, t=t, vb=vb: e.tensor_copy(out=vA[:, t, 0:128], in_=pb[vb][:, 0:128]),
                              reads=[rpb[vb]], writes=[res("vA")])
                wb, rw = load_win(32 + h)
                for j in range(4):
                    ts = slice(j * 512, (j + 1) * 512)
                    gb = 4 + j
                    for kc in range(8):
                        sc.op("pe", lambda e, wb=wb, kc=kc, ts=ts, gb=gb: e.matmul(
                            pb[gb][:], lhsT=wb[:, kc, :], rhs=hT[:, kc, ts], start=(kc == 0), stop=(kc == 7)),
                            reads=[rw, rhT[2 * j], rhT[2 * j + 1]], writes=[rpb[gb]])
                    sc.op("act", lambda e, ts=ts, gb=gb: e.activation(gaT[:, ts], pb[gb][:], AF.Sigmoid),
                          reads=[rpb[gb]], writes=[res("gaT")])
                tiles = [(j, comp, kb) for j in range(4) for comp in range(2) for kb in range(16)]

                def emit_st(n):
                    j, comp, kb = tiles[n]
                    bi = 4 + (n % 3)
                    ks = slice(kb * 128, (kb + 1) * 128)
                    qs = slice(j * 512, (j + 1) * 512)
                    sc.op("pe", lambda e, bi=bi, comp=comp, ks=ks, qs=qs: e.matmul(
                        pb[bi][:], lhsT=kT[:, ks], rhs=qz[comp][:, qs], start=True, stop=True),
                        reads=[res("qT"), res("kT")], writes=[rpb[bi]])

                emit_st(0)
                emit_st(1)
                for n, (j, comp, kb) in enumerate(tiles):
                    if n + 2 < len(tiles):
                        emit_st(n + 2)
                    bi = 4 + (n % 3)
                    ei = n % 3
                    o = kb - 4 * j
                    rET = res("ET%d" % ei)
                    if o <= -2 or o >= 5:
                        col = (MW - 1) if o <= -2 else 0
                        sc.op("act", lambda e, bi=bi, ei=ei, col=col: e.activation(
                            ET[ei][:], pb[bi][:], AF.Exp, bias=mst[:, col:col + 1], scale=1.0),
                            reads=[rpb[bi], res("mst")], writes=[rET])
                    else:
                        m0 = 640 - 128 * o
                        si = n % 2
                        rsa = res("sadd%d" % si)
                        sc.op("dve", lambda e, bi=bi, si=si, m0=m0: e.tensor_tensor(
                            sadd[si][:], pb[bi][:], mst[:, m0:m0 + 512], ALU.add),
                            reads=[rpb[bi], res("mst")], writes=[rsa])
                        sc.op("act", lambda e, ei=ei, si=si: e.activation(ET[ei][:], sadd[si][:], AF.Exp),
                              reads=[rsa], writes=[rET])
                    for sub in range(4):
                        sc.op("pe", lambda e, sub=sub, ei=ei, kb=kb: e.matmul(
                            pb[sub][:, 0:129], lhsT=ET[ei][:, sub * 128:(sub + 1) * 128], rhs=vA[:, kb, 0:129],
                            start=(kb == 0), stop=(kb == 15)),
                            reads=[rET, res("vA")], writes=[rpb[sub]])
                    if kb == 15:
                        for sub in range(4):
                            eng = "act" if sub % 2 == 0 else "dve"
                            if eng == "act":
                                sc.op("act", lambda e, sub=sub, comp=comp: e.activation(
                                    O[comp][:, sub, 0:129], pb[sub][:, 0:129], AF.Copy),
                                    reads=[rpb[sub]], writes=[res("O%d" % comp)])
                            else:
                                sc.op("dve", lambda e, sub=sub, comp=comp: e.tensor_copy(
                                    out=O[comp][:, sub, 0:129], in_=pb[sub][:, 0:129]),
                                    reads=[rpb[sub]], writes=[res("O%d" % comp)])
                    if kb == 15 and comp == 1:
                        ts = slice(j * 512, (j + 1) * 512)
                        sc.op("dve", lambda e: e.reciprocal(rr[:, 0:4], O[0][:, :, 128]),
                              reads=[res("O0")], writes=[res("rr")])
                        sc.op("dve", lambda e: e.reciprocal(rr[:, 4:8], O[1][:, :, 128]),
                              reads=[res("O1"), res("rr")], writes=[res("rr")])
                        sc.op("dve", lambda e: e.tensor_scalar(rr[:, 4:8], rr[:, 4:8], nlam[:, 0:1], None, ALU.mult),
                              reads=[res("rr"), res("nlam")], writes=[res("rr")])
                        sc.op("dve", lambda e: e.tensor_tensor(
                            att4[:, :, :], O[0][:, :, 0:128], rr[:, 0:4].unsqueeze(2).to_broadcast([128, 4, 128]), ALU.mult),
                            reads=[res("O0"), res("rr")], writes=[res("att4")])
                        sc.op("dve", lambda e: e.tensor_tensor(
                            att4b[:, :, :], O[1][:, :, 0:128], rr[:, 4:8].unsqueeze(2).to_broadcast([128, 4, 128]), ALU.mult),
                            reads=[res("O1"), res("rr")], writes=[res("att4b")])
                        sc.op("dve", lambda e: e.tensor_tensor(att4[:, :, :], att4[:, :, :], att4b[:, :, :], ALU.add),
                              reads=[res("att4"), res("att4b")], writes=[res("att4")])
                        sc.op("dve", lambda e: e.tensor_tensor(att4b[:, :, :], att4[:, :, :], att4[:, :, :], ALU.mult),
                              reads=[res("att4")], writes=[res("att4b")])
                        sc.op("dve", lambda e: e.tensor_reduce(ssq4[:, 0:4], att4b[:, :, :], AX.X, ALU.add),
                              reads=[res("att4b")], writes=[res("ssq4")])
                        sc.op("act", lambda e: e.activation(ssq4[:, 4:8], ssq4[:, 0:4], AF.Ln, bias=epsc[:, 0:1], scale=1.0 / 128),
                              reads=[res("ssq4"), res("epsc")], writes=[res("ssq4")])
                        sc.op("act", lambda e: e.activation(ssq4[:, 4:8], ssq4[:, 4:8], AF.Exp, scale=-0.5),
                              reads=[res("ssq4")], writes=[res("ssq4")])
                        sc.op("dve", lambda e: e.tensor_tensor(
                            att4[:, :, :], att4[:, :, :], ssq4[:, 4:8].unsqueeze(2).to_broadcast([128, 4, 128]), ALU.mult),
                            reads=[res("att4"), res("ssq4")], writes=[res("att4")])
                        sc.op("dve", lambda e: e.tensor_tensor(
                            att4[:, :, :], att4[:, :, :], sg[:, :].unsqueeze(1).to_broadcast([128, 4, 128]), ALU.mult),
                            reads=[res("att4"), res("sg")], writes=[res("att4")])
                        for sub in range(4):
                            sc.op("pe", lambda e, sub=sub: e.transpose(pb[7][:, sub * 128:(sub + 1) * 128], att4[:, sub, :], idt[:]),
                                  reads=[res("att4"), res("idt")], writes=[rpb[7]])
                        sc.op("dve", lambda e, ts=ts, h=h: e.tensor_tensor(mT[:, h, ts], pb[7][:, :], gaT[:, ts], ALU.mult),
                              reads=[rpb[7], res("gaT")], writes=[rmT[h][j]])

            sc.fence()
            sc.op("dve", lambda e: e.memset(pP[:], 0.0), writes=[res("pP")])
            for g in range(4):
                wnd = (2, 4, 8, 16)[g]
                for dc in range(2):
                    sc.dma("pool", "ld_pw", pwS[:, dc, :, :], pw_d[g, dc], writes=[res("pw")])
                for cc in range(2):
                    chunk = 2 * g + cc
                    wb, rw = load_win(24 + chunk)
                    for j in range(4):
                        ts = slice(j * 512, (j + 1) * 512)
                        for kc in range(8):
                            sc.op("pe", lambda e, wb=wb, kc=kc, ts=ts: e.matmul(
                                pb[6][:], lhsT=wb[:, kc, :], rhs=hT[:, kc, ts], start=(kc == 0), stop=(kc == 7)),
                                reads=[rw, rhT[2 * j], rhT[2 * j + 1]], writes=[rpb[6]])
                        sc.op("act", lambda e, j=j: e.activation(pP[:, PADL + j * 512:PADL + (j + 1) * 512], pb[6][:],
                                                                 AF.Copy),
                              reads=[rpb[6]], writes=[res("pP")])
                    L = LP
                    sc.op("dve", lambda e: e.tensor_tensor(pW[0][:, 1:L], pP[:, 0:L - 1], pP[:, 1:L], ALU.add),
                          reads=[res("pP")], writes=[res("pW0")])
                    cur = 0
                    for lvl, sh in ((4, 1), (8, 2), (16, 4)):
                        if wnd < lvl:
                            break
                        nxt = 1 - cur
                        sc.op("dve", lambda e, cur=cur, nxt=nxt, sh=sh: e.tensor_tensor(
                            pW[nxt][:, sh:L - sh], pW[cur][:, 0:L - 2 * sh], pW[cur][:, 2 * sh:L], ALU.add),
                            reads=[res("pW%d" % cur)], writes=[res("pW%d" % nxt)])
                        cur = nxt
                    Wc = pW[cur]
                    rWc = res("pW%d" % cur)
                    sc.op("dve", lambda e, Wc=Wc, cc=cc, wnd=wnd: e.scalar_tensor_tensor(
                        mixT[:, cc, :], Wc[:, PADL:PADL + S], 1.0 / wnd, pP[:, PADL:PADL + S], ALU.mult, ALU.subtract),
                        reads=[rWc, res("pP")], writes=[res("mixT")])
                    for (c0, r0) in ((0, 0), (S - 8, 8)):
                        sc.op("dve", lambda e, Wc=Wc, c0=c0, r0=r0, g=g: e.tensor_tensor(
                            ptmp[:, 0:8], Wc[:, PADL + c0:PADL + c0 + 8], rc[:, g, r0:r0 + 8], ALU.mult),
                            reads=[rWc, res("rc")], writes=[res("ptmp")])
                        sc.op("dve", lambda e, c0=c0, cc=cc: e.tensor_tensor(
                            mixT[:, cc, c0:c0 + 8], ptmp[:, 0:8], pP[:, PADL + c0:PADL + c0 + 8], ALU.subtract),
                            reads=[res("ptmp"), res("pP"), res("mixT")], writes=[res("mixT")])
                for dc in range(2):
                    chunk = 2 * g + dc
                    wb, rw = load_win(40 + chunk)
                    for j in range(4):
                        ts = slice(j * 512, (j + 1) * 512)
                        for kc in range(8):
                            sc.op("pe", lambda e, wb=wb, kc=kc, ts=ts: e.matmul(
                                pb[6][:], lhsT=wb[:, kc, :], rhs=hT[:, kc, ts], start=(kc == 0), stop=(kc == 7)),
                                reads=[rw, rhT[2 * j], rhT[2 * j + 1]], writes=[rpb[6]])
                        sc.op("act", lambda e: e.activation(gpT[:], pb[6][:], AF.Sigmoid),
                              reads=[rpb[6]], writes=[res("gpT")])
                        for cc in range(2):
                            sc.op("pe", lambda e, dc=dc, cc=cc, ts=ts: e.matmul(
                                pb[5][:], lhsT=pwS[:, dc, cc, :], rhs=mixT[:, cc, ts], start=(cc == 0), stop=(cc == 1)),
                                reads=[res("pw"), res("mixT")], writes=[rpb[5]])
                        sc.op("dve", lambda e, chunk=chunk: e.scalar_tensor_tensor(
                            ptmp[:], pb[5][:], psc[:, chunk:chunk + 1], gpT[:], ALU.mult, ALU.mult),
                            reads=[rpb[5], res("gpT"), res("psc")], writes=[res("ptmp")])
                        sc.op("dve", lambda e, chunk=chunk, ts=ts: e.tensor_tensor(
                            mT[:, chunk, ts], mT[:, chunk, ts], ptmp[:], ALU.add),
                            reads=[res("ptmp"), rmT[chunk][j]], writes=[rmT[chunk][j]])

            sc.fence()
            sc.op("dve", lambda e, s=s: e.scalar_tensor_tensor(A2[:], modT[:, 32:40, s], 1.0, n2g[:], ALU.add, ALU.mult),
                  reads=[res("modT"), res("n2g")], writes=[res("Acol")])

            def router(j, part):
                for sub in range(CW // 128):
                    gt = s * 16 + (CW // 128) * j + sub
                    Wt = Wtok[gt % 2]
                    rWt = res("Wtok%d" % (gt % 2))
                    pS = posS[gt % 2]
                    rpS = res("posS%d" % (gt % 2))
                    if part == "b":
                        sc.op("pe", lambda e, sub=sub: e.matmul(pb[5][:, 0:NE], lhsT=lsm[:], rhs=selT2[sub][:], start=True, stop=True),
                              reads=[res("lsm"), res("selT%d" % sub)], writes=[rpb[5]])
                        sc.op("dve", lambda e, pS=pS: e.tensor_tensor(pS[:], pb[5][:, 0:NE], base[:], ALU.add),
                              reads=[rpb[5], res("base")], writes=[rpS])
                        sc.op("pe", lambda e, sub=sub: e.matmul(pb[4][:, 0:NE], lhsT=ones[:], rhs=selT2[sub][:], start=True, stop=True),
                              reads=[res("ones"), res("selT%d" % sub)], writes=[rpb[4]])
                        sc.op("dve", lambda e: e.tensor_tensor(base[:], pb[4][:, 0:NE], base[:], ALU.add),
                              reads=[rpb[4], res("base")], writes=[res("base")])
                        sc.dma("sp", "st_pos", pos_d[gt], pS[:], reads=[rpS], writes=[res("pos_d")])
                        sc.dma("sp", "st_wt", wt_d[gt], Wt[:], reads=[rWt], writes=[res("wt_d")])
                        continue
                    if part == "h":
                        hb = h2tm[gt % 2]
                        rhb = res("h2tm%d" % (gt % 2))
                        tl = slice((CW * j) + sub * 128, (CW * j) + (sub + 1) * 128)
                        pbt = pb[7][:, :].bitcast(BF16)
                        for kc in range(8):
                            sc.op("pe", lambda e, kc=kc, tl=tl, pbt=pbt: e.transpose(pbt[:, kc * 128:(kc + 1) * 128], hT[:, kc, tl], idtb[:]),
                                  reads=[rhT[j], res("idtb")], writes=[rpb[7]])
                        sc.op("act", lambda e, hb=hb, pbt=pbt: e.activation(hb[:], pbt[:, :], AF.Copy),
                              reads=[rpb[7]], writes=[rhb])
                        sc.dma("sp", "st_h2", h2_d[gt * 128:(gt + 1) * 128, :], hb[:], reads=[rhb], writes=[res("h2_d")])
                        continue
                    for kc in range(8):
                        sc.op("pe", lambda e, kc=kc, sub=sub: e.matmul(
                            pb[6][:, 0:NE], lhsT=tmpF[:, kc, sub * 128:(sub + 1) * 128], rhs=wr[:, kc, :],
                            start=(kc == 0), stop=(kc == 7)),
                            reads=[rtmpF[kc], res("wr")], writes=[rpb[6]])
                    sc.op("act", lambda e: e.activation(scr[:], pb[6][:, 0:NE], AF.Sigmoid),
                          reads=[rpb[6]], writes=[res("scr")])
                    sc.op("dve", lambda e: e.tensor_tensor(bia[:], scr[:], brt[:], ALU.add),
                          reads=[res("scr"), res("brt")], writes=[res("bia")])
                    for gi in range(8):
                        sc.op("dve", lambda e, gi=gi: e.max(out=m8[:, gi, :], in_=bia[:, gi * 32:(gi + 1) * 32]),
                              reads=[res("bia")], writes=[res("m8")])
                    sc.op("dve", lambda e: e.tensor_tensor(gsc[:], m8[:, :, 0], m8[:, :, 1], ALU.add),
                          reads=[res("m8")], writes=[res("gsc")])
                    sc.op("dve", lambda e: e.max(out=gm8[:], in_=gsc[:]), reads=[res("gsc")], writes=[res("gm8")])
                    sc.op("dve", lambda e: e.tensor_scalar(gmk[:], gsc[:], gm8[:, 3:4], None, ALU.is_ge),
                          reads=[res("gsc"), res("gm8")], writes=[res("gmk")])
                    sc.op("dve", lambda e: e.tensor_scalar(msk[:], bia[:], 2.0, None, ALU.add),
                          reads=[res("bia")], writes=[res("msk")])
                    sc.op("dve", lambda e: e.tensor_tensor(
                        msk[:, :].rearrange("p (g k) -> p g k", g=8), msk[:, :].rearrange("p (g k) -> p g k", g=8),
                        gmk[:, :].unsqueeze(2).to_broadcast([128, 8, 32]), ALU.mult),
                        reads=[res("msk"), res("gmk")], writes=[res("msk")])
                    sc.op("dve", lambda e: e.max(out=t8[:], in_=msk[:]), reads=[res("msk")], writes=[res("t8")])
                    sc.op("dve", lambda e, Wt=Wt: e.scalar_tensor_tensor(
                        Wt[:], msk[:], t8[:, 7:8], scr[:], ALU.is_ge, ALU.mult),
                        reads=[res("msk"), res("t8"), res("scr")], writes=[rWt])
                    sc.op("dve", lambda e, Wt=Wt: e.tensor_reduce(den[:, 0:1], Wt[:], AX.X, ALU.add),
                          reads=[rWt], writes=[res("den")])
                    sc.op("dve", lambda e: e.reciprocal(den[:, 1:2], den[:, 0:1]),
                          reads=[res("den")], writes=[res("den")])
                    sc.op("dve", lambda e, Wt=Wt: e.tensor_scalar(Wt[:], Wt[:], den[:, 1:2], 2.5, ALU.mult, ALU.mult),
                          reads=[rWt, res("den")], writes=[rWt])
                    sc.op("dve", lambda e, Wt=Wt, sub=sub: e.tensor_scalar(selT2[sub][:], Wt[:], 0.0, None, ALU.is_gt),
                          reads=[rWt], writes=[res("selT%d" % sub)])

            for j in range(8):
                ts = slice(j * CW, (j + 1) * CW)
                sc.dma("sp", "ld_x", xch[:], xT_d[s][:, :, ts], writes=[res("xch")])
                for oc in range(8):
                    wb = woS[oc % 2]
                    rw = res("wo%d" % (oc % 2))
                    sc.dma("pool", "ld_wo%d" % (oc % 2), wb[:], wo_d[oc], writes=[rw])
                    for kc in range(8):
                        sc.op("pe", lambda e, wb=wb, kc=kc, ts=ts: e.matmul(
                            pb[4][:, 0:CW], lhsT=wb[:, kc, :], rhs=mT[:, kc, ts], start=(kc == 0), stop=(kc == 7)),
                            reads=[rw], writes=[rpb[4]])
                    sc.op("dve", lambda e, oc=oc, s=s: e.scalar_tensor_tensor(
                        xch[:, oc, :], pb[4][:, 0:CW], modT[:, 16 + oc, s:s + 1], xch[:, oc, :], ALU.mult, ALU.add),
                        reads=[rpb[4], res("modT"), res("xch")], writes=[res("xch")])
                rmsnorm_chunk(A2, 24, s, ts, rhT[j], True)
                router(j, "a")
                router(j, "h")
                for half in range(2):
                    hs = slice(half * 128, (half + 1) * 128)
                    for kc in range(8):
                        sc.op("pe", lambda e, kc=kc, hs=hs, ts=ts, half=half: e.matmul(
                            pb[half][:, 0:CW], lhsT=wsg[:, kc, hs], rhs=hT[:, kc, ts], start=(kc == 0), stop=(kc == 7)),
                            reads=[res("wsg"), rhT[j]], writes=[rpb[half]])
                    for kc in range(8):
                        sc.op("pe", lambda e, kc=kc, hs=hs, ts=ts, half=half: e.matmul(
                            pb[2 + half][:, 0:CW], lhsT=wsu[:, kc, hs], rhs=hT[:, kc, ts], start=(kc == 0), stop=(kc == 7)),
                            reads=[res("wsu"), rhT[j]], writes=[rpb[2 + half]])
                router(j, "b")
                for half in range(2):
                    sc.op("act", lambda e, half=half: e.activation(sil[:, half, 0:CW], pb[half][:, 0:CW], AF.Silu),
                          reads=[rpb[half]], writes=[res("sil%d" % half)])
                    sc.op("dve", lambda e, half=half: e.tensor_tensor(
                        aT[:, half, 0:CW], sil[:, half, 0:CW], pb[2 + half][:, 0:CW], ALU.mult),
                        reads=[res("sil%d" % half), rpb[2 + half]], writes=[res("aT")])
                for dcn in range(8):
                    bi = 5 + (dcn % 2)
                    for cc in range(2):
                        sc.op("pe", lambda e, cc=cc, dcn=dcn, bi=bi: e.matmul(
                            pb[bi][:, 0:CW], lhsT=wsd[:, cc, dcn * 128:(dcn + 1) * 128], rhs=aT[:, cc, 0:CW],
                            start=(cc == 0), stop=(cc == 1)),
                            reads=[res("wsd"), res("aT")], writes=[rpb[bi]])
                    sc.op("dve", lambda e, dcn=dcn, bi=bi, s=s: e.scalar_tensor_tensor(
                        xch[:, dcn, :], pb[bi][:, 0:CW], modT[:, 40 + dcn, s:s + 1], xch[:, dcn, :], ALU.mult, ALU.add),
                        reads=[rpb[bi], res("modT"), res("xch")], writes=[res("xch")])
                sc.dma("sp", "st_xs", xs_d[s][:, :, ts], xch[:], reads=[res("xch")], writes=[res("xs_d")])

        sc.fence()
        us = [cvA(0, 256), cvA(256, 256)]
        ui = [cvA(512, 256), cvA(768, 256)]
        iot = cvA(1024, 1024)
        for c in range(2):
            sc.dma("sp", "ld_us", us[c][:], us_d[c], writes=[res("us%d" % c)])
            sc.dma("sp", "ld_ui", ui[c][:], ui_d[c], writes=[res("ui%d" % c)])
        sc.dma("sp", "ld_iot", iot[:], iot_d, writes=[res("iot")])
        sc.op("dve", lambda e: e.tensor_scalar(nbf[:], base[:], 127.0, None, ALU.add), reads=[res("base")], writes=[res("nbf")])
        sc.op("dve", lambda e: e.tensor_copy(out=nbi[:], in_=nbf[:]), reads=[res("nbf")], writes=[res("nbi")])
        sc.op("dve", lambda e: e.tensor_single_scalar(nbi[:], nbi[:], 7, ALU.arith_shift_right),
              reads=[res("nbi")], writes=[res("nbi")])
        sc.op("dve", lambda e: e.tensor_copy(out=nbf[:], in_=nbi[:]), reads=[res("nbi")], writes=[res("nbf")])
        for c in range(2):
            sc.op("pe", lambda e, c=c: e.transpose(pb[c][:, 0:128], nbf[:, c * 128:(c + 1) * 128], idt[:]),
                  reads=[res("nbf"), res("idt")], writes=[rpb[c]])
            sc.op("dve", lambda e, c=c: e.tensor_copy(out=nbT[c][:], in_=pb[c][:, 0:128]), reads=[rpb[c]], writes=[res("nbT%d" % c)])
        for c in range(2):
            sc.op("pe", lambda e, c=c: e.matmul(pb[2][:, 0:NE], lhsT=nbT[c][:], rhs=us[c][:], start=(c == 0), stop=(c == 1)),
                  reads=[res("nbT%d" % c), res("us%d" % c)], writes=[rpb[2]])
        sc.op("dve", lambda e: e.tensor_scalar(sbase[:], pb[2][:, 0:NE], 128.0, None, ALU.mult), reads=[rpb[2]], writes=[res("sbase")])
        for c2 in range(2):
            for c in range(2):
                sc.op("pe", lambda e, c=c, c2=c2: e.matmul(pb[3][:, 0:2], lhsT=ui[c][:, c2 * 128:(c2 + 1) * 128], rhs=nbT[c][:, 0:2],
                                                           start=(c == 0), stop=(c == 1)),
                      reads=[res("nbT%d" % c), res("ui%d" % c)], writes=[rpb[3]])
            sc.op("dve", lambda e, c2=c2: e.tensor_copy(out=bend[:, c2:c2 + 1], in_=pb[3][:, 0:1]), reads=[rpb[3]], writes=[res("bend")])
        for c2 in range(2):
            sc.op("dve", lambda e, c2=c2: e.tensor_scalar(Cm[c2][:], iot[:, 0:NBLK], bend[:, c2:c2 + 1], None, ALU.is_ge),
                  reads=[res("iot"), res("bend")], writes=[res("Cm%d" % c2)])
        for c0 in range(0, NBLK, 512):
            cw = min(512, NBLK - c0)
            for c2 in range(2):
                sc.op("pe", lambda e, c0=c0, cw=cw, c2=c2: e.matmul(pb[4][:, 0:cw], lhsT=ones[:], rhs=Cm[c2][:, c0:c0 + cw],
                                                                    start=(c2 == 0), stop=(c2 == 1)),
                      reads=[res("ones"), res("Cm%d" % c2)], writes=[rpb[4]])
            sc.op("dve", lambda e, c0=c0, cw=cw: e.tensor_scalar(bef[:, c0:c0 + cw], pb[4][:, 0:cw], 128.0, None, ALU.mult),
                  reads=[rpb[4]], writes=[res("bef")])
        sc.op("dve", lambda e: e.tensor_scalar(idxW[:], bef[:], pio8[:, 0:1], None, ALU.add),
              reads=[res("bef"), res("pio8")], writes=[res("idxW")])
        sc.op("dve", lambda e: e.memset(zslot[:], 0.0), writes=[res("zslot")])
        sc.dma("sp", "st_z", slot_d.rearrange("(p a) b -> p (a b)", p=128), zslot[:], reads=[res("zslot")], writes=[res("slot_d")])
        def loadB(gt):
            bp = gt % NPB
            sc.dma("sp", "ld_pos%d" % bp, posL[bp][:], pos_d[gt], reads=[res("pos_d")], writes=[res("posL%d" % bp)])
            sc.dma("sp", "ld_wt%d" % bp, wtL[bp][:], wt_d[gt], reads=[res("wt_d")], writes=[res("wtL%d" % bp)])

        for g0 in range(min(NPB - 1, NT)):
            loadB(g0)
        for gt in range(NT):
            bp, br = gt % NPB, gt % NRB
            if gt + NPB - 1 < NT:
                loadB(gt + NPB - 1)
            sc.op("dve", lambda e, bp=bp: e.scalar_tensor_tensor(dp1[:], posL[bp][:], 1.0, sbase[:], ALU.add, ALU.add),
                  reads=[res("posL%d" % bp), res("sbase")], writes=[res("dp1")])
            sc.op("dve", lambda e, bp=bp: e.scalar_tensor_tensor(keyb[:], wtL[bp][:], 0.0, dp1[:], ALU.is_gt, ALU.mult),
                  reads=[res("wtL%d" % bp), res("dp1")], writes=[res("keyb")])
            sc.op("dve", lambda e: e.max(out=d8[:], in_=keyb[:]), reads=[res("keyb")], writes=[res("d8")])
            rrw = res("rows%d" % br)
            sc.op("dve", lambda e, br=br, gt=gt: e.tensor_scalar(rows[br][:, :, 0], pio8[:], float(gt * 128), None, ALU.add),
                  reads=[res("pio8")], writes=[rrw])
            for k in range(8):
                sc.op("dve", lambda e, bp=bp, br=br, k=k: e.scalar_tensor_tensor(
                    junkb[:], keyb[:], d8[:, k:k + 1], wtL[bp][:], ALU.is_equal, ALU.mult, accum_out=rows[br][:, k, 1:2]),
                    reads=[res("keyb"), res("d8"), res("wtL%d" % bp)], writes=[res("junkb"), rrw])
            sc.op("dve", lambda e, gt=gt: e.tensor_scalar(dstu[:, gt, :], d8[:], -1.0, None, ALU.add),
                  reads=[res("d8")], writes=[res("dstu%d" % gt)])
            for k in range(8):
                sc.idma("sc_slot%d" % br, slot_d, dstu[:, gt, k:k + 1], rows[br][:, k, :], None,
                        reads=[rrw, res("dstu%d" % gt)], writes=[res("slot_d%d" % br)])

        sc.fence()
        slotv = slot_d.rearrange("(i p) c -> i (p c)", p=128)
        for t6 in range(NBLK // 128):
            b2 = t6 % 2
            sc.dma("sp", "ld_srow%d" % b2, sl6[b2][:], slotv[t6 * 128:(t6 + 1) * 128, :], reads=[res("slot_d")],
                   writes=[res("sl6_%d" % b2)])
            v3 = sl6[b2][:, :].rearrange("i (p c) -> i p c", c=2)
            for c in range(2):
                sc.op("dve", lambda e, c=c, v3=v3: e.tensor_copy(out=sl6c[c][:], in_=v3[:, :, c]),
                      reads=[res("sl6_%d" % b2)], writes=[res("sl6c%d" % c)])
                sc.op("pe", lambda e, c=c: e.transpose(pb[c][:, 0:128], sl6c[c][:], idt[:]),
                      reads=[res("sl6c%d" % c), res("idt")], writes=[rpb[c]])
            sc.op("dve", lambda e, t6=t6: e.tensor_copy(out=tokuA[:, t6 * 128:(t6 + 1) * 128], in_=pb[0][:, 0:128]),
                  reads=[rpb[0]], writes=[res("tokuA")])
            sc.op("act", lambda e, t6=t6: e.activation(wA[:, t6 * 128:(t6 + 1) * 128], pb[1][:, 0:128], AF.Copy),
                  reads=[rpb[1]], writes=[res("srowA")])
        WB = NE * 128 - 1

        def gathers(i):
            b3 = i % 3
            sc.idma("g_x%d" % b3, xgC[b3][:], None, h2_d, tokuA[:, i:i + 1], reads=[res("tokuA"), res("h2_d")],
                    writes=[res("xg%d" % b3)])
            sc.idma("g_wg%d" % b3, wgC[b3][:], None, wg2, idxW[:, i:i + 1], reads=[res("idxW")], writes=[res("wgC%d" % b3)], bound=WB)
            sc.idma("g_wu%d" % b3, wuC[b3][:], None, wu2, idxW[:, i:i + 1], reads=[res("idxW")], writes=[res("wuC%d" % b3)], bound=WB)
            sc.idma("g_wd%d" % b3, wdC[b3][:, :, :].rearrange("p a b -> p (a b)"), None, wd2, idxW[:, i:i + 1],
                    reads=[res("idxW")], writes=[res("wdC%d" % b3)], bound=WB)

        def stage1(i):
            b2, b3 = i % 2, i % 3
            pT_, rT_ = pb[4 * b2], rpb[4 * b2]
            pbt = pT_[:, :].bitcast(BF16)
            for kc in range(8):
                sc.op("pe", lambda e, kc=kc, b3=b3, pbt=pbt: e.transpose(
                    pbt[:, kc * 128:(kc + 1) * 128], xgC[b3][:, kc * 128:(kc + 1) * 128], idtb[:]),
                    reads=[res("xg%d" % b3), res("idtb")], writes=[rT_])
            sc.op("act", lambda e, b2=b2, pbt=pbt: e.activation(xgT[b2][:, :, :].rearrange("p a b -> p (a b)"), pbt[:, :], AF.Copy),
                  reads=[rT_], writes=[res("xgT%d" % b2)])

        def stage2(i):
            b2, b3 = i % 2, i % 3
            pG_, rG_ = pb[4 * b2 + 1], rpb[4 * b2 + 1]
            for gu, wC, rn in ((0, wgC, "wgC%d"), (1, wuC, "wuC%d")):
                for half in range(2):
                    col = (gu * 2 + half) * 128
                    for kc in range(8):
                        sc.op("pe", lambda e, kc=kc, b2=b2, b3=b3, wC=wC, half=half, col=col, pG_=pG_: e.matmul(
                            pG_[:, col:col + 128], lhsT=wC[b3][:, kc * 256 + half * 128:kc * 256 + (half + 1) * 128],
                            rhs=xgT[b2][:, kc, :], start=(kc == 0), stop=(kc == 7)),
                            reads=[res(rn % b3), res("xgT%d" % b2)], writes=[rG_])
            sc.op("act", lambda e, b2=b2, pG_=pG_: e.activation(silC[b2][:], pG_[:, 0:256], AF.Silu),
                  reads=[rG_], writes=[res("silC%d" % b2)])
            sc.op("dve", lambda e, b2=b2, pG_=pG_: e.tensor_tensor(
                aTC[b2][:, :, :].rearrange("p a b -> p (a b)"), silC[b2][:], pG_[:, 256:512], ALU.mult),
                reads=[res("silC%d" % b2), rG_], writes=[res("aTC%d" % b2)])

        def stage3(i):
            b2, b3 = i % 2, i % 3
            pY0, pY1, rY0, rY1 = pb[4 * b2 + 2], pb[4 * b2 + 3], rpb[4 * b2 + 2], rpb[4 * b2 + 3]
            for dh, pY_, rY_ in ((0, pY0, rY0), (1, pY1, rY1)):
                for cc in range(2):
                    sc.op("pe", lambda e, cc=cc, b2=b2, b3=b3, dh=dh, pY_=pY_: e.matmul(
                        pY_[:], lhsT=aTC[b2][:, cc, :], rhs=wdC[b3][:, cc, dh * 512:(dh + 1) * 512],
                        start=(cc == 0), stop=(cc == 1)),
                        reads=[res("aTC%d" % b2), res("wdC%d" % b3)], writes=[rY_])
            sc.op("act", lambda e, b2=b2, pY0=pY0, i=i: e.activation(ysb[b2][:, 0:512], pY0[:], AF.Copy, scale=wA[:, i:i + 1]),
                  reads=[rY0, res("srowA")], writes=[res("ysb%d" % b2)])
            sc.op("dve", lambda e, b2=b2, pY1=pY1, i=i: e.tensor_scalar(ysb[b2][:, 512:1024], pY1[:], wA[:, i:i + 1], None, ALU.mult),
                  reads=[rY1, res("srowA")], writes=[res("ysb%d" % b2)])
            sc.dma("sp", "st_y", y_d[i * 128:(i + 1) * 128, :], ysb[b2][:], reads=[res("ysb%d" % b2)], writes=[res("y_d")])

        gathers(0)
        gathers(1)
        stage1(0)
        for i in range(NBLK):
            if i + 2 < NBLK:
                gathers(i + 2)
            stage2(i)
            if i + 1 < NBLK:
                stage1(i + 1)
            stage3(i)

        sc.fence()
        xstL = [cvB(0, 1024).rearrange("p (a b) -> p a b", a=8), cvB(2048, 1024).rearrange("p (a b) -> p a b", a=8)]
        otlL = [cvB(1024, 1024).rearrange("p (a b) -> p a b", a=8), cvB(3072, 1024).rearrange("p (a b) -> p a b", a=8)]

        def gathD(gt):
            b3 = gt % NYG
            for k in range(8):
                sc.idma("g_y%d" % b3, yg[b3][:, k, :], None, y_d, dstu[:, gt, k:k + 1], reads=[res("dstu"), res("y_d")],
                        writes=[res("yg%d" % b3)])

        for g0 in range(min(NYG - 1, NT)):
            gathD(g0)
        for gt in range(NT):
            b2, b3 = gt % 2, gt % NYG
            if gt + NYG - 1 < NT:
                gathD(gt + NYG - 1)
            s_, t_ = gt // 16, gt % 16
            tt = slice(t_ * 128, (t_ + 1) * 128)
            ryg = res("yg%d" % b3)
            acc_, racc = accD[b2], res("accD%d" % b2)
            xst_, rxst = xstL[b2], res("xst%d" % b2)
            otl_, rotl = otlL[b2], res("otl%d" % b2)
            if gt == 0:
                sc.dma("sp", "ld_xs%d" % b2, xst_[:, :, :], xs_d[s_][:, :, tt], reads=[res("xs_d")], writes=[rxst])
            if gt + 1 < NT:
                g1 = gt + 1
                s1, t1 = g1 // 16, g1 % 16
                sc.dma("sp", "ld_xs%d" % (g1 % 2), xstL[g1 % 2][:, :, :], xs_d[s1][:, :, t1 * 128:(t1 + 1) * 128],
                       reads=[res("xs_d")], writes=[res("xst%d" % (g1 % 2))])
            sc.op("dve", lambda e, b3=b3, acc_=acc_: e.tensor_tensor(acc_[:], yg[b3][:, 0, :], yg[b3][:, 1, :], ALU.add),
                  reads=[ryg], writes=[racc])
            for k in range(2, 8):
                sc.op("dve", lambda e, k=k, b3=b3, acc_=acc_: e.tensor_tensor(acc_[:], acc_[:], yg[b3][:, k, :], ALU.add),
                      reads=[ryg, racc], writes=[racc])
            for c in range(8):
                bi = 4 * b2 + c // 4
                sc.op("pe", lambda e, c=c, bi=bi, acc_=acc_: e.transpose(pb[bi][:, (c % 4) * 128:(c % 4 + 1) * 128],
                                                                          acc_[:, c * 128:(c + 1) * 128], idt[:]),
                      reads=[racc, res("idt")], writes=[rpb[bi]])
            for c in range(8):
                bi = 4 * b2 + c // 4
                sc.op("dve", lambda e, c=c, bi=bi, s_=s_, otl_=otl_, xst_=xst_: e.scalar_tensor_tensor(
                    otl_[:, c, :], pb[bi][:, (c % 4) * 128:(c % 4 + 1) * 128], modT[:, 40 + c, s_:s_ + 1], xst_[:, c, :],
                    ALU.mult, ALU.add),
                    reads=[rpb[bi], res("modT"), rxst], writes=[rotl])
            tok = sc.dma("sp", "st_out", out_d[s_][:, :, tt], otl_[:, :, :], reads=[rotl], writes=[res("out_d")])
        sc.wait("sp", tok)
        sc.emit()
    return nc


def _t5_bucket_np(rel):
    import jax
    import jax.numpy as jnp
    with jax.default_device(jax.devices("cpu")[0]):
        nb = NB // 2
        max_exact = nb // 2
        rel = jnp.asarray(np.asarray(rel, dtype=np.int32))
        n = jnp.abs(rel)
        large = max_exact + (jnp.log(jnp.maximum(n, 1).astype(jnp.float32) / max_exact)
                             / math.log(128 / max_exact) * (nb - max_exact)).astype(jnp.int32)
        large = jnp.minimum(large, nb - 1)
        return np.asarray(jnp.where(rel > 0, nb, 0) + jnp.where(n < max_exact, n, large))


def _chunk_w(w, ncol_chunk=128):
    K, N = w.shape
    return np.ascontiguousarray(w.reshape(K // 128, 128, N // ncol_chunk, ncol_chunk).transpose(2, 1, 0, 3))


def _prep_shared(inp):
    f = np.float32
    g = {}
    g["w_ada"] = _chunk_w(inp["w_ada"][0])
    g["b_adaT"] = np.ascontiguousarray(inp["b_ada"][0].reshape(48, 128).T)
    g["n1g"] = np.ascontiguousarray(inp["norm1_g"][0].reshape(8, 128).T)
    g["n2g"] = np.ascontiguousarray(inp["norm2_g"][0].reshape(8, 128).T)
    g["w_in"] = _chunk_w(inp["w_in"][0])
    g["qkg"] = np.ascontiguousarray(np.stack([np.tile(inp["q_norm_g"][0], 2), np.tile(inp["k_norm_g"][0], 2)], axis=1))
    lam = np.stack([inp["lambda_q1"][0], inp["lambda_k1"][0], inp["lambda_q2"][0], inp["lambda_k2"][0]], axis=0)
    g["lam_in"] = np.ascontiguousarray(np.broadcast_to(lam[None], (128, 4, 64)))
    g["subln_g"] = np.ascontiguousarray(np.broadcast_to(inp["subln_g"][0][None], (128, 128)))
    pw = inp["pool_w"][0]
    g["pool_w"] = np.ascontiguousarray(pw.reshape(4, 2, 128, 2, 128).transpose(0, 3, 2, 1, 4))
    g["pool_scaleT"] = np.ascontiguousarray(inp["pool_scale"][0].reshape(8, 128).T)
    rc = np.zeros((4, 16), f)
    for gi, w in enumerate((2, 4, 8, 16)):
        for k, pos in enumerate(list(range(8)) + list(range(S - 8, S))):
            lo = min(max(pos - w // 2, 0), S - 1)
            hi = min(max(pos + w // 2 - 1, 0), S - 1)
            rc[gi, k] = 1.0 / float(hi - lo + 1)
    g["pool_rc"] = np.ascontiguousarray(np.broadcast_to(rc[None], (128, 4, 16)))
    g["w_out"] = _chunk_w(inp["w_out"][0])
    g["w_router"] = np.ascontiguousarray(inp["w_router"][0].reshape(8, 128, NE).transpose(1, 0, 2))
    g["b_router"] = np.ascontiguousarray(np.broadcast_to(inp["b_router"][0][None], (128, NE)))
    wg = np.concatenate([inp["w_exp_gate"][0], inp["w_sh_gate"]], axis=0)
    wu = np.concatenate([inp["w_exp_up"][0], inp["w_sh_up"]], axis=0)
    wd = np.concatenate([inp["w_exp_down"][0], inp["w_sh_down"]], axis=0)
    g["w_eg"] = np.ascontiguousarray(wg.reshape(NE + 1, 8, 128, 256).transpose(0, 2, 1, 3))
    g["w_eu"] = np.ascontiguousarray(wu.reshape(NE + 1, 8, 128, 256).transpose(0, 2, 1, 3))
    g["w_ed"] = np.ascontiguousarray(wd.reshape(NE + 1, 2, 128, 1024).transpose(0, 2, 1, 3))
    g["rel_tab"] = np.ascontiguousarray(inp["rel_bias_table"])
    jj = np.arange(FVW)
    bk = _t5_bucket_np(767 - jj)
    oh = np.zeros((NB, FVW), f)
    oh[bk, jj] = 1.0
    g["bias_oh"] = oh
    g["ident"] = np.eye(128, dtype=f)
    g["antiid"] = np.ascontiguousarray(np.eye(128, dtype=f)[::-1])
    bo = np.zeros((128, 128), f)
    bo[:64, :64] = 1.0
    bo[64:, 64:] = 1.0
    g["blockones"] = bo
    ar = np.arange(128)
    g["lstrict"] = (ar[:, None] < ar[None, :]).astype(f)
    ee = np.arange(NE)
    g["ustrict"] = np.stack([((c * 128 + ar)[:, None] < ee[None, :]).astype(f) for c in range(2)])
    g["uincl"] = np.stack([((c * 128 + ar)[:, None] <= ee[None, :]).astype(f) for c in range(2)])
    g["iota_row"] = np.ascontiguousarray(np.broadcast_to(np.arange(1024, dtype=f)[None], (128, 1024)))
    g["piota8"] = np.ascontiguousarray(np.broadcast_to(ar.astype(f)[:, None], (128, 8)))
    return {k: np.asarray(v, dtype=f) for k, v in g.items()}


def _core_inputs(shared, x, c, b0, nseq):
    m = dict(shared)
    xs = x[b0:b0 + nseq]
    m["xT"] = np.ascontiguousarray(xs.reshape(nseq, S, 8, 128).transpose(0, 3, 2, 1))
    m["cT"] = np.ascontiguousarray(c[b0:b0 + nseq].reshape(nseq, 8, 128).transpose(2, 1, 0))
    return m


def _unpack(outT):
    return np.ascontiguousarray(outT.transpose(0, 3, 2, 1).reshape(outT.shape[0], S, D))


def kernel(**inputs):
    inp = {k: np.asarray(v, dtype=np.float32) for k, v in inputs.items()}
    ncores = 8
    nseq = inp["x"].shape[0] // ncores
    shared = _prep_shared(inp)
    nc = build(nseq)
    in_maps = [_core_inputs(shared, inp["x"], inp["c"], i * nseq, nseq) for i in range(ncores)]
    res = run_bass_kernel_spmd(nc, in_maps, core_ids=list(range(ncores)))
    return np.concatenate([_unpack(r["outT"]) for r in res.results], axis=0)
```

```python
import math
import numpy as np
from contextlib import ExitStack
import concourse.bass as bass
import concourse.mybir as mybir
from concourse.bass_utils import run_bass_kernel_spmd

F32 = mybir.dt.float32
BF16 = mybir.dt.bfloat16
AF = mybir.ActivationFunctionType
ALU = mybir.AluOpType
AX = mybir.AxisListType

D = 1024
S = 2048
NB = 32
NH = 8
NE = 256
EPS = 1e-6
LAM_INIT = 0.8 - 0.6 * math.exp(-0.3 * 0)
PADL = 16
LP = S + 2 * PADL
MW = 1280
FVW = MW + 127


class Res:
    __slots__ = ("w", "r")

    def __init__(self):
        self.w = None
        self.r = {}


class Sched:
    ENG = ("pe", "dve", "act", "pool", "sp")

    def __init__(self, nc, es):
        self.nc = nc
        self.es = es
        self.sem = {k: es.enter_context(nc.semaphore("s_" + k)) for k in self.ENG}
        self.cnt = {k: 0 for k in self.ENG}
        self.seen = {k: {} for k in self.ENG}
        self.prog = {k: [] for k in self.ENG}
        self.dsem = {}
        self.dcnt = {}

    def _wait(self, e, tok):
        if tok is None:
            return
        key, val = tok
        if key == e and e == "pe":
            return
        if self.seen[e].get(key, 0) >= val:
            return
        sem = self.sem[key] if key in self.sem else self.dsem[key]
        self.prog[e].append(("w", sem, val))
        self.seen[e][key] = val

    def _deps(self, e, reads, writes):
        for r in reads:
            self._wait(e, r.w)
        for w in writes:
            self._wait(e, w.w)
            for k, v in w.r.items():
                self._wait(e, (k, v))

    def _commit(self, tok, reads, writes):
        for r in reads:
            if r.r.get(tok[0], 0) < tok[1]:
                r.r[tok[0]] = tok[1]
        for w in writes:
            w.w = tok
            w.r = {}

    def op(self, e, fn, reads=(), writes=()):
        self._deps(e, reads, writes)
        self.cnt[e] += 1
        self.prog[e].append(("i", fn, self.sem[e], 1))
        tok = (e, self.cnt[e])
        self._commit(tok, reads, writes)
        return tok

    def dma(self, e, chan, out, in_, reads=(), writes=()):
        if chan not in self.dsem:
            self.dsem[chan] = self.es.enter_context(self.nc.semaphore("dm_" + chan))
            self.dcnt[chan] = 0
        self._deps(e, reads, writes)
        self.dcnt[chan] += 16
        self.prog[e].append(("i", lambda eng: eng.dma_start(out=out, in_=in_), self.dsem[chan], 16))
        tok = (chan, self.dcnt[chan])
        self._commit(tok, reads, writes)
        return tok

    def idma(self, chan, out, out_off, in_, in_off, reads=(), writes=(), bound=None):
        e = "pool"
        if chan not in self.dsem:
            self.dsem[chan] = self.es.enter_context(self.nc.semaphore("dm_" + chan))
            self.dcnt[chan] = 0
        self._deps(e, reads, writes)
        self.dcnt[chan] += 16
        oo = None if out_off is None else bass.IndirectOffsetOnAxis(ap=out_off, axis=0)
        io = None if in_off is None else bass.IndirectOffsetOnAxis(ap=in_off, axis=0)
        if bound is None:
            self.prog[e].append(("i", lambda eng: eng.indirect_dma_start(out=out, out_offset=oo, in_=in_, in_offset=io),
                                 self.dsem[chan], 16))
        else:
            self.bound_val = bound
            self.prog[e].append(("i", lambda eng: eng.indirect_dma_start(out=out, out_offset=oo, in_=in_, in_offset=io,
                                                                         bounds_check=self.bound_reg, oob_is_err=False),
                                 self.dsem[chan], 16))
        tok = (chan, self.dcnt[chan])
        self._commit(tok, reads, writes)
        return tok

    def wait(self, e, tok):
        self._wait(e, tok)

    def fence(self):
        toks = [(k, self.cnt[k]) for k in self.ENG if self.cnt[k] > 0]
        toks += [(k, v) for k, v in self.dcnt.items() if v > 0]
        for e in self.ENG:
            for t in toks:
                if t[0] == e:
                    continue
                self._wait(e, t)

    def emit(self):
        nc = self.nc
        with nc.Block() as block:
            def mk(e):
                def f(eng):
                    if e == "pool" and getattr(self, "bound_val", None) is not None:
                        self.bound_reg = eng.alloc_register("oob_bound")
                        eng.reg_mov(self.bound_reg, int(self.bound_val))
                    for it in self.prog[e]:
                        if it[0] == "w":
                            eng.wait_ge(it[1], it[2])
                        else:
                            it[1](eng).then_inc(it[2], it[3])
                return f
            block.tensor(mk("pe"))
            block.vector(mk("dve"))
            block.scalar(mk("act"))
            block.gpsimd(mk("pool"))
            block.sync(mk("sp"))


def build(NSEQ, n_exp=NE, dbg=False):
    nc = bass.Bass("TRN2", target_bir_lowering=False)

    def din(name, shape):
        return nc.dram_tensor(name, list(shape), F32, kind="ExternalInput").ap()

    xT_d = din("xT", [NSEQ, 128, 8, S])
    cT_d = din("cT", [128, 8, NSEQ])
    wada_d = din("w_ada", [48, 128, 8, 128])
    bada_d = din("b_adaT", [128, 48])
    n1g_d = din("n1g", [128, 8])
    n2g_d = din("n2g", [128, 8])
    win_d = din("w_in", [48, 128, 8, 128])
    qkg_d = din("qkg", [128, 2])
    lam_d = din("lam_in", [128, 4, 64])
    sg_d = din("subln_g", [128, 128])
    pw_d = din("pool_w", [4, 2, 128, 2, 128])
    psc_d = din("pool_scaleT", [128, 8])
    rc_d = din("pool_rc", [128, 4, 16])
    wo_d = din("w_out", [8, 128, 8, 128])
    wr_d = din("w_router", [128, 8, NE])
    br_d = din("b_router", [128, NE])
    wg_d = din("w_eg", [NE + 1, 128, 8, 256])
    wu_d = din("w_eu", [NE + 1, 128, 8, 256])
    wd_d = din("w_ed", [NE + 1, 128, 2, 1024])
    tab_d = din("rel_tab", [NB, NH])
    oh_d = din("bias_oh", [NB, FVW])
    idt_d = din("ident", [128, 128])
    aid_d = din("antiid", [128, 128])
    bones_d = din("blockones", [128, 128])
    out_d = nc.dram_tensor("outT", [NSEQ, 128, 8, S], F32, kind="ExternalOutput").ap()
    fv_d = nc.dram_tensor("fv_scr", [NH, FVW], F32, kind="Internal").ap()
    mst_d = nc.dram_tensor("mst_scr", [NH, 128, MW], F32, kind="Internal").ap()
    T = NSEQ * S
    NT = T // 128
    NBLK = T * 8 // 128 + NE
    NSLOT = NBLK * 128
    ls_d = din("lstrict", [128, 128])
    us_d = din("ustrict", [2, 128, NE])
    ui_d = din("uincl", [2, 128, NE])
    iot_d = din("iota_row", [128, 1024])
    pio_d = din("piota8", [128, 8])
    U32 = mybir.dt.uint32
    I32 = mybir.dt.int32
    h2_d = nc.dram_tensor("h2_scr", [T, D], BF16, kind="Internal").ap()
    pos_d = nc.dram_tensor("pos_scr", [NT, 128, NE], F32, kind="Internal").ap()
    wt_d = nc.dram_tensor("wt_scr", [NT, 128, NE], F32, kind="Internal").ap()
    xs_d = nc.dram_tensor("xs_scr", [NSEQ, 128, 8, S], F32, kind="ExternalOutput").ap() if dbg else out_d
    slot_d = nc.dram_tensor("slot_scr", [NSLOT, 2], F32, kind="Internal").ap()
    y_d = nc.dram_tensor("y_scr", [NSLOT, D], BF16, kind="Internal").ap()
    wg2 = wg_d.rearrange("e p a b -> (e p) (a b)")
    wu2 = wu_d.rearrange("e p a b -> (e p) (a b)")
    wd2 = wd_d.rearrange("e p a b -> (e p) (a b)")

    with ExitStack() as es:
        es.enter_context(nc.allow_low_precision("bf16 matmul operands, fp32 accumulation"))
        es.enter_context(nc.allow_non_contiguous_dma("overlapping-window bias load"))
        sc = Sched(nc, es)

        def sb(name, shape, dt=F32):
            return es.enter_context(nc.sbuf_tensor(name, list(shape), dt))

        CW = 256
        arA = sb("arA", [128, 16384])
        arB = sb("arB", [128, 8320])

        def cvA(off, n, dt=F32):
            return arA[:, off:off + n] if dt == F32 else arA[:, off:off + n].bitcast(dt)

        def cvB(off, n, dt=F32):
            return arB[:, off:off + n] if dt == F32 else arB[:, off:off + n].bitcast(dt)

        bigA = arA[:, :].rearrange("p (a b) -> p a b", a=8)
        mT = cvA(0, 8192, BF16).rearrange("p (a b) -> p a b", a=8)
        qz = [cvA(8192, 1024, BF16), cvB(0, 1024, BF16)]
        kT = cvA(9216, 1024, BF16)
        vA = cvA(10240, 1040, BF16).rearrange("p (a b) -> p a b", a=16)
        gaT = cvA(11280, 1024, BF16)
        mst = cvA(12304, 1280)
        O = [cvA(13584, 520).rearrange("p (a b) -> p a b", a=4), cvA(14104, 520).rearrange("p (a b) -> p a b", a=4)]
        sadd = [cvA(14624, 512), cvA(15136, 512)]
        hank = cvA(0, 1280)
        oh = arA[0:NB, 1280:1280 + FVW]
        fvs = arA[0:NH, 2688:2688 + FVW]
        wadaS = [cvA(4096, 1024).rearrange("p (a b) -> p a b", a=8), cvA(5120, 1024).rearrange("p (a b) -> p a b", a=8)]
        lam_in = cvA(6144, 256).rearrange("p (a b) -> p a b", a=4)
        lamt = cvA(6400, 256).rearrange("p (a b) -> p a b", a=4)
        sil = cvB(0, 1024).rearrange("p (a b) -> p a b", a=2)
        aT = cvB(1024, 512, BF16).rearrange("p (a b) -> p a b", a=2)
        scr = cvB(1536, 256)
        bia = cvB(1792, 256)
        msk = cvB(2048, 256)
        Wtok = [cvB(2304, 256), cvB(2560, 256)]
        selT2 = [cvB(2816, 256), cvB(4736, 256)]
        posS = [cvB(3072, 256), cvB(3328, 256)]
        m8 = cvB(3584, 64).rearrange("p (a b) -> p a b", a=8)
        gsc = cvB(3648, 8)
        gm8 = cvB(3656, 8)
        gmk = cvB(3664, 8)
        t8 = cvB(3672, 8)
        den = cvB(3680, 2)
        h2tm = [cvB(3712, 512, BF16), cvB(4224, 512, BF16)]
        pP = cvB(0, LP)
        pW = [cvB(2080, LP), cvB(4160, LP)]
        mixT = cvB(6240, 2048, BF16).rearrange("p (a b) -> p a b", a=2)
        nbf = cvB(0, 256)
        nbi = cvB(256, 256, I32)
        sbase = cvB(512, 256)
        nbT = [cvB(768, 128), cvB(896, 128)]
        bend = cvB(1024, 2)
        Cm = [cvB(1032, NBLK), cvB(1032 + NBLK, NBLK)]
        bef = cvB(1032 + 2 * NBLK, NBLK)
        o_b = 1032 + 3 * NBLK
        NPB = 4
        NRB = 8
        posL = [cvB(o_b + 256 * i, 256) for i in range(NPB)]
        wtL = [cvB(o_b + 256 * NPB + 256 * i, 256) for i in range(NPB)]
        o_c = o_b + 512 * NPB
        dp1 = cvB(o_c, 256)
        keyb = cvB(o_c + 256, 256)
        junkb = cvB(o_c + 512, 256)
        d8 = cvB(o_c + 768, 8)
        rows = [cvB(o_c + 776 + 16 * i, 16).rearrange("p (a b) -> p a b", a=8) for i in range(NRB)]
        zslot = cvB(o_c + 776 + 16 * NRB, NSLOT * 2 // 128)
        assert o_c + 776 + 16 * NRB + NSLOT * 2 // 128 <= 8320
        wgC = [cvA(3072 * i, 1024, BF16) for i in range(3)]
        wuC = [cvA(3072 * i + 1024, 1024, BF16) for i in range(3)]
        wdC = [cvA(3072 * i + 2048, 1024, BF16).rearrange("p (a b) -> p a b", a=2) for i in range(3)]
        xgC = [cvA(9216 + 512 * i, 512, BF16) for i in range(3)]
        xgT = [cvA(10752 + 512 * i, 512, BF16).rearrange("p (a b) -> p a b", a=8) for i in range(2)]
        silC = [cvA(11776, 256), cvA(12032, 256)]
        aTC = [cvA(12288, 128, BF16).rearrange("p (a b) -> p a b", a=2), cvA(12416, 128, BF16).rearrange("p (a b) -> p a b", a=2)]
        ysb = [cvA(12544, 512, BF16), cvA(13056, 512, BF16)]
        wA = cvA(13568, NBLK)
        tokuA = cvA(13568 + NBLK, NBLK, U32)
        sl6 = [cvA(13568 + 2 * NBLK, 256), cvA(13568 + 2 * NBLK + 256, 256)]
        sl6c = [cvA(13568 + 2 * NBLK + 512, 128), cvA(13568 + 2 * NBLK + 640, 128)]
        assert 13568 + 2 * NBLK + 768 <= 16384 and NBLK % 128 == 0
        NYG = 4
        yg = [cvA(4096 * i, 4096, BF16).rearrange("p (a b) -> p a b", a=8) for i in range(NYG)]
        accD = [cvB(4096, 1024), cvB(5120, 1024)]

        hT = sb("hT", [128, 8, S], BF16)
        tmpF = sb("tmpF", [128, 8, CW])
        xch = sb("xch", [128, 8, CW])
        sqc = [sb("sqc%d" % i, [128, CW]) for i in range(2)]
        rstd = sb("rstd", [128, CW])
        modT = sb("modT", [128, 48, NSEQ])
        bada = sb("bada", [128, 48])
        n1g = sb("n1g_s", [128, 8])
        n2g = sb("n2g_s", [128, 8])
        A1 = sb("A1", [128, 8])
        A2 = sb("A2", [128, 8])
        cT = sb("cT_s", [128, 8, NSEQ])
        scT = sb("scT", [128, 8, NSEQ])
        winS = [sb("win%d" % i, [128, 8, 128], BF16) for i in range(2)]
        qkg = sb("qkg_s", [128, 2])
        lamv = sb("lamv", [128, 4])
        nlam = sb("nlam", [128, 1])
        sg = sb("sg_s", [128, 128])
        pwS = sb("pw_s", [128, 2, 2, 128], BF16)
        psc = sb("psc_s", [128, 8])
        rc = sb("rc_s", [128, 4, 16])
        woS = [sb("wo%d" % i, [128, 8, 128], BF16) for i in range(2)]
        wr = sb("wr_s", [128, 8, NE])
        wsg = sb("wsg", [128, 8, 256], BF16)
        wsu = sb("wsu", [128, 8, 256], BF16)
        wsd = sb("wsd", [128, 2, 1024], BF16)
        base = sb("base", [128, NE])
        lsm = sb("lsm", [128, 128])
        idtb = sb("idtb", [128, 128], BF16)
        pio8 = sb("pio8", [128, 8])
        dstu = sb("dstu", [128, NT, 8], U32)
        idxW = sb("idxW", [128, NBLK], U32)
        brt = sb("br_s", [128, NE])
        tab = sb("tab_s", [NB, NH])
        idt = sb("idt", [128, 128])
        aid = sb("aid", [128, 128])
        bones = sb("bones", [128, 128])
        ones = sb("ones", [128, 128])
        sqh2 = [sb("sqh%d" % i, [128, 512]) for i in range(2)]
        rsh2 = [sb("rsh%d" % i, [128, 512]) for i in range(2)]
        ET = [sb("ET%d" % i, [128, 512], BF16) for i in range(3)]
        rr = sb("rr", [128, 8])
        att4 = sb("att4", [128, 4, 128])
        att4b = sb("att4b", [128, 4, 128])
        ssq4 = sb("ssq4", [128, 8])
        epsc = sb("epsc", [128, 2])
        gpT = sb("gpT", [128, 512])
        ptmp = sb("ptmp", [128, 512])

        pb = [es.enter_context(nc.psum_tensor("pb%d" % i, [128, 512], F32)) for i in range(8)]
        rpb = [Res() for _ in range(8)]

        R = {}

        def res(name):
            if name not in R:
                R[name] = Res()
            return R[name]

        rbigA = [Res() for _ in range(4)]
        rhT = [Res() for _ in range(8)]
        rmT = [[Res() for _ in range(4)] for _ in range(8)]

        def load(dst, src, name, eng="sp"):
            sc.dma(eng, "ld_" + name, dst, src, writes=[res(name)])

        load(bada[:], bada_d, "bada")
        load(n1g[:], n1g_d, "n1g")
        load(n2g[:], n2g_d, "n2g")
        load(cT[:], cT_d, "cT")
        load(qkg[:], qkg_d, "qkg")
        load(lam_in[:], lam_d, "lam_in")
        load(sg[:], sg_d, "sg")
        load(psc[:], psc_d, "psc")
        load(rc[:], rc_d, "rc")
        load(wr[:], wr_d, "wr")
        load(brt[:], br_d, "brt")
        load(tab[:], tab_d, "tab")
        load(oh[:], oh_d, "oh")
        load(idt[:], idt_d, "idt")
        load(aid[:], aid_d, "aid")
        load(bones[:], bones_d, "bones")
        load(lsm[:], ls_d, "lsm")
        load(pio8[:], pio_d, "pio8")
        sc.dma("pool", "ld_wsg", wsg[:], wg_d[NE], writes=[res("wsg")])
        sc.dma("pool", "ld_wsu", wsu[:], wu_d[NE], writes=[res("wsu")])
        sc.dma("pool", "ld_wsd", wsd[:], wd_d[NE], writes=[res("wsd")])
        sc.op("dve", lambda e: e.memset(base[:], 0.0), writes=[res("base")])
        sc.op("dve", lambda e: e.tensor_copy(out=idtb[:], in_=idt[:]), reads=[res("idt")], writes=[res("idtb")])
        sc.op("dve", lambda e: e.memset(ones[:], 1.0), writes=[res("ones")])
        sc.op("dve", lambda e: e.memset(epsc[:, 0:1], float(EPS)), writes=[res("epsc")])
        sc.op("dve", lambda e: e.memset(epsc[:, 1:2], float(64 * EPS)), reads=[res("epsc")], writes=[res("epsc")])
        sc.op("dve", lambda e: e.tensor_scalar(sg[:], sg[:], float(1.0 - LAM_INIT), None, ALU.mult),
              reads=[res("sg")], writes=[res("sg")])
        sc.op("dve", lambda e: e.tensor_tensor(lamt[:, 0, :], lam_in[:, 0, :], lam_in[:, 1, :], ALU.mult),
              reads=[res("lam_in")], writes=[res("lamt")])
        sc.op("dve", lambda e: e.tensor_tensor(lamt[:, 1, :], lam_in[:, 2, :], lam_in[:, 3, :], ALU.mult),
              reads=[res("lam_in"), res("lamt")], writes=[res("lamt")])
        sc.op("dve", lambda e: e.tensor_reduce(lamv[:, 0:2], lamt[:, 0:2, :], AX.X, ALU.add),
              reads=[res("lamt")], writes=[res("lamv")])
        sc.op("act", lambda e: e.activation(lamv[:, 2:4], lamv[:, 0:2], AF.Exp),
              reads=[res("lamv")], writes=[res("lamv")])
        sc.op("dve", lambda e: e.scalar_tensor_tensor(nlam[:], lamv[:, 3:4], float(-LAM_INIT), lamv[:, 2:3],
                                                      ALU.add, ALU.subtract),
              reads=[res("lamv")], writes=[res("nlam")])

        sc.op("act", lambda e: e.activation(scT[:], cT[:], AF.Silu), reads=[res("cT")], writes=[res("scT")])
        for fc in range(48):
            wb = wadaS[fc % 2]
            rw = res("wada%d" % (fc % 2))
            sc.dma("sp", "ld_wada%d" % (fc % 2), wb[:], wada_d[fc], writes=[rw])
            for kc in range(8):
                sc.op("pe", lambda e, wb=wb, kc=kc: e.matmul(pb[7][:, 0:NSEQ], lhsT=wb[:, kc, :], rhs=scT[:, kc, :],
                                                              start=(kc == 0), stop=(kc == 7)),
                      reads=[rw, res("scT")], writes=[rpb[7]])
            sc.op("dve", lambda e, fc=fc: e.tensor_scalar(modT[:, fc, :], pb[7][:, 0:NSEQ], bada[:, fc:fc + 1], None,
                                                           ALU.add),
                  reads=[rpb[7], res("bada")], writes=[res("modT")])

        for c0 in range(0, FVW, 512):
            cw = min(512, FVW - c0)
            sc.op("pe", lambda e, c0=c0, cw=cw: e.matmul(pb[7][0:NH, 0:cw], lhsT=tab[:, :], rhs=oh[:, c0:c0 + cw],
                                                          start=True, stop=True),
                  reads=[res("tab"), res("oh")], writes=[rpb[7]])
            sc.op("dve", lambda e, c0=c0, cw=cw: e.tensor_copy(out=fvs[:, c0:c0 + cw], in_=pb[7][0:NH, 0:cw]),
                  reads=[rpb[7]], writes=[res("fvs")])
        sc.dma("sp", "st_fv", fv_d, fvs[:], reads=[res("fvs")], writes=[res("fv_d")])
        for h in range(NH):
            src = bass.AP(fv_d.tensor, h * FVW, [[1, 128], [1, MW]])
            sc.dma("sp", "ld_hank", hank[:], src, reads=[res("fv_d")], writes=[res("hank")])
            for c0 in range(0, MW, 512):
                cw = min(512, MW - c0)
                sc.op("pe", lambda e, c0=c0, cw=cw: e.matmul(pb[7][:, 0:cw], lhsT=aid[:], rhs=hank[:, c0:c0 + cw],
                                                              start=True, stop=True),
                      reads=[res("aid"), res("hank")], writes=[rpb[7]])
                sc.op("dve", lambda e, c0=c0, cw=cw: e.tensor_copy(out=mst[:, c0:c0 + cw], in_=pb[7][:, 0:cw]),
                      reads=[rpb[7]], writes=[res("mst")])
            sc.dma("sp", "st_mst", mst_d[h], mst[:], reads=[res("mst")], writes=[res("mst_d")])

        win_ctr = [0]

        def load_win(chunk):
            i = win_ctr[0] % 2
            win_ctr[0] += 1
            sc.dma("pool", "ld_win%d" % i, winS[i][:], win_d[chunk], writes=[res("win%d" % i)])
            return winS[i], res("win%d" % i)

        def rmsnorm_chunk(Acol, shcol0, s, ts, rdst, f32_out):
            for c in range(8):
                q = sqc[c % 2]
                rq = res("sqc%d" % (c % 2))
                sc.op("act", lambda e, c=c, q=q: e.activation(q[:], xch[:, c, :], AF.Square),
                      reads=[res("xch")], writes=[rq])
                sc.op("pe", lambda e, c=c, q=q: e.matmul(pb[7][:, 0:CW], lhsT=ones[:], rhs=q[:],
                                                         start=(c == 0), stop=(c == 7)),
                      reads=[res("ones"), rq], writes=[rpb[7]])
            sc.op("act", lambda e: e.activation(rstd[:], pb[7][:, 0:CW], AF.Sqrt, bias=float(EPS), scale=1.0 / D),
                  reads=[rpb[7]], writes=[res("rstd")])
            sc.op("dve", lambda e: e.reciprocal(rstd[:], rstd[:]), reads=[res("rstd")], writes=[res("rstd")])
            for c in range(8):
                sc.op("dve", lambda e, c=c: e.scalar_tensor_tensor(
                    tmpF[:, c, :], xch[:, c, :], Acol[:, c:c + 1], rstd[:], ALU.mult, ALU.mult),
                    reads=[res("xch"), res("rstd"), res("Acol")], writes=[res("tmpF%d" % c)])
                if not f32_out:
                    sc.op("act", lambda e, c=c: e.activation(
                        hT[:, c, ts], tmpF[:, c, :], AF.Identity, bias=modT[:, shcol0 + c, s:s + 1], scale=1.0),
                        reads=[res("tmpF%d" % c), res("modT")], writes=[rdst])
                else:
                    sc.op("act", lambda e, c=c: e.activation(
                        tmpF[:, c, :], tmpF[:, c, :], AF.Identity, bias=modT[:, shcol0 + c, s:s + 1], scale=1.0),
                        reads=[res("tmpF%d" % c), res("modT")], writes=[res("tmpF%d" % c)])
                    sc.op("dve", lambda e, c=c: e.tensor_copy(out=hT[:, c, ts], in_=tmpF[:, c, :]),
                          reads=[res("tmpF%d" % c)], writes=[rdst])

        rtmpF = [res("tmpF%d" % c) for c in range(8)]

        for s in range(NSEQ):
            sc.fence()
            sc.op("dve", lambda e: e.memset(vA[:], 1.0), writes=[res("vA")])
            sc.op("dve", lambda e: e.memset(qz[0][64:128, :], 0.0), writes=[res("qT")])
            sc.op("dve", lambda e: e.memset(qz[1][0:64, :], 0.0), writes=[res("qT")])
            sc.op("dve", lambda e, s=s: e.scalar_tensor_tensor(A1[:], modT[:, 8:16, s], 1.0, n1g[:], ALU.add, ALU.mult),
                  reads=[res("modT"), res("n1g")], writes=[res("Acol")])
            for j in range(8):
                ts = slice(j * CW, (j + 1) * CW)
                sc.dma("sp", "ld_x", xch[:], xT_d[s][:, :, ts], writes=[res("xch")])
                rmsnorm_chunk(A1, 0, s, ts, rhT[j], False)

            for h in range(NH):
                sc.dma("sp", "ld_mst", mst[:], mst_d[h], reads=[res("mst_d")], writes=[res("mst")])
                for which, dstT, rname in ((0, None, "qT"), (1, kT, "kT")):
                    wb, rw = load_win(which * 8 + h)
                    for j in range(4):
                        ts = slice(j * 512, (j + 1) * 512)
                        pa = 4 + 2 * (j % 2)
                        pn = pa + 1
                        sq_, rs_ = sqh2[j % 2], rsh2[j % 2]
                        rsq, rrs = res("sqh%d" % (j % 2)), res("rsh%d" % (j % 2))
                        for kc in range(8):
                            sc.op("pe", lambda e, wb=wb, kc=kc, ts=ts, pa=pa: e.matmul(
                                pb[pa][:], lhsT=wb[:, kc, :], rhs=hT[:, kc, ts], start=(kc == 0), stop=(kc == 7)),
                                reads=[rw, rhT[2 * j], rhT[2 * j + 1]], writes=[rpb[pa]])
                        sc.op("act", lambda e, pa=pa, sq_=sq_: e.activation(sq_[:], pb[pa][:], AF.Square),
                              reads=[rpb[pa]], writes=[rsq])
                        sc.op("pe", lambda e, pn=pn, sq_=sq_: e.matmul(pb[pn][:], lhsT=bones[:], rhs=sq_[:], start=True, stop=True),
                              reads=[res("bones"), rsq], writes=[rpb[pn]])
                        if which == 0:
                            sc.op("act", lambda e, pn=pn, rs_=rs_: e.activation(rs_[:], pb[pn][:], AF.Ln, bias=epsc[:, 1:2], scale=1.0),
                                  reads=[rpb[pn], res("epsc")], writes=[rrs])
                        else:
                            sc.op("act", lambda e, pn=pn, rs_=rs_: e.activation(rs_[:], pb[pn][:], AF.Ln, bias=epsc[:, 0:1], scale=1.0 / 64),
                                  reads=[rpb[pn], res("epsc")], writes=[rrs])
                        sc.op("act", lambda e, rs_=rs_: e.activation(rs_[:], rs_[:], AF.Exp, scale=-0.5), reads=[rrs], writes=[rrs])
                        if which == 1:
                            sc.op("dve", lambda e, dstT=dstT, ts=ts, which=which, pa=pa, rs_=rs_: e.scalar_tensor_tensor(
                                dstT[:, ts], pb[pa][:], qkg[:, which:which + 1], rs_[:], ALU.mult, ALU.mult),
                                reads=[rpb[pa], rrs, res("qkg")], writes=[res(rname)])
                        else:
                            for comp in range(2):
                                pr = slice(comp * 64, (comp + 1) * 64)
                                sc.op("dve", lambda e, ts=ts, pa=pa, rs_=rs_, comp=comp, pr=pr: e.scalar_tensor_tensor(
                                    qz[comp][pr, ts], pb[pa][pr, :], qkg[pr, 0:1], rs_[pr, :], ALU.mult, ALU.mult),
                                    reads=[rpb[pa], rrs, res("qkg")], writes=[res(rname)])
                wb, rw = load_win(16 + h)
                for t in range(16):
                    tt = slice(t * 128, (t + 1) * 128)
                    vb = 4 + (t % 4)
                    for kc in range(8):
                        sc.op("pe", lambda e, wb=wb, kc=kc, tt=tt, vb=vb: e.matmul(
                            pb[vb][:, 0:128], lhsT=hT[:, kc, tt], rhs=wb[:, kc, :], start=(kc == 0), stop=(kc == 7)),
                            reads=[rw, rhT[t // 2]], writes=[rpb[vb]])
                    if t % 2 == 0:
                        sc.op("act", lambda e, t=t, vb=vb: e.activation(vA[:, t, 0:128], pb[vb][:, 0:128], AF.Copy),
                              reads=[rpb[vb]], writes=[res("vA")])
                    else:
                        sc.op("dve", lambda e, t=t, vb=vb: e.tensor_copy(out=vA[:, t, 0:128], in_=pb[vb][:, 0:128]),
                              reads=[rpb[vb]], writes=[res("vA")])
                wb, rw = load_win(32 + h)
                for j in range(4):
                    ts = slice(j * 512, (j + 1) * 512)
                    gb = 4 + j
                    for kc in range(8):
                        sc.op("pe", lambda e, wb=wb, kc=kc, ts=ts, gb=gb: e.matmul(
                            pb[gb][:], lhsT=wb[:, kc, :], rhs=hT[:, kc, ts], start=(kc == 0), stop=(kc == 7)),
                            reads=[rw, rhT[2 * j], rhT[2 * j + 1]], writes=[rpb[gb]])
                    sc.op("act", lambda e, ts=ts, gb=gb: e.activation(gaT[:, ts], pb[gb][:], AF.Sigmoid),
                          reads=[rpb[gb]], writes=[res("gaT")])
                tiles = [(j, comp, kb) for j in range(4) for comp in range(2) for kb in range(16)]

                def emit_st(n):
                    j, comp, kb = tiles[n]
                    bi = 4 + (n % 3)
                    ks = slice(kb * 128, (kb + 1) * 128)
                    qs = slice(j * 512, (j + 1) * 512)
                    sc.op("pe", lambda e, bi=bi, comp=comp, ks=ks, qs=qs: e.matmul(
                        pb[bi][:], lhsT=kT[:, ks], rhs=qz[comp][:, qs], start=True, stop=True),
                        reads=[res("qT"), res("kT")], writes=[rpb[bi]])

                emit_st(0)
                emit_st(1)
                for n, (j, comp, kb) in enumerate(tiles):
                    if n + 2 < len(tiles):
                        emit_st(n + 2)
                    bi = 4 + (n % 3)
                    ei = n % 3
                    o = kb - 4 * j
                    rET = res("ET%d" % ei)
                    if o <= -2 or o >= 5:
                        col = (MW - 1) if o <= -2 else 0
                        sc.op("act", lambda e, bi=bi, ei=ei, col=col: e.activation(
                            ET[ei][:], pb[bi][:], AF.Exp, bias=mst[:, col:col + 1], scale=1.0),
                            reads=[rpb[bi], res("mst")], writes=[rET])
                    else:
                        m0 = 640 - 128 * o
                        si = n % 2
                        rsa = res("sadd%d" % si)
                        sc.op("dve", lambda e, bi=bi, si=si, m0=m0: e.tensor_tensor(
                            sadd[si][:], pb[bi][:], mst[:, m0:m0 + 512], ALU.add),
                            reads=[rpb[bi], res("mst")], writes=[rsa])
                        sc.op("act", lambda e, ei=ei, si=si: e.activation(ET[ei][:], sadd[si][:], AF.Exp),
                              reads=[rsa], writes=[rET])
                    for sub in range(4):
                        sc.op("pe", lambda e, sub=sub, ei=ei, kb=kb: e.matmul(
                            pb[sub][:, 0:129], lhsT=ET[ei][:, sub * 128:(sub + 1) * 128], rhs=vA[:, kb, 0:129],
                            start=(kb == 0), stop=(kb == 15)),
                            reads=[rET, res("vA")], writes=[rpb[sub]])
                    if kb == 15:
                        for sub in range(4):
                            eng = "act" if sub % 2 == 0 else "dve"
                            if eng == "act":
                                sc.op("act", lambda e, sub=sub, comp=comp: e.activation(
                                    O[comp][:, sub, 0:129], pb[sub][:, 0:129], AF.Copy),
                                    reads=[rpb[sub]], writes=[res("O%d" % comp)])
                            else:
                                sc.op("dve", lambda e, sub=sub, comp=comp: e.tensor_copy(
                                    out=O[comp][:, sub, 0:129], in_=pb[sub][:, 0:129]),
                                    reads=[rpb[sub]], writes=[res("O%d" % comp)])
                    if kb == 15 and comp == 1:
                        ts = slice(j * 512, (j + 1) * 512)
                        sc.op("dve", lambda e: e.reciprocal(rr[:, 0:4], O[0][:, :, 128]),
                              reads=[res("O0")], writes=[res("rr")])
                        sc.op("dve", lambda e: e.reciprocal(rr[:, 4:8], O[1][:, :, 128]),
                              reads=[res("O1"), res("rr")], writes=[res("rr")])
                        sc.op("dve", lambda e: e.tensor_scalar(rr[:, 4:8], rr[:, 4:8], nlam[:, 0:1], None, ALU.mult),
                              reads=[res("rr"), res("nlam")], writes=[res("rr")])
                        sc.op("dve", lambda e: e.tensor_tensor(
                            att4[:, :, :], O[0][:, :, 0:128], rr[:, 0:4].unsqueeze(2).to_broadcast([128, 4, 128]), ALU.mult),
                            reads=[res("O0"), res("rr")], writes=[res("att4")])
                        sc.op("dve", lambda e: e.tensor_tensor(
                            att4b[:, :, :], O[1][:, :, 0:128], rr[:, 4:8].unsqueeze(2).to_broadcast([128, 4, 128]), ALU.mult),
                            reads=[res("O1"), res("rr")], writes=[res("att4b")])
                        sc.op("dve", lambda e: e.tensor_tensor(att4[:, :, :], att4[:, :, :], att4b[:, :, :], ALU.add),
                              reads=[res("att4"), res("att4b")], writes=[res("att4")])
                        sc.op("dve", lambda e: e.tensor_tensor(att4b[:, :, :], att4[:, :, :], att4[:, :, :], ALU.mult),
                              reads=[res("att4")], writes=[res("att4b")])
                        sc.op("dve", lambda e: e.tensor_reduce(ssq4[:, 0:4], att4b[:, :, :], AX.X, ALU.add),
                              reads=[res("att4b")], writes=[res("ssq4")])
                        sc.op("act", lambda e: e.activation(ssq4[:, 4:8], ssq4[:, 0:4], AF.Ln, bias=epsc[:, 0:1], scale=1.0 / 128),
                              reads=[res("ssq4"), res("epsc")], writes=[res("ssq4")])
                        sc.op("act", lambda e: e.activation(ssq4[:, 4:8], ssq4[:, 4:8], AF.Exp, scale=-0.5),
                              reads=[res("ssq4")], writes=[res("ssq4")])
                        sc.op("dve", lambda e: e.tensor_tensor(
                            att4[:, :, :], att4[:, :, :], ssq4[:, 4:8].unsqueeze(2).to_broadcast([128, 4, 128]), ALU.mult),
                            reads=[res("att4"), res("ssq4")], writes=[res("att4")])
                        sc.op("dve", lambda e: e.tensor_tensor(
                            att4[:, :, :], att4[:, :, :], sg[:, :].unsqueeze(1).to_broadcast([128, 4, 128]), ALU.mult),
                            reads=[res("att4"), res("sg")], writes=[res("att4")])
                        for sub in range(4):
                            sc.op("pe", lambda e, sub=sub: e.transpose(pb[7][:, sub * 128:(sub + 1) * 128], att4[:, sub, :], idt[:]),
                                  reads=[res("att4"), res("idt")], writes=[rpb[7]])
                        sc.op("dve", lambda e, ts=ts, h=h: e.tensor_tensor(mT[:, h, ts], pb[7][:, :], gaT[:, ts], ALU.mult),
                              reads=[rpb[7], res("gaT")], writes=[rmT[h][j]])

            sc.fence()
            sc.op("dve", lambda e: e.memset(pP[:], 0.0), writes=[res("pP")])
            for g in range(4):
                wnd = (2, 4, 8, 16)[g]
                for dc in range(2):
                    sc.dma("pool", "ld_pw", pwS[:, dc, :, :], pw_d[g, dc], writes=[res("pw")])
                for cc in range(2):
                    chunk = 2 * g + cc
                    wb, rw = load_win(24 + chunk)
                    for j in range(4):
                        ts = slice(j * 512, (j + 1) * 512)
                        for kc in range(8):
                            sc.op("pe", lambda e, wb=wb, kc=kc, ts=ts: e.matmul(
                                pb[6][:], lhsT=wb[:, kc, :], rhs=hT[:, kc, ts], start=(kc == 0), stop=(kc == 7)),
                                reads=[rw, rhT[2 * j], rhT[2 * j + 1]], writes=[rpb[6]])
                        sc.op("act", lambda e, j=j: e.activation(pP[:, PADL + j * 512:PADL + (j + 1) * 512], pb[6][:],
                                                                 AF.Copy),
                              reads=[rpb[6]], writes=[res("pP")])
                    L = LP
                    sc.op("dve", lambda e: e.tensor_tensor(pW[0][:, 1:L], pP[:, 0:L - 1], pP[:, 1:L], ALU.add),
                          reads=[res("pP")], writes=[res("pW0")])
                    cur = 0
                    for lvl, sh in ((4, 1), (8, 2), (16, 4)):
                        if wnd < lvl:
                            break
                        nxt = 1 - cur
                        sc.op("dve", lambda e, cur=cur, nxt=nxt, sh=sh: e.tensor_tensor(
                            pW[nxt][:, sh:L - sh], pW[cur][:, 0:L - 2 * sh], pW[cur][:, 2 * sh:L], ALU.add),
                            reads=[res("pW%d" % cur)], writes=[res("pW%d" % nxt)])
                        cur = nxt
                    Wc = pW[cur]
                    rWc = res("pW%d" % cur)
                    sc.op("dve", lambda e, Wc=Wc, cc=cc, wnd=wnd: e.scalar_tensor_tensor(
                        mixT[:, cc, :], Wc[:, PADL:PADL + S], 1.0 / wnd, pP[:, PADL:PADL + S], ALU.mult, ALU.subtract),
                        reads=[rWc, res("pP")], writes=[res("mixT")])
                    for (c0, r0) in ((0, 0), (S - 8, 8)):
                        sc.op("dve", lambda e, Wc=Wc, c0=c0, r0=r0, g=g: e.tensor_tensor(
                            ptmp[:, 0:8], Wc[:, PADL + c0:PADL + c0 + 8], rc[:, g, r0:r0 + 8], ALU.mult),
                            reads=[rWc, res("rc")], writes=[res("ptmp")])
                        sc.op("dve", lambda e, c0=c0, cc=cc: e.tensor_tensor(
                            mixT[:, cc, c0:c0 + 8], ptmp[:, 0:8], pP[:, PADL + c0:PADL + c0 + 8], ALU.subtract),
                            reads=[res("ptmp"), res("pP"), res("mixT")], writes=[res("mixT")])
                for dc in range(2):
                    chunk = 2 * g + dc
                    wb, rw = load_win(40 + chunk)
                    for j in range(4):
                        ts = slice(j * 512, (j + 1) * 512)
                        for kc in range(8):
                            sc.op("pe", lambda e, wb=wb, kc=kc, ts=ts: e.matmul(
                                pb[6][:], lhsT=wb[:, kc, :], rhs=hT[:, kc, ts], start=(kc == 0), stop=(kc == 7)),
                                reads=[rw, rhT[2 * j], rhT[2 * j + 1]], writes=[rpb[6]])
                        sc.op("act", lambda e: e.activation(gpT[:], pb[6][:], AF.Sigmoid),
                              reads=[rpb[6]], writes=[res("gpT")])
                        for cc in range(2):
                            sc.op("pe", lambda e, dc=dc, cc=cc, ts=ts: e.matmul(
                                pb[5][:], lhsT=pwS[:, dc, cc, :], rhs=mixT[:, cc, ts], start=(cc == 0), stop=(cc == 1)),
                                reads=[res("pw"), res("mixT")], writes=[rpb[5]])
                        sc.op("dve", lambda e, chunk=chunk: e.scalar_tensor_tensor(
                            ptmp[:], pb[5][:], psc[:, chunk:chunk + 1], gpT[:], ALU.mult, ALU.mult),
                            reads=[rpb[5], res("gpT"), res("psc")], writes=[res("ptmp")])
                        sc.op("dve", lambda e, chunk=chunk, ts=ts: e.tensor_tensor(
                            mT[:, chunk, ts], mT[:, chunk, ts], ptmp[:], ALU.add),
                            reads=[res("ptmp"), rmT[chunk][j]], writes=[rmT[chunk][j]])

            sc.fence()
            sc.op("dve", lambda e, s=s: e.scalar_tensor_tensor(A2[:], modT[:, 32:40, s], 1.0, n2g[:], ALU.add, ALU.mult),
                  reads=[res("modT"), res("n2g")], writes=[res("Acol")])

            def router(j, part):
                for sub in range(CW // 128):
                    gt = s * 16 + (CW // 128) * j + sub
                    Wt = Wtok[gt % 2]
                    rWt = res("Wtok%d" % (gt % 2))
                    pS = posS[gt % 2]
                    rpS = res("posS%d" % (gt % 2))
                    if part == "b":
                        sc.op("pe", lambda e, sub=sub: e.matmul(pb[5][:, 0:NE], lhsT=lsm[:], rhs=selT2[sub][:], start=True, stop=True),
                              reads=[res("lsm"), res("selT%d" % sub)], writes=[rpb[5]])
                        sc.op("dve", lambda e, pS=pS: e.tensor_tensor(pS[:], pb[5][:, 0:NE], base[:], ALU.add),
                              reads=[rpb[5], res("base")], writes=[rpS])
                        sc.op("pe", lambda e, sub=sub: e.matmul(pb[4][:, 0:NE], lhsT=ones[:], rhs=selT2[sub][:], start=True, stop=True),
                              reads=[res("ones"), res("selT%d" % sub)], writes=[rpb[4]])
                        sc.op("dve", lambda e: e.tensor_tensor(base[:], pb[4][:, 0:NE], base[:], ALU.add),
                              reads=[rpb[4], res("base")], writes=[res("base")])
                        sc.dma("sp", "st_pos", pos_d[gt], pS[:], reads=[rpS], writes=[])
                        sc.dma("sp", "st_wt", wt_d[gt], Wt[:], reads=[rWt], writes=[])
                        continue
                    if part == "h":
                        hb = h2tm[gt % 2]
                        rhb = res("h2tm%d" % (gt % 2))
                        tl = slice((CW * j) + sub * 128, (CW * j) + (sub + 1) * 128)
                        pbt = pb[7][:, :].bitcast(BF16)
                        for kc in range(8):
                            sc.op("pe", lambda e, kc=kc, tl=tl, pbt=pbt: e.transpose(pbt[:, kc * 128:(kc + 1) * 128], hT[:, kc, tl], idtb[:]),
                                  reads=[rhT[j], res("idtb")], writes=[rpb[7]])
                        sc.op("act", lambda e, hb=hb, pbt=pbt: e.activation(hb[:], pbt[:, :], AF.Copy),
                              reads=[rpb[7]], writes=[rhb])
                        sc.dma("sp", "st_h2", h2_d[gt * 128:(gt + 1) * 128, :], hb[:], reads=[rhb], writes=[])
                        continue
                    for kc in range(8):
                        sc.op("pe", lambda e, kc=kc, sub=sub: e.matmul(
                            pb[6][:, 0:NE], lhsT=tmpF[:, kc, sub * 128:(sub + 1) * 128], rhs=wr[:, kc, :],
                            start=(kc == 0), stop=(kc == 7)),
                            reads=[rtmpF[kc], res("wr")], writes=[rpb[6]])
                    sc.op("act", lambda e: e.activation(scr[:], pb[6][:, 0:NE], AF.Sigmoid),
                          reads=[rpb[6]], writes=[res("scr")])
                    sc.op("dve", lambda e: e.tensor_tensor(bia[:], scr[:], brt[:], ALU.add),
                          reads=[res("scr"), res("brt")], writes=[res("bia")])
                    for gi in range(8):
                        sc.op("dve", lambda e, gi=gi: e.max(out=m8[:, gi, :], in_=bia[:, gi * 32:(gi + 1) * 32]),
                              reads=[res("bia")], writes=[res("m8")])
                    sc.op("dve", lambda e: e.tensor_tensor(gsc[:], m8[:, :, 0], m8[:, :, 1], ALU.add),
                          reads=[res("m8")], writes=[res("gsc")])
                    sc.op("dve", lambda e: e.max(out=gm8[:], in_=gsc[:]), reads=[res("gsc")], writes=[res("gm8")])
                    sc.op("dve", lambda e: e.tensor_scalar(gmk[:], gsc[:], gm8[:, 3:4], None, ALU.is_ge),
                          reads=[res("gsc"), res("gm8")], writes=[res("gmk")])
                    sc.op("dve", lambda e: e.tensor_scalar(msk[:], bia[:], 2.0, None, ALU.add),
                          reads=[res("bia")], writes=[res("msk")])
                    sc.op("dve", lambda e: e.tensor_tensor(
                        msk[:, :].rearrange("p (g k) -> p g k", g=8), msk[:, :].rearrange("p (g k) -> p g k", g=8),
                        gmk[:, :].unsqueeze(2).to_broadcast([128, 8, 32]), ALU.mult),
                        reads=[res("msk"), res("gmk")], writes=[res("msk")])
                    sc.op("dve", lambda e: e.max(out=t8[:], in_=msk[:]), reads=[res("msk")], writes=[res("t8")])
                    sc.op("dve", lambda e, Wt=Wt: e.scalar_tensor_tensor(
                        Wt[:], msk[:], t8[:, 7:8], scr[:], ALU.is_ge, ALU.mult),
                        reads=[res("msk"), res("t8"), res("scr")], writes=[rWt])
                    sc.op("dve", lambda e, Wt=Wt: e.tensor_reduce(den[:, 0:1], Wt[:], AX.X, ALU.add),
                          reads=[rWt], writes=[res("den")])
                    sc.op("dve", lambda e: e.reciprocal(den[:, 1:2], den[:, 0:1]),
                          reads=[res("den")], writes=[res("den")])
                    sc.op("dve", lambda e, Wt=Wt: e.tensor_scalar(Wt[:], Wt[:], den[:, 1:2], 2.5, ALU.mult, ALU.mult),
                          reads=[rWt, res("den")], writes=[rWt])
                    sc.op("dve", lambda e, Wt=Wt, sub=sub: e.tensor_scalar(selT2[sub][:], Wt[:], 0.0, None, ALU.is_gt),
                          reads=[rWt], writes=[res("selT%d" % sub)])

            for j in range(8):
                ts = slice(j * CW, (j + 1) * CW)
                sc.dma("sp", "ld_x", xch[:], xT_d[s][:, :, ts], writes=[res("xch")])
                for oc in range(8):
                    wb = woS[oc % 2]
                    rw = res("wo%d" % (oc % 2))
                    sc.dma("pool", "ld_wo%d" % (oc % 2), wb[:], wo_d[oc], writes=[rw])
                    for kc in range(8):
                        sc.op("pe", lambda e, wb=wb, kc=kc, ts=ts: e.matmul(
                            pb[4][:, 0:CW], lhsT=wb[:, kc, :], rhs=mT[:, kc, ts], start=(kc == 0), stop=(kc == 7)),
                            reads=[rw], writes=[rpb[4]])
                    sc.op("dve", lambda e, oc=oc, s=s: e.scalar_tensor_tensor(
                        xch[:, oc, :], pb[4][:, 0:CW], modT[:, 16 + oc, s:s + 1], xch[:, oc, :], ALU.mult, ALU.add),
                        reads=[rpb[4], res("modT"), res("xch")], writes=[res("xch")])
                rmsnorm_chunk(A2, 24, s, ts, rhT[j], True)
                router(j, "a")
                router(j, "h")
                for half in range(2):
                    hs = slice(half * 128, (half + 1) * 128)
                    for kc in range(8):
                        sc.op("pe", lambda e, kc=kc, hs=hs, ts=ts, half=half: e.matmul(
                            pb[half][:, 0:CW], lhsT=wsg[:, kc, hs], rhs=hT[:, kc, ts], start=(kc == 0), stop=(kc == 7)),
                            reads=[res("wsg"), rhT[j]], writes=[rpb[half]])
                    for kc in range(8):
                        sc.op("pe", lambda e, kc=kc, hs=hs, ts=ts, half=half: e.matmul(
                            pb[2 + half][:, 0:CW], lhsT=wsu[:, kc, hs], rhs=hT[:, kc, ts], start=(kc == 0), stop=(kc == 7)),
                            reads=[res("wsu"), rhT[j]], writes=[rpb[2 + half]])
                router(j, "b")
                for half in range(2):
                    sc.op("act", lambda e, half=half: e.activation(sil[:, half, 0:CW], pb[half][:, 0:CW], AF.Silu),
                          reads=[rpb[half]], writes=[res("sil%d" % half)])
                    sc.op("dve", lambda e, half=half: e.tensor_tensor(
                        aT[:, half, 0:CW], sil[:, half, 0:CW], pb[2 + half][:, 0:CW], ALU.mult),
                        reads=[res("sil%d" % half), rpb[2 + half]], writes=[res("aT")])
                for dcn in range(8):
                    bi = 5 + (dcn % 2)
                    for cc in range(2):
                        sc.op("pe", lambda e, cc=cc, dcn=dcn, bi=bi: e.matmul(
                            pb[bi][:, 0:CW], lhsT=wsd[:, cc, dcn * 128:(dcn + 1) * 128], rhs=aT[:, cc, 0:CW],
                            start=(cc == 0), stop=(cc == 1)),
                            reads=[res("wsd"), res("aT")], writes=[rpb[bi]])
                    sc.op("dve", lambda e, dcn=dcn, bi=bi, s=s: e.scalar_tensor_tensor(
                        xch[:, dcn, :], pb[bi][:, 0:CW], modT[:, 40 + dcn, s:s + 1], xch[:, dcn, :], ALU.mult, ALU.add),
                        reads=[rpb[bi], res("modT"), res("xch")], writes=[res("xch")])
                sc.dma("sp", "st_xs", xs_d[s][:, :, ts], xch[:], reads=[res("xch")], writes=[])

        sc.fence()
        us = [cvA(0, 256), cvA(256, 256)]
        ui = [cvA(512, 256), cvA(768, 256)]
        iot = cvA(1024, 1024)
        for c in range(2):
            sc.dma("sp", "ld_us", us[c][:], us_d[c], writes=[res("us%d" % c)])
            sc.dma("sp", "ld_ui", ui[c][:], ui_d[c], writes=[res("ui%d" % c)])
        sc.dma("sp", "ld_iot", iot[:], iot_d, writes=[res("iot")])
        sc.op("dve", lambda e: e.tensor_scalar(nbf[:], base[:], 127.0, None, ALU.add), reads=[res("base")], writes=[res("nbf")])
        sc.op("dve", lambda e: e.tensor_copy(out=nbi[:], in_=nbf[:]), reads=[res("nbf")], writes=[res("nbi")])
        sc.op("dve", lambda e: e.tensor_single_scalar(nbi[:], nbi[:], 7, ALU.arith_shift_right),
              reads=[res("nbi")], writes=[res("nbi")])
        sc.op("dve", lambda e: e.tensor_copy(out=nbf[:], in_=nbi[:]), reads=[res("nbi")], writes=[res("nbf")])
        for c in range(2):
            sc.op("pe", lambda e, c=c: e.transpose(pb[c][:, 0:128], nbf[:, c * 128:(c + 1) * 128], idt[:]),
                  reads=[res("nbf"), res("idt")], writes=[rpb[c]])
            sc.op("dve", lambda e, c=c: e.tensor_copy(out=nbT[c][:], in_=pb[c][:, 0:128]), reads=[rpb[c]], writes=[res("nbT%d" % c)])
        for c in range(2):
            sc.op("pe", lambda e, c=c: e.matmul(pb[2][:, 0:NE], lhsT=nbT[c][:], rhs=us[c][:], start=(c == 0), stop=(c == 1)),
                  reads=[res("nbT%d" % c), res("us%d" % c)], writes=[rpb[2]])
        sc.op("dve", lambda e: e.tensor_scalar(sbase[:], pb[2][:, 0:NE], 128.0, None, ALU.mult), reads=[rpb[2]], writes=[res("sbase")])
        for c2 in range(2):
            for c in range(2):
                sc.op("pe", lambda e, c=c, c2=c2: e.matmul(pb[3][:, 0:2], lhsT=ui[c][:, c2 * 128:(c2 + 1) * 128], rhs=nbT[c][:, 0:2],
                                                           start=(c == 0), stop=(c == 1)),
                      reads=[res("nbT%d" % c), res("ui%d" % c)], writes=[rpb[3]])
            sc.op("dve", lambda e, c2=c2: e.tensor_copy(out=bend[:, c2:c2 + 1], in_=pb[3][:, 0:1]), reads=[rpb[3]], writes=[res("bend")])
        for c2 in range(2):
            sc.op("dve", lambda e, c2=c2: e.tensor_scalar(Cm[c2][:], iot[:, 0:NBLK], bend[:, c2:c2 + 1], None, ALU.is_ge),
                  reads=[res("iot"), res("bend")], writes=[res("Cm%d" % c2)])
        for c0 in range(0, NBLK, 512):
            cw = min(512, NBLK - c0)
            for c2 in range(2):
                sc.op("pe", lambda e, c0=c0, cw=cw, c2=c2: e.matmul(pb[4][:, 0:cw], lhsT=ones[:], rhs=Cm[c2][:, c0:c0 + cw],
                                                                    start=(c2 == 0), stop=(c2 == 1)),
                      reads=[res("ones"), res("Cm%d" % c2)], writes=[rpb[4]])
            sc.op("dve", lambda e, c0=c0, cw=cw: e.tensor_scalar(bef[:, c0:c0 + cw], pb[4][:, 0:cw], 128.0, None, ALU.mult),
                  reads=[rpb[4]], writes=[res("bef")])
        sc.op("dve", lambda e: e.tensor_scalar(idxW[:], bef[:], pio8[:, 0:1], None, ALU.add),
              reads=[res("bef"), res("pio8")], writes=[res("idxW")])
        sc.op("dve", lambda e: e.memset(zslot[:], 0.0), writes=[res("zslot")])
        sc.dma("sp", "st_z", slot_d.rearrange("(p a) b -> p (a b)", p=128), zslot[:], reads=[res("zslot")], writes=[res("slot_d")])
        def loadB(gt):
            bp = gt % NPB
            sc.dma("sp", "ld_pos%d" % bp, posL[bp][:], pos_d[gt], reads=[res("pos_d")], writes=[res("posL%d" % bp)])
            sc.dma("sp", "ld_wt%d" % bp, wtL[bp][:], wt_d[gt], reads=[res("wt_d")], writes=[res("wtL%d" % bp)])

        for g0 in range(min(NPB - 1, NT)):
            loadB(g0)
        for gt in range(NT):
            bp, br = gt % NPB, gt % NRB
            if gt + NPB - 1 < NT:
                loadB(gt + NPB - 1)
            sc.op("dve", lambda e, bp=bp: e.scalar_tensor_tensor(dp1[:], posL[bp][:], 1.0, sbase[:], ALU.add, ALU.add),
                  reads=[res("posL%d" % bp), res("sbase")], writes=[res("dp1")])
            sc.op("dve", lambda e, bp=bp: e.scalar_tensor_tensor(keyb[:], wtL[bp][:], 0.0, dp1[:], ALU.is_gt, ALU.mult),
                  reads=[res("wtL%d" % bp), res("dp1")], writes=[res("keyb")])
            sc.op("dve", lambda e: e.max(out=d8[:], in_=keyb[:]), reads=[res("keyb")], writes=[res("d8")])
            rrw = res("rows%d" % br)
            sc.op("dve", lambda e, br=br, gt=gt: e.tensor_scalar(rows[br][:, :, 0], pio8[:], float(gt * 128), None, ALU.add),
                  reads=[res("pio8")], writes=[rrw])
            for k in range(8):
                sc.op("dve", lambda e, bp=bp, br=br, k=k: e.scalar_tensor_tensor(
                    junkb[:], keyb[:], d8[:, k:k + 1], wtL[bp][:], ALU.is_equal, ALU.mult, accum_out=rows[br][:, k, 1:2]),
                    reads=[res("keyb"), res("d8"), res("wtL%d" % bp)], writes=[res("junkb"), rrw])
            sc.op("dve", lambda e, gt=gt: e.tensor_scalar(dstu[:, gt, :], d8[:], -1.0, None, ALU.add),
                  reads=[res("d8")], writes=[res("dstu%d" % gt)])
            for k in range(8):
                sc.idma("sc_slot%d" % br, slot_d, dstu[:, gt, k:k + 1], rows[br][:, k, :], None,
                        reads=[rrw, res("dstu%d" % gt), res("slot_d")], writes=[])

        sc.fence()
        slotv = slot_d.rearrange("(i p) c -> i (p c)", p=128)
        for t6 in range(NBLK // 128):
            b2 = t6 % 2
            sc.dma("sp", "ld_srow%d" % b2, sl6[b2][:], slotv[t6 * 128:(t6 + 1) * 128, :], reads=[res("slot_d")],
                   writes=[res("sl6_%d" % b2)])
            v3 = sl6[b2][:, :].rearrange("i (p c) -> i p c", c=2)
            for c in range(2):
                sc.op("dve", lambda e, c=c, v3=v3: e.tensor_copy(out=sl6c[c][:], in_=v3[:, :, c]),
                      reads=[res("sl6_%d" % b2)], writes=[res("sl6c%d" % c)])
                sc.op("pe", lambda e, c=c: e.transpose(pb[c][:, 0:128], sl6c[c][:], idt[:]),
                      reads=[res("sl6c%d" % c), res("idt")], writes=[rpb[c]])
            sc.op("dve", lambda e, t6=t6: e.tensor_copy(out=tokuA[:, t6 * 128:(t6 + 1) * 128], in_=pb[0][:, 0:128]),
                  reads=[rpb[0]], writes=[res("tokuA")])
            sc.op("act", lambda e, t6=t6: e.activation(wA[:, t6 * 128:(t6 + 1) * 128], pb[1][:, 0:128], AF.Copy),
                  reads=[rpb[1]], writes=[res("srowA")])
        WB = NE * 128 - 1

        def gathers(i):
            b3 = i % 3
            sc.idma("g_x%d" % b3, xgC[b3][:], None, h2_d, tokuA[:, i:i + 1], reads=[res("tokuA"), res("h2_d")],
                    writes=[res("xg%d" % b3)])
            sc.idma("g_wg%d" % b3, wgC[b3][:], None, wg2, idxW[:, i:i + 1], reads=[res("idxW")], writes=[res("wgC%d" % b3)], bound=WB)
            sc.idma("g_wu%d" % b3, wuC[b3][:], None, wu2, idxW[:, i:i + 1], reads=[res("idxW")], writes=[res("wuC%d" % b3)], bound=WB)
            sc.idma("g_wd%d" % b3, wdC[b3][:, :, :].rearrange("p a b -> p (a b)"), None, wd2, idxW[:, i:i + 1],
                    reads=[res("idxW")], writes=[res("wdC%d" % b3)], bound=WB)

        def stage1(i):
            b2, b3 = i % 2, i % 3
            pT_, rT_ = pb[4 * b2], rpb[4 * b2]
            pbt = pT_[:, :].bitcast(BF16)
            for kc in range(8):
                sc.op("pe", lambda e, kc=kc, b3=b3, pbt=pbt: e.transpose(
                    pbt[:, kc * 128:(kc + 1) * 128], xgC[b3][:, kc * 128:(kc + 1) * 128], idtb[:]),
                    reads=[res("xg%d" % b3), res("idtb")], writes=[rT_])
            sc.op("act", lambda e, b2=b2, pbt=pbt: e.activation(xgT[b2][:, :, :].rearrange("p a b -> p (a b)"), pbt[:, :], AF.Copy),
                  reads=[rT_], writes=[res("xgT%d" % b2)])

        def stage2(i):
            b2, b3 = i % 2, i % 3
            pG_, rG_ = pb[4 * b2 + 1], rpb[4 * b2 + 1]
            for gu, wC, rn in ((0, wgC, "wgC%d"), (1, wuC, "wuC%d")):
                for half in range(2):
                    col = (gu * 2 + half) * 128
                    for kc in range(8):
                        sc.op("pe", lambda e, kc=kc, b2=b2, b3=b3, wC=wC, half=half, col=col, pG_=pG_: e.matmul(
                            pG_[:, col:col + 128], lhsT=wC[b3][:, kc * 256 + half * 128:kc * 256 + (half + 1) * 128],
                            rhs=xgT[b2][:, kc, :], start=(kc == 0), stop=(kc == 7)),
                            reads=[res(rn % b3), res("xgT%d" % b2)], writes=[rG_])
            sc.op("act", lambda e, b2=b2, pG_=pG_: e.activation(silC[b2][:], pG_[:, 0:256], AF.Silu),
                  reads=[rG_], writes=[res("silC%d" % b2)])
            sc.op("dve", lambda e, b2=b2, pG_=pG_: e.tensor_tensor(
                aTC[b2][:, :, :].rearrange("p a b -> p (a b)"), silC[b2][:], pG_[:, 256:512], ALU.mult),
                reads=[res("silC%d" % b2), rG_], writes=[res("aTC%d" % b2)])

        def stage3(i):
            b2, b3 = i % 2, i % 3
            pY0, pY1, rY0, rY1 = pb[4 * b2 + 2], pb[4 * b2 + 3], rpb[4 * b2 + 2], rpb[4 * b2 + 3]
            for dh, pY_, rY_ in ((0, pY0, rY0), (1, pY1, rY1)):
                for cc in range(2):
                    sc.op("pe", lambda e, cc=cc, b2=b2, b3=b3, dh=dh, pY_=pY_: e.matmul(
                        pY_[:], lhsT=aTC[b2][:, cc, :], rhs=wdC[b3][:, cc, dh * 512:(dh + 1) * 512],
                        start=(cc == 0), stop=(cc == 1)),
                        reads=[res("aTC%d" % b2), res("wdC%d" % b3)], writes=[rY_])
            sc.op("act", lambda e, b2=b2, pY0=pY0, i=i: e.activation(ysb[b2][:, 0:512], pY0[:], AF.Copy, scale=wA[:, i:i + 1]),
                  reads=[rY0, res("srowA")], writes=[res("ysb%d" % b2)])
            sc.op("dve", lambda e, b2=b2, pY1=pY1, i=i: e.tensor_scalar(ysb[b2][:, 512:1024], pY1[:], wA[:, i:i + 1], None, ALU.mult),
                  reads=[rY1, res("srowA")], writes=[res("ysb%d" % b2)])
            sc.dma("sp", "st_y", y_d[i * 128:(i + 1) * 128, :], ysb[b2][:], reads=[res("ysb%d" % b2)], writes=[])

        gathers(0)
        gathers(1)
        stage1(0)
        for i in range(NBLK):
            if i + 2 < NBLK:
                gathers(i + 2)
            stage2(i)
            if i + 1 < NBLK:
                stage1(i + 1)
            stage3(i)

        sc.fence()
        xstL = [cvB(0, 1024).rearrange("p (a b) -> p a b", a=8), cvB(2048, 1024).rearrange("p (a b) -> p a b", a=8)]
        otlL = [cvB(1024, 1024).rearrange("p (a b) -> p a b", a=8), cvB(3072, 1024).rearrange("p (a b) -> p a b", a=8)]

        def gathD(gt):
            b3 = gt % NYG
            for k in range(8):
                sc.idma("g_y%d" % b3, yg[b3][:, k, :], None, y_d, dstu[:, gt, k:k + 1], reads=[res("dstu"), res("y_d")],
                        writes=[res("yg%d_%d" % (b3, k))])

        for g0 in range(min(NYG - 1, NT)):
            gathD(g0)
        for gt in range(NT):
            b2, b3 = gt % 2, gt % NYG
            if gt + NYG - 1 < NT:
                gathD(gt + NYG - 1)
            s_, t_ = gt // 16, gt % 16
            tt = slice(t_ * 128, (t_ + 1) * 128)
            ryg = res("yg%d" % b3)
            acc_, racc = accD[b2], res("accD%d" % b2)
            xst_, rxst = xstL[b2], res("xst%d" % b2)
            otl_, rotl = otlL[b2], res("otl%d" % b2)
            if gt == 0:
                sc.dma("sp", "ld_xs%d" % b2, xst_[:, :, :], xs_d[s_][:, :, tt], reads=[res("xs_d")], writes=[rxst])
            if gt + 1 < NT:
                g1 = gt + 1
                s1, t1 = g1 // 16, g1 % 16
                sc.dma("sp", "ld_xs%d" % (g1 % 2), xstL[g1 % 2][:, :, :], xs_d[s1][:, :, t1 * 128:(t1 + 1) * 128],
                       reads=[res("xs_d")], writes=[res("xst%d" % (g1 % 2))])
            sc.op("dve", lambda e, b3=b3, acc_=acc_: e.tensor_tensor(acc_[:], yg[b3][:, 0, :], yg[b3][:, 1, :], ALU.add),
                  reads=[res("yg%d_0" % b3), res("yg%d_1" % b3)], writes=[racc])
            for k in range(2, 8):
                sc.op("dve", lambda e, k=k, b3=b3, acc_=acc_: e.tensor_tensor(acc_[:], acc_[:], yg[b3][:, k, :], ALU.add),
                      reads=[res("yg%d_%d" % (b3, k)), racc], writes=[racc])
            for c in range(8):
                bi = 4 * b2 + c // 4
                sc.op("pe", lambda e, c=c, bi=bi, acc_=acc_: e.transpose(pb[bi][:, (c % 4) * 128:(c % 4 + 1) * 128],
                                                                          acc_[:, c * 128:(c + 1) * 128], idt[:]),
                      reads=[racc, res("idt")], writes=[rpb[bi]])
            for c in range(8):
                bi = 4 * b2 + c // 4
                sc.op("dve", lambda e, c=c, bi=bi, s_=s_, otl_=otl_, xst_=xst_: e.scalar_tensor_tensor(
                    otl_[:, c, :], pb[bi][:, (c % 4) * 128:(c % 4 + 1) * 128], modT[:, 40 + c, s_:s_ + 1], xst_[:, c, :],
                    ALU.mult, ALU.add),
                    reads=[rpb[bi], res("modT"), rxst], writes=[rotl])
            tok = sc.dma("sp", "st_out", out_d[s_][:, :, tt], otl_[:, :, :], reads=[rotl], writes=[])
        sc.wait("sp", tok)
        sc.emit()
    return nc


def _t5_bucket_np(rel):
    import jax
    import jax.numpy as jnp
    with jax.default_device(jax.devices("cpu")[0]):
        nb = NB // 2
        max_exact = nb // 2
        rel = jnp.asarray(np.asarray(rel, dtype=np.int32))
        n = jnp.abs(rel)
        large = max_exact + (jnp.log(jnp.maximum(n, 1).astype(jnp.float32) / max_exact)
                             / math.log(128 / max_exact) * (nb - max_exact)).astype(jnp.int32)
        large = jnp.minimum(large, nb - 1)
        return np.asarray(jnp.where(rel > 0, nb, 0) + jnp.where(n < max_exact, n, large))


def _chunk_w(w, ncol_chunk=128):
    K, N = w.shape
    return np.ascontiguousarray(w.reshape(K // 128, 128, N // ncol_chunk, ncol_chunk).transpose(2, 1, 0, 3))


def _prep_shared(inp):
    f = np.float32
    g = {}
    g["w_ada"] = _chunk_w(inp["w_ada"][0])
    g["b_adaT"] = np.ascontiguousarray(inp["b_ada"][0].reshape(48, 128).T)
    g["n1g"] = np.ascontiguousarray(inp["norm1_g"][0].reshape(8, 128).T)
    g["n2g"] = np.ascontiguousarray(inp["norm2_g"][0].reshape(8, 128).T)
    g["w_in"] = _chunk_w(inp["w_in"][0])
    g["qkg"] = np.ascontiguousarray(np.stack([np.tile(inp["q_norm_g"][0], 2), np.tile(inp["k_norm_g"][0], 2)], axis=1))
    lam = np.stack([inp["lambda_q1"][0], inp["lambda_k1"][0], inp["lambda_q2"][0], inp["lambda_k2"][0]], axis=0)
    g["lam_in"] = np.ascontiguousarray(np.broadcast_to(lam[None], (128, 4, 64)))
    g["subln_g"] = np.ascontiguousarray(np.broadcast_to(inp["subln_g"][0][None], (128, 128)))
    pw = inp["pool_w"][0]
    g["pool_w"] = np.ascontiguousarray(pw.reshape(4, 2, 128, 2, 128).transpose(0, 3, 2, 1, 4))
    g["pool_scaleT"] = np.ascontiguousarray(inp["pool_scale"][0].reshape(8, 128).T)
    rc = np.zeros((4, 16), f)
    for gi, w in enumerate((2, 4, 8, 16)):
        for k, pos in enumerate(list(range(8)) + list(range(S - 8, S))):
            lo = min(max(pos - w // 2, 0), S - 1)
            hi = min(max(pos + w // 2 - 1, 0), S - 1)
            rc[gi, k] = 1.0 / float(hi - lo + 1)
    g["pool_rc"] = np.ascontiguousarray(np.broadcast_to(rc[None], (128, 4, 16)))
    g["w_out"] = _chunk_w(inp["w_out"][0])
    g["w_router"] = np.ascontiguousarray(inp["w_router"][0].reshape(8, 128, NE).transpose(1, 0, 2))
    g["b_router"] = np.ascontiguousarray(np.broadcast_to(inp["b_router"][0][None], (128, NE)))
    wg = np.concatenate([inp["w_exp_gate"][0], inp["w_sh_gate"]], axis=0)
    wu = np.concatenate([inp["w_exp_up"][0], inp["w_sh_up"]], axis=0)
    wd = np.concatenate([inp["w_exp_down"][0], inp["w_sh_down"]], axis=0)
    g["w_eg"] = np.ascontiguousarray(wg.reshape(NE + 1, 8, 128, 256).transpose(0, 2, 1, 3))
    g["w_eu"] = np.ascontiguousarray(wu.reshape(NE + 1, 8, 128, 256).transpose(0, 2, 1, 3))
    g["w_ed"] = np.ascontiguousarray(wd.reshape(NE + 1, 2, 128, 1024).transpose(0, 2, 1, 3))
    g["rel_tab"] = np.ascontiguousarray(inp["rel_bias_table"])
    jj = np.arange(FVW)
    bk = _t5_bucket_np(767 - jj)
    oh = np.zeros((NB, FVW), f)
    oh[bk, jj] = 1.0
    g["bias_oh"] = oh
    g["ident"] = np.eye(128, dtype=f)
    g["antiid"] = np.ascontiguousarray(np.eye(128, dtype=f)[::-1])
    bo = np.zeros((128, 128), f)
    bo[:64, :64] = 1.0
    bo[64:, 64:] = 1.0
    g["blockones"] = bo
    ar = np.arange(128)
    g["lstrict"] = (ar[:, None] < ar[None, :]).astype(f)
    ee = np.arange(NE)
    g["ustrict"] = np.stack([((c * 128 + ar)[:, None] < ee[None, :]).astype(f) for c in range(2)])
    g["uincl"] = np.stack([((c * 128 + ar)[:, None] <= ee[None, :]).astype(f) for c in range(2)])
    g["iota_row"] = np.ascontiguousarray(np.broadcast_to(np.arange(1024, dtype=f)[None], (128, 1024)))
    g["piota8"] = np.ascontiguousarray(np.broadcast_to(ar.astype(f)[:, None], (128, 8)))
    return {k: np.asarray(v, dtype=f) for k, v in g.items()}


def _core_inputs(shared, x, c, b0, nseq):
    m = dict(shared)
    xs = x[b0:b0 + nseq]
    m["xT"] = np.ascontiguousarray(xs.reshape(nseq, S, 8, 128).transpose(0, 3, 2, 1))
    m["cT"] = np.ascontiguousarray(c[b0:b0 + nseq].reshape(nseq, 8, 128).transpose(2, 1, 0))
    return m


def _unpack(outT):
    return np.ascontiguousarray(outT.transpose(0, 3, 2, 1).reshape(outT.shape[0], S, D))


def kernel(**inputs):
    inp = {k: np.asarray(v, dtype=np.float32) for k, v in inputs.items()}
    ncores = 8
    nseq = inp["x"].shape[0] // ncores
    shared = _prep_shared(inp)
    nc = build(nseq)
    in_maps = [_core_inputs(shared, inp["x"], inp["c"], i * nseq, nseq) for i in range(ncores)]
    res = run_bass_kernel_spmd(nc, in_maps, core_ids=list(range(ncores)))
    return np.concatenate([_unpack(r["outT"]) for r in res.results], axis=0)
```

```python
import math
import numpy as np
from contextlib import ExitStack
import concourse.bass as bass
import concourse.mybir as mybir
from concourse.bass_utils import run_bass_kernel_spmd

F32 = mybir.dt.float32
BF16 = mybir.dt.bfloat16
AF = mybir.ActivationFunctionType
ALU = mybir.AluOpType
AX = mybir.AxisListType

D = 1024
S = 2048
NB = 32
NH = 8
NE = 256
EPS = 1e-6
LAM_INIT = 0.8 - 0.6 * math.exp(-0.3 * 0)
PADL = 16
LP = S + 2 * PADL
MW = 1280
FVW = MW + 127


class Res:
    __slots__ = ("w", "r")

    def __init__(self):
        self.w = None
        self.r = {}


class Sched:
    ENG = ("pe", "dve", "act", "pool", "sp")

    def __init__(self, nc, es):
        self.nc = nc
        self.es = es
        self.sem = {k: es.enter_context(nc.semaphore("s_" + k)) for k in self.ENG}
        self.cnt = {k: 0 for k in self.ENG}
        self.seen = {k: {} for k in self.ENG}
        self.prog = {k: [] for k in self.ENG}
        self.dsem = {}
        self.dcnt = {}

    def _wait(self, e, tok):
        if tok is None:
            return
        key, val = tok
        if key == e and e == "pe":
            return
        if self.seen[e].get(key, 0) >= val:
            return
        sem = self.sem[key] if key in self.sem else self.dsem[key]
        self.prog[e].append(("w", sem, val))
        self.seen[e][key] = val

    def _deps(self, e, reads, writes):
        for r in reads:
            self._wait(e, r.w)
        for w in writes:
            self._wait(e, w.w)
            for k, v in w.r.items():
                self._wait(e, (k, v))

    def _commit(self, tok, reads, writes):
        for r in reads:
            if r.r.get(tok[0], 0) < tok[1]:
                r.r[tok[0]] = tok[1]
        for w in writes:
            w.w = tok
            w.r = {}

    def op(self, e, fn, reads=(), writes=()):
        self._deps(e, reads, writes)
        self.cnt[e] += 1
        self.prog[e].append(("i", fn, self.sem[e], 1))
        tok = (e, self.cnt[e])
        self._commit(tok, reads, writes)
        return tok

    def dma(self, e, chan, out, in_, reads=(), writes=()):
        if chan not in self.dsem:
            self.dsem[chan] = self.es.enter_context(self.nc.semaphore("dm_" + chan))
            self.dcnt[chan] = 0
        self._deps(e, reads, writes)
        self.dcnt[chan] += 16
        self.prog[e].append(("i", lambda eng: eng.dma_start(out=out, in_=in_), self.dsem[chan], 16))
        tok = (chan, self.dcnt[chan])
        self._commit(tok, reads, writes)
        return tok

    def idma(self, chan, out, out_off, in_, in_off, reads=(), writes=(), bound=None):
        e = "pool"
        if chan not in self.dsem:
            self.dsem[chan] = self.es.enter_context(self.nc.semaphore("dm_" + chan))
            self.dcnt[chan] = 0
        self._deps(e, reads, writes)
        self.dcnt[chan] += 16
        oo = None if out_off is None else bass.IndirectOffsetOnAxis(ap=out_off, axis=0)
        io = None if in_off is None else bass.IndirectOffsetOnAxis(ap=in_off, axis=0)
        if bound is None:
            self.prog[e].append(("i", lambda eng: eng.indirect_dma_start(out=out, out_offset=oo, in_=in_, in_offset=io),
                                 self.dsem[chan], 16))
        else:
            self.bound_val = bound
            self.prog[e].append(("i", lambda eng: eng.indirect_dma_start(out=out, out_offset=oo, in_=in_, in_offset=io,
                                                                         bounds_check=self.bound_reg, oob_is_err=False),
                                 self.dsem[chan], 16))
        tok = (chan, self.dcnt[chan])
        self._commit(tok, reads, writes)
        return tok

    def wait(self, e, tok):
        self._wait(e, tok)

    def fence(self):
        toks = [(k, self.cnt[k]) for k in self.ENG if self.cnt[k] > 0]
        toks += [(k, v) for k, v in self.dcnt.items() if v > 0]
        for e in self.ENG:
            for t in toks:
                if t[0] == e:
                    continue
                self._wait(e, t)

    def emit(self):
        nc = self.nc
        with nc.Block() as block:
            def mk(e):
                def f(eng):
                    if e == "pool" and getattr(self, "bound_val", None) is not None:
                        self.bound_reg = eng.alloc_register("oob_bound")
                        eng.reg_mov(self.bound_reg, int(self.bound_val))
                    for it in self.prog[e]:
                        if it[0] == "w":
                            eng.wait_ge(it[1], it[2])
                        else:
                            it[1](eng).then_inc(it[2], it[3])
                return f
            block.tensor(mk("pe"))
            block.vector(mk("dve"))
            block.scalar(mk("act"))
            block.gpsimd(mk("pool"))
            block.sync(mk("sp"))


def build(NSEQ, n_exp=NE, dbg=False):
    nc = bass.Bass("TRN2", target_bir_lowering=False)

    def din(name, shape):
        return nc.dram_tensor(name, list(shape), F32, kind="ExternalInput").ap()

    xT_d = din("xT", [NSEQ, 128, 8, S])
    cT_d = din("cT", [128, 8, NSEQ])
    wada_d = din("w_ada", [48, 128, 8, 128])
    bada_d = din("b_adaT", [128, 48])
    n1g_d = din("n1g", [128, 8])
    n2g_d = din("n2g", [128, 8])
    win_d = din("w_in", [48, 128, 8, 128])
    qkg_d = din("qkg", [128, 2])
    lam_d = din("lam_in", [128, 4, 64])
    sg_d = din("subln_g", [128, 128])
    pw_d = din("pool_w", [4, 2, 128, 2, 128])
    psc_d = din("pool_scaleT", [128, 8])
    rc_d = din("pool_rc", [128, 4, 16])
    wo_d = din("w_out", [8, 128, 8, 128])
    wr_d = din("w_router", [128, 8, NE])
    br_d = din("b_router", [128, NE])
    wg_d = din("w_eg", [NE + 1, 128, 8, 256])
    wu_d = din("w_eu", [NE + 1, 128, 8, 256])
    wd_d = din("w_ed", [NE + 1, 128, 2, 1024])
    tab_d = din("rel_tab", [NB, NH])
    oh_d = din("bias_oh", [NB, FVW])
    idt_d = din("ident", [128, 128])
    aid_d = din("antiid", [128, 128])
    bones_d = din("blockones", [128, 128])
    out_d = nc.dram_tensor("outT", [NSEQ, 128, 8, S], F32, kind="ExternalOutput").ap()
    fv_d = nc.dram_tensor("fv_scr", [NH, FVW], F32, kind="Internal").ap()
    mst_d = nc.dram_tensor("mst_scr", [NH, 128, MW], F32, kind="Internal").ap()
    T = NSEQ * S
    NT = T // 128
    NBLK = T * 8 // 128 + NE
    NSLOT = NBLK * 128
    ls_d = din("lstrict", [128, 128])
    us_d = din("ustrict", [2, 128, NE])
    ui_d = din("uincl", [2, 128, NE])
    iot_d = din("iota_row", [128, 1024])
    pio_d = din("piota8", [128, 8])
    U32 = mybir.dt.uint32
    I32 = mybir.dt.int32
    h2_d = nc.dram_tensor("h2_scr", [T, D], BF16, kind="Internal").ap()
    pos_d = nc.dram_tensor("pos_scr", [NT, 128, NE], F32, kind="Internal").ap()
    wt_d = nc.dram_tensor("wt_scr", [NT, 128, NE], F32, kind="Internal").ap()
    xs_d = nc.dram_tensor("xs_scr", [NSEQ, 128, 8, S], F32, kind="ExternalOutput").ap() if dbg else out_d
    slot_d = nc.dram_tensor("slot_scr", [NSLOT, 2], F32, kind="Internal").ap()
    y_d = nc.dram_tensor("y_scr", [NSLOT, D], BF16, kind="Internal").ap()
    wg2 = wg_d.rearrange("e p a b -> (e p) (a b)")
    wu2 = wu_d.rearrange("e p a b -> (e p) (a b)")
    wd2 = wd_d.rearrange("e p a b -> (e p) (a b)")

    with ExitStack() as es:
        es.enter_context(nc.allow_low_precision("bf16 matmul operands, fp32 accumulation"))
        es.enter_context(nc.allow_non_contiguous_dma("overlapping-window bias load"))
        sc = Sched(nc, es)

        def sb(name, shape, dt=F32):
            return es.enter_context(nc.sbuf_tensor(name, list(shape), dt))

        CW = 256
        arA = sb("arA", [128, 16384])
        arB = sb("arB", [128, 8320])

        def cvA(off, n, dt=F32):
            return arA[:, off:off + n] if dt == F32 else arA[:, off:off + n].bitcast(dt)

        def cvB(off, n, dt=F32):
            return arB[:, off:off + n] if dt == F32 else arB[:, off:off + n].bitcast(dt)

        bigA = arA[:, :].rearrange("p (a b) -> p a b", a=8)
        mT = cvA(0, 8192, BF16).rearrange("p (a b) -> p a b", a=8)
        qz = [cvA(8192, 1024, BF16), cvB(0, 1024, BF16)]
        kT = cvA(9216, 1024, BF16)
        vA = cvA(10240, 1040, BF16).rearrange("p (a b) -> p a b", a=16)
        gaT = cvA(11280, 1024, BF16)
        mst = cvA(12304, 1280)
        O = [cvA(13584, 520).rearrange("p (a b) -> p a b", a=4), cvA(14104, 520).rearrange("p (a b) -> p a b", a=4)]
        sadd = [cvA(14624, 512), cvA(15136, 512)]
        hank = cvA(0, 1280)
        oh = arA[0:NB, 1280:1280 + FVW]
        fvs = arA[0:NH, 2688:2688 + FVW]
        wadaS = [cvA(4096, 1024).rearrange("p (a b) -> p a b", a=8), cvA(5120, 1024).rearrange("p (a b) -> p a b", a=8)]
        lam_in = cvA(6144, 256).rearrange("p (a b) -> p a b", a=4)
        lamt = cvA(6400, 256).rearrange("p (a b) -> p a b", a=4)
        sil = cvB(0, 1024).rearrange("p (a b) -> p a b", a=2)
        aT = cvB(1024, 512, BF16).rearrange("p (a b) -> p a b", a=2)
        scr = cvB(1536, 256)
        bia = cvB(1792, 256)
        msk = cvB(2048, 256)
        Wtok = [cvB(2304, 256), cvB(2560, 256)]
        selT2 = [cvB(2816, 256), cvB(4736, 256)]
        posS = [cvB(3072, 256), cvB(3328, 256)]
        m8 = cvB(3584, 64).rearrange("p (a b) -> p a b", a=8)
        gsc = cvB(3648, 8)
        gm8 = cvB(3656, 8)
        gmk = cvB(3664, 8)
        t8 = cvB(3672, 8)
        den = cvB(3680, 2)
        h2tm = [cvB(3712, 512, BF16), cvB(4224, 512, BF16)]
        pP = cvB(0, LP)
        pW = [cvB(2080, LP), cvB(4160, LP)]
        mixT = cvB(6240, 2048, BF16).rearrange("p (a b) -> p a b", a=2)
        nbf = cvB(0, 256)
        nbi = cvB(256, 256, I32)
        sbase = cvB(512, 256)
        nbT = [cvB(768, 128), cvB(896, 128)]
        bend = cvB(1024, 2)
        Cm = [cvB(1032, NBLK), cvB(1032 + NBLK, NBLK)]
        bef = cvB(1032 + 2 * NBLK, NBLK)
        o_b = 1032 + 3 * NBLK
        NPB = 4
        NRB = 8
        posL = [cvB(o_b + 256 * i, 256) for i in range(NPB)]
        wtL = [cvB(o_b + 256 * NPB + 256 * i, 256) for i in range(NPB)]
        o_c = o_b + 512 * NPB
        dp1 = cvB(o_c, 256)
        keyb = cvB(o_c + 256, 256)
        junkb = cvB(o_c + 512, 256)
        d8 = cvB(o_c + 768, 8)
        rows = [cvB(o_c + 776 + 16 * i, 16).rearrange("p (a b) -> p a b", a=8) for i in range(NRB)]
        zslot = cvB(o_c + 776 + 16 * NRB, NSLOT * 2 // 128)
        assert o_c + 776 + 16 * NRB + NSLOT * 2 // 128 <= 8320
        wgC = [cvA(3072 * i, 1024, BF16) for i in range(3)]
        wuC = [cvA(3072 * i + 1024, 1024, BF16) for i in range(3)]
        wdC = [cvA(3072 * i + 2048, 1024, BF16).rearrange("p (a b) -> p a b", a=2) for i in range(3)]
        xgC = [cvA(9216 + 512 * i, 512, BF16) for i in range(3)]
        xgT = [cvA(10752 + 512 * i, 512, BF16).rearrange("p (a b) -> p a b", a=8) for i in range(2)]
        silC = [cvA(11776, 256), cvA(12032, 256)]
        aTC = [cvA(12288, 128, BF16).rearrange("p (a b) -> p a b", a=2), cvA(12416, 128, BF16).rearrange("p (a b) -> p a b", a=2)]
        ysb = [cvA(12544, 512, BF16), cvA(13056, 512, BF16)]
        wA = cvA(13568, NBLK)
        tokuA = cvA(13568 + NBLK, NBLK, U32)
        sl6 = [cvA(13568 + 2 * NBLK, 256), cvA(13568 + 2 * NBLK + 256, 256)]
        sl6c = [cvA(13568 + 2 * NBLK + 512, 128), cvA(13568 + 2 * NBLK + 640, 128)]
        assert 13568 + 2 * NBLK + 768 <= 16384 and NBLK % 128 == 0
        NYG = 4
        yg = [cvA(4096 * i, 4096, BF16).rearrange("p (a b) -> p a b", a=8) for i in range(NYG)]
        accD = [cvB(4096, 1024), cvB(5120, 1024)]

        hT = sb("hT", [128, 8, S], BF16)
        tmpF = sb("tmpF", [128, 8, CW])
        xch = sb("xch", [128, 8, CW])
        sqc = [sb("sqc%d" % i, [128, CW]) for i in range(2)]
        rstd = sb("rstd", [128, CW])
        modT = sb("modT", [128, 48, NSEQ])
        bada = sb("bada", [128, 48])
        n1g = sb("n1g_s", [128, 8])
        n2g = sb("n2g_s", [128, 8])
        A1 = sb("A1", [128, 8])
        A2 = sb("A2", [128, 8])
        cT = sb("cT_s", [128, 8, NSEQ])
        scT = sb("scT", [128, 8, NSEQ])
        winS = [sb("win%d" % i, [128, 8, 128], BF16) for i in range(2)]
        qkg = sb("qkg_s", [128, 2])
        lamv = sb("lamv", [128, 4])
        nlam = sb("nlam", [128, 1])
        sg = sb("sg_s", [128, 128])
        pwS = sb("pw_s", [128, 2, 2, 128], BF16)
        psc = sb("psc_s", [128, 8])
        rc = sb("rc_s", [128, 4, 16])
        woS = [sb("wo%d" % i, [128, 8, 128], BF16) for i in range(2)]
        wr = sb("wr_s", [128, 8, NE])
        wsg = sb("wsg", [128, 8, 256], BF16)
        wsu = sb("wsu", [128, 8, 256], BF16)
        wsd = sb("wsd", [128, 2, 1024], BF16)
        base = sb("base", [128, NE])
        lsm = sb("lsm", [128, 128])
        idtb = sb("idtb", [128, 128], BF16)
        pio8 = sb("pio8", [128, 8])
        dstu = sb("dstu", [128, NT, 8], U32)
        idxW = sb("idxW", [128, NBLK], U32)
        brt = sb("br_s", [128, NE])
        tab = sb("tab_s", [NB, NH])
        idt = sb("idt", [128, 128])
        aid = sb("aid", [128, 128])
        bones = sb("bones", [128, 128])
        ones = sb("ones", [128, 128])
        sqh2 = [sb("sqh%d" % i, [128, 512]) for i in range(2)]
        rsh2 = [sb("rsh%d" % i, [128, 512]) for i in range(2)]
        ET = [sb("ET%d" % i, [128, 512], BF16) for i in range(3)]
        rr = sb("rr", [128, 8])
        att4 = sb("att4", [128, 4, 128])
        att4b = sb("att4b", [128, 4, 128])
        ssq4 = sb("ssq4", [128, 8])
        epsc = sb("epsc", [128, 2])
        gpT = sb("gpT", [128, 512])
        ptmp = sb("ptmp", [128, 512])

        pb = [es.enter_context(nc.psum_tensor("pb%d" % i, [128, 512], F32)) for i in range(8)]
        rpb = [Res() for _ in range(8)]

        R = {}

        def res(name):
            if name not in R:
                R[name] = Res()
            return R[name]

        rbigA = [Res() for _ in range(4)]
        rhT = [Res() for _ in range(8)]
        rmT = [[Res() for _ in range(4)] for _ in range(8)]

        def load(dst, src, name, eng="sp"):
            sc.dma(eng, "ld_" + name, dst, src, writes=[res(name)])

        load(bada[:], bada_d, "bada")
        load(n1g[:], n1g_d, "n1g")
        load(n2g[:], n2g_d, "n2g")
        load(cT[:], cT_d, "cT")
        load(qkg[:], qkg_d, "qkg")
        load(lam_in[:], lam_d, "lam_in")
        load(sg[:], sg_d, "sg")
        load(psc[:], psc_d, "psc")
        load(rc[:], rc_d, "rc")
        load(wr[:], wr_d, "wr")
        load(brt[:], br_d, "brt")
        load(tab[:], tab_d, "tab")
        load(oh[:], oh_d, "oh")
        load(idt[:], idt_d, "idt")
        load(aid[:], aid_d, "aid")
        load(bones[:], bones_d, "bones")
        load(lsm[:], ls_d, "lsm")
        load(pio8[:], pio_d, "pio8")
        sc.dma("pool", "ld_wsg", wsg[:], wg_d[NE], writes=[res("wsg")])
        sc.dma("pool", "ld_wsu", wsu[:], wu_d[NE], writes=[res("wsu")])
        sc.dma("pool", "ld_wsd", wsd[:], wd_d[NE], writes=[res("wsd")])
        sc.op("dve", lambda e: e.memset(base[:], 0.0), writes=[res("base")])
        sc.op("dve", lambda e: e.tensor_copy(out=idtb[:], in_=idt[:]), reads=[res("idt")], writes=[res("idtb")])
        sc.op("dve", lambda e: e.memset(ones[:], 1.0), writes=[res("ones")])
        sc.op("dve", lambda e: e.memset(epsc[:, 0:1], float(EPS)), writes=[res("epsc")])
        sc.op("dve", lambda e: e.memset(epsc[:, 1:2], float(64 * EPS)), reads=[res("epsc")], writes=[res("epsc")])
        sc.op("dve", lambda e: e.tensor_scalar(sg[:], sg[:], float(1.0 - LAM_INIT), None, ALU.mult),
              reads=[res("sg")], writes=[res("sg")])
        sc.op("dve", lambda e: e.tensor_tensor(lamt[:, 0, :], lam_in[:, 0, :], lam_in[:, 1, :], ALU.mult),
              reads=[res("lam_in")], writes=[res("lamt")])
        sc.op("dve", lambda e: e.tensor_tensor(lamt[:, 1, :], lam_in[:, 2, :], lam_in[:, 3, :], ALU.mult),
              reads=[res("lam_in"), res("lamt")], writes=[res("lamt")])
        sc.op("dve", lambda e: e.tensor_reduce(lamv[:, 0:2], lamt[:, 0:2, :], AX.X, ALU.add),
              reads=[res("lamt")], writes=[res("lamv")])
        sc.op("act", lambda e: e.activation(lamv[:, 2:4], lamv[:, 0:2], AF.Exp),
              reads=[res("lamv")], writes=[res("lamv")])
        sc.op("dve", lambda e: e.scalar_tensor_tensor(nlam[:], lamv[:, 3:4], float(-LAM_INIT), lamv[:, 2:3],
                                                      ALU.add, ALU.subtract),
              reads=[res("lamv")], writes=[res("nlam")])

        sc.op("act", lambda e: e.activation(scT[:], cT[:], AF.Silu), reads=[res("cT")], writes=[res("scT")])
        for fc in range(48):
            wb = wadaS[fc % 2]
            rw = res("wada%d" % (fc % 2))
            sc.dma("sp", "ld_wada%d" % (fc % 2), wb[:], wada_d[fc], writes=[rw])
            for kc in range(8):
                sc.op("pe", lambda e, wb=wb, kc=kc: e.matmul(pb[7][:, 0:NSEQ], lhsT=wb[:, kc, :], rhs=scT[:, kc, :],
                                                              start=(kc == 0), stop=(kc == 7)),
                      reads=[rw, res("scT")], writes=[rpb[7]])
            sc.op("dve", lambda e, fc=fc: e.tensor_scalar(modT[:, fc, :], pb[7][:, 0:NSEQ], bada[:, fc:fc + 1], None,
                                                           ALU.add),
                  reads=[rpb[7], res("bada")], writes=[res("modT")])

        for c0 in range(0, FVW, 512):
            cw = min(512, FVW - c0)
            sc.op("pe", lambda e, c0=c0, cw=cw: e.matmul(pb[7][0:NH, 0:cw], lhsT=tab[:, :], rhs=oh[:, c0:c0 + cw],
                                                          start=True, stop=True),
                  reads=[res("tab"), res("oh")], writes=[rpb[7]])
            sc.op("dve", lambda e, c0=c0, cw=cw: e.tensor_copy(out=fvs[:, c0:c0 + cw], in_=pb[7][0:NH, 0:cw]),
                  reads=[rpb[7]], writes=[res("fvs")])
        sc.dma("sp", "st_fv", fv_d, fvs[:], reads=[res("fvs")], writes=[res("fv_d")])
        for h in range(NH):
            src = bass.AP(fv_d.tensor, h * FVW, [[1, 128], [1, MW]])
            sc.dma("sp", "ld_hank", hank[:], src, reads=[res("fv_d")], writes=[res("hank")])
            for c0 in range(0, MW, 512):
                cw = min(512, MW - c0)
                sc.op("pe", lambda e, c0=c0, cw=cw: e.matmul(pb[7][:, 0:cw], lhsT=aid[:], rhs=hank[:, c0:c0 + cw],
                                                              start=True, stop=True),
                      reads=[res("aid"), res("hank")], writes=[rpb[7]])
                sc.op("dve", lambda e, c0=c0, cw=cw: e.tensor_copy(out=mst[:, c0:c0 + cw], in_=pb[7][:, 0:cw]),
                      reads=[rpb[7]], writes=[res("mst")])
            sc.dma("sp", "st_mst", mst_d[h], mst[:], reads=[res("mst")], writes=[res("mst_d")])

        win_ctr = [0]

        def load_win(chunk):
            i = win_ctr[0] % 2
            win_ctr[0] += 1
            sc.dma("pool", "ld_win%d" % i, winS[i][:], win_d[chunk], writes=[res("win%d" % i)])
            return winS[i], res("win%d" % i)

        def rmsnorm_chunk(Acol, shcol0, s, ts, rdst, f32_out):
            for c in range(8):
                q = sqc[c % 2]
                rq = res("sqc%d" % (c % 2))
                sc.op("act", lambda e, c=c, q=q: e.activation(q[:], xch[:, c, :], AF.Square),
                      reads=[res("xch")], writes=[rq])
                sc.op("pe", lambda e, c=c, q=q: e.matmul(pb[7][:, 0:CW], lhsT=ones[:], rhs=q[:],
                                                         start=(c == 0), stop=(c == 7)),
                      reads=[res("ones"), rq], writes=[rpb[7]])
            sc.op("act", lambda e: e.activation(rstd[:], pb[7][:, 0:CW], AF.Sqrt, bias=float(EPS), scale=1.0 / D),
                  reads=[rpb[7]], writes=[res("rstd")])
            sc.op("dve", lambda e: e.reciprocal(rstd[:], rstd[:]), reads=[res("rstd")], writes=[res("rstd")])
            for c in range(8):
                sc.op("dve", lambda e, c=c: e.scalar_tensor_tensor(
                    tmpF[:, c, :], xch[:, c, :], Acol[:, c:c + 1], rstd[:], ALU.mult, ALU.mult),
                    reads=[res("xch"), res("rstd"), res("Acol")], writes=[res("tmpF%d" % c)])
                if not f32_out:
                    sc.op("act", lambda e, c=c: e.activation(
                        hT[:, c, ts], tmpF[:, c, :], AF.Identity, bias=modT[:, shcol0 + c, s:s + 1], scale=1.0),
                        reads=[res("tmpF%d" % c), res("modT")], writes=[rdst])
                else:
                    sc.op("act", lambda e, c=c: e.activation(
                        tmpF[:, c, :], tmpF[:, c, :], AF.Identity, bias=modT[:, shcol0 + c, s:s + 1], scale=1.0),
                        reads=[res("tmpF%d" % c), res("modT")], writes=[res("tmpF%d" % c)])
                    sc.op("dve", lambda e, c=c: e.tensor_copy(out=hT[:, c, ts], in_=tmpF[:, c, :]),
                          reads=[res("tmpF%d" % c)], writes=[rdst])

        rtmpF = [res("tmpF%d" % c) for c in range(8)]

        for s in range(NSEQ):
            sc.fence()
            sc.op("dve", lambda e: e.memset(vA[:], 1.0), writes=[res("vA%d" % t_) for t_ in range(16)])
            sc.op("dve", lambda e: e.memset(qz[0][64:128, :], 0.0), writes=[res("qT")])
            sc.op("dve", lambda e: e.memset(qz[1][0:64, :], 0.0), writes=[res("qT")])
            sc.op("dve", lambda e, s=s: e.scalar_tensor_tensor(A1[:], modT[:, 8:16, s], 1.0, n1g[:], ALU.add, ALU.mult),
                  reads=[res("modT"), res("n1g")], writes=[res("Acol")])
            for j in range(8):
                ts = slice(j * CW, (j + 1) * CW)
                sc.dma("sp", "ld_x", xch[:], xT_d[s][:, :, ts], writes=[res("xch")])
                rmsnorm_chunk(A1, 0, s, ts, rhT[j], False)

            for h in range(NH):
                sc.dma("sp", "ld_mst", mst[:], mst_d[h], reads=[res("mst_d")], writes=[res("mst")])
                for which, dstT, rname in ((0, None, "qT"), (1, kT, "kT")):
                    wb, rw = load_win(which * 8 + h)
                    for j in range(4):
                        ts = slice(j * 512, (j + 1) * 512)
                        pa = 4 + 2 * (j % 2)
                        pn = pa + 1
                        sq_, rs_ = sqh2[j % 2], rsh2[j % 2]
                        rsq, rrs = res("sqh%d" % (j % 2)), res("rsh%d" % (j % 2))
                        for kc in range(8):
                            sc.op("pe", lambda e, wb=wb, kc=kc, ts=ts, pa=pa: e.matmul(
                                pb[pa][:], lhsT=wb[:, kc, :], rhs=hT[:, kc, ts], start=(kc == 0), stop=(kc == 7)),
                                reads=[rw, rhT[2 * j], rhT[2 * j + 1]], writes=[rpb[pa]])
                        sc.op("act", lambda e, pa=pa, sq_=sq_: e.activation(sq_[:], pb[pa][:], AF.Square),
                              reads=[rpb[pa]], writes=[rsq])
                        sc.op("pe", lambda e, pn=pn, sq_=sq_: e.matmul(pb[pn][:], lhsT=bones[:], rhs=sq_[:], start=True, stop=True),
                              reads=[res("bones"), rsq], writes=[rpb[pn]])
                        if which == 0:
                            sc.op("act", lambda e, pn=pn, rs_=rs_: e.activation(rs_[:], pb[pn][:], AF.Ln, bias=epsc[:, 1:2], scale=1.0),
                                  reads=[rpb[pn], res("epsc")], writes=[rrs])
                        else:
                            sc.op("act", lambda e, pn=pn, rs_=rs_: e.activation(rs_[:], pb[pn][:], AF.Ln, bias=epsc[:, 0:1], scale=1.0 / 64),
                                  reads=[rpb[pn], res("epsc")], writes=[rrs])
                        sc.op("act", lambda e, rs_=rs_: e.activation(rs_[:], rs_[:], AF.Exp, scale=-0.5), reads=[rrs], writes=[rrs])
                        if which == 1:
                            sc.op("dve", lambda e, dstT=dstT, ts=ts, which=which, pa=pa, rs_=rs_: e.scalar_tensor_tensor(
                                dstT[:, ts], pb[pa][:], qkg[:, which:which + 1], rs_[:], ALU.mult, ALU.mult),
                                reads=[rpb[pa], rrs, res("qkg")], writes=[res(rname)])
                        else:
                            for comp in range(2):
                                pr = slice(comp * 64, (comp + 1) * 64)
                                sc.op("dve", lambda e, ts=ts, pa=pa, rs_=rs_, comp=comp, pr=pr: e.scalar_tensor_tensor(
                                    qz[comp][pr, ts], pb[pa][pr, :], qkg[pr, 0:1], rs_[pr, :], ALU.mult, ALU.mult),
                                    reads=[rpb[pa], rrs, res("qkg")], writes=[res(rname)])
                wb, rw = load_win(16 + h)
                for t in range(16):
                    tt = slice(t * 128, (t + 1) * 128)
                    vb = 4 + (t % 4)
                    for kc in range(8):
                        sc.op("pe", lambda e, wb=wb, kc=kc, tt=tt, vb=vb: e.matmul(
                            pb[vb][:, 0:128], lhsT=hT[:, kc, tt], rhs=wb[:, kc, :], start=(kc == 0), stop=(kc == 7)),
                            reads=[rw, rhT[t // 2]], writes=[rpb[vb]])
                    if t % 2 == 0:
                        sc.op("act", lambda e, t=t, vb=vb: e.activation(vA[:, t, 0:128], pb[vb][:, 0:128], AF.Copy),
                              reads=[rpb[vb]], writes=[res("vA%d" % t)])
                    else:
                        sc.op("dve", lambda e, t=t, vb=vb: e.tensor_copy(out=vA[:, t, 0:128], in_=pb[vb][:, 0:128]),
                              reads=[rpb[vb]], writes=[res("vA%d" % t)])
                wb, rw = load_win(32 + h)
                for j in range(4):
                    ts = slice(j * 512, (j + 1) * 512)
                    gb = 4 + j
                    for kc in range(8):
                        sc.op("pe", lambda e, wb=wb, kc=kc, ts=ts, gb=gb: e.matmul(
                            pb[gb][:], lhsT=wb[:, kc, :], rhs=hT[:, kc, ts], start=(kc == 0), stop=(kc == 7)),
                            reads=[rw, rhT[2 * j], rhT[2 * j + 1]], writes=[rpb[gb]])
                    sc.op("act", lambda e, ts=ts, gb=gb: e.activation(gaT[:, ts], pb[gb][:], AF.Sigmoid),
                          reads=[rpb[gb]], writes=[res("gaT")])
                tiles = [(j, comp, kb) for j in range(4) for comp in range(2) for kb in range(16)]

                def emit_st(n):
                    j, comp, kb = tiles[n]
                    bi = 4 + (n % 3)
                    ks = slice(kb * 128, (kb + 1) * 128)
                    qs = slice(j * 512, (j + 1) * 512)
                    sc.op("pe", lambda e, bi=bi, comp=comp, ks=ks, qs=qs: e.matmul(
                        pb[bi][:], lhsT=kT[:, ks], rhs=qz[comp][:, qs], start=True, stop=True),
                        reads=[res("qT"), res("kT")], writes=[rpb[bi]])

                emit_st(0)
                emit_st(1)
                for n, (j, comp, kb) in enumerate(tiles):
                    if n + 2 < len(tiles):
                        emit_st(n + 2)
                    bi = 4 + (n % 3)
                    ei = n % 3
                    o = kb - 4 * j
                    rET = res("ET%d" % ei)
                    if o <= -2 or o >= 5:
                        col = (MW - 1) if o <= -2 else 0
                        sc.op("act", lambda e, bi=bi, ei=ei, col=col: e.activation(
                            ET[ei][:], pb[bi][:], AF.Exp, bias=mst[:, col:col + 1], scale=1.0),
                            reads=[rpb[bi], res("mst")], writes=[rET])
                    else:
                        m0 = 640 - 128 * o
                        si = n % 2
                        rsa = res("sadd%d" % si)
                        sc.op("dve", lambda e, bi=bi, si=si, m0=m0: e.tensor_tensor(
                            sadd[si][:], pb[bi][:], mst[:, m0:m0 + 512], ALU.add),
                            reads=[rpb[bi], res("mst")], writes=[rsa])
                        sc.op("act", lambda e, ei=ei, si=si: e.activation(ET[ei][:], sadd[si][:], AF.Exp),
                              reads=[rsa], writes=[rET])
                    for sub in range(4):
                        sc.op("pe", lambda e, sub=sub, ei=ei, kb=kb: e.matmul(
                            pb[sub][:, 0:129], lhsT=ET[ei][:, sub * 128:(sub + 1) * 128], rhs=vA[:, kb, 0:129],
                            start=(kb == 0), stop=(kb == 15)),
                            reads=[rET, res("vA%d" % kb)], writes=[rpb[sub]])
                    if kb == 15:
                        for sub in range(4):
                            eng = "act" if sub % 2 == 0 else "dve"
                            if eng == "act":
                                sc.op("act", lambda e, sub=sub, comp=comp: e.activation(
                                    O[comp][:, sub, 0:129], pb[sub][:, 0:129], AF.Copy),
                                    reads=[rpb[sub]], writes=[res("O%d_%d" % (comp, sub))])
                            else:
                                sc.op("dve", lambda e, sub=sub, comp=comp: e.tensor_copy(
                                    out=O[comp][:, sub, 0:129], in_=pb[sub][:, 0:129]),
                                    reads=[rpb[sub]], writes=[res("O%d_%d" % (comp, sub))])
                    if kb == 15 and comp == 1:
                        ts = slice(j * 512, (j + 1) * 512)
                        sc.op("dve", lambda e: e.reciprocal(rr[:, 0:4], O[0][:, :, 128]),
                              reads=[res("O0_%d" % u) for u in range(4)], writes=[res("rr")])
                        sc.op("dve", lambda e: e.reciprocal(rr[:, 4:8], O[1][:, :, 128]),
                              reads=[res("O1_%d" % u) for u in range(4)] + [res("rr")], writes=[res("rr")])
                        sc.op("dve", lambda e: e.tensor_scalar(rr[:, 4:8], rr[:, 4:8], nlam[:, 0:1], None, ALU.mult),
                              reads=[res("rr"), res("nlam")], writes=[res("rr")])
                        sc.op("dve", lambda e: e.tensor_tensor(
                            att4[:, :, :], O[0][:, :, 0:128], rr[:, 0:4].unsqueeze(2).to_broadcast([128, 4, 128]), ALU.mult),
                            reads=[res("O0_%d" % u) for u in range(4)] + [res("rr")], writes=[res("att4")])
                        sc.op("dve", lambda e: e.tensor_tensor(
                            att4b[:, :, :], O[1][:, :, 0:128], rr[:, 4:8].unsqueeze(2).to_broadcast([128, 4, 128]), ALU.mult),
                            reads=[res("O1_%d" % u) for u in range(4)] + [res("rr")], writes=[res("att4b")])
                        sc.op("dve", lambda e: e.tensor_tensor(att4[:, :, :], att4[:, :, :], att4b[:, :, :], ALU.add),
                              reads=[res("att4"), res("att4b")], writes=[res("att4")])
                        sc.op("dve", lambda e: e.tensor_tensor(att4b[:, :, :], att4[:, :, :], att4[:, :, :], ALU.mult),
                              reads=[res("att4")], writes=[res("att4b")])
                        sc.op("dve", lambda e: e.tensor_reduce(ssq4[:, 0:4], att4b[:, :, :], AX.X, ALU.add),
                              reads=[res("att4b")], writes=[res("ssq4")])
                        sc.op("act", lambda e: e.activation(ssq4[:, 4:8], ssq4[:, 0:4], AF.Ln, bias=epsc[:, 0:1], scale=1.0 / 128),
                              reads=[res("ssq4"), res("epsc")], writes=[res("ssq4")])
                        sc.op("act", lambda e: e.activation(ssq4[:, 4:8], ssq4[:, 4:8], AF.Exp, scale=-0.5),
                              reads=[res("ssq4")], writes=[res("ssq4")])
                        sc.op("dve", lambda e: e.tensor_tensor(
                            att4[:, :, :], att4[:, :, :], ssq4[:, 4:8].unsqueeze(2).to_broadcast([128, 4, 128]), ALU.mult),
                            reads=[res("att4"), res("ssq4")], writes=[res("att4")])
                        sc.op("dve", lambda e: e.tensor_tensor(
                            att4[:, :, :], att4[:, :, :], sg[:, :].unsqueeze(1).to_broadcast([128, 4, 128]), ALU.mult),
                            reads=[res("att4"), res("sg")], writes=[res("att4")])
                        for sub in range(4):
                            sc.op("pe", lambda e, sub=sub: e.transpose(pb[7][:, sub * 128:(sub + 1) * 128], att4[:, sub, :], idt[:]),
                                  reads=[res("att4"), res("idt")], writes=[rpb[7]])
                        sc.op("dve", lambda e, ts=ts, h=h: e.tensor_tensor(mT[:, h, ts], pb[7][:, :], gaT[:, ts], ALU.mult),
                              reads=[rpb[7], res("gaT")], writes=[rmT[h][j]])

            sc.fence()
            sc.op("dve", lambda e: e.memset(pP[:], 0.0), writes=[res("pP")])
            for g in range(4):
                wnd = (2, 4, 8, 16)[g]
                for dc in range(2):
                    sc.dma("pool", "ld_pw", pwS[:, dc, :, :], pw_d[g, dc], writes=[res("pw")])
                for cc in range(2):
                    chunk = 2 * g + cc
                    wb, rw = load_win(24 + chunk)
                    for j in range(4):
                        ts = slice(j * 512, (j + 1) * 512)
                        for kc in range(8):
                            sc.op("pe", lambda e, wb=wb, kc=kc, ts=ts: e.matmul(
                                pb[6][:], lhsT=wb[:, kc, :], rhs=hT[:, kc, ts], start=(kc == 0), stop=(kc == 7)),
                                reads=[rw, rhT[2 * j], rhT[2 * j + 1]], writes=[rpb[6]])
                        sc.op("act", lambda e, j=j: e.activation(pP[:, PADL + j * 512:PADL + (j + 1) * 512], pb[6][:],
                                                                 AF.Copy),
                              reads=[rpb[6]], writes=[res("pP")])
                    L = LP
                    sc.op("dve", lambda e: e.tensor_tensor(pW[0][:, 1:L], pP[:, 0:L - 1], pP[:, 1:L], ALU.add),
                          reads=[res("pP")], writes=[res("pW0")])
                    cur = 0
                    for lvl, sh in ((4, 1), (8, 2), (16, 4)):
                        if wnd < lvl:
                            break
                        nxt = 1 - cur
                        sc.op("dve", lambda e, cur=cur, nxt=nxt, sh=sh: e.tensor_tensor(
                            pW[nxt][:, sh:L - sh], pW[cur][:, 0:L - 2 * sh], pW[cur][:, 2 * sh:L], ALU.add),
                            reads=[res("pW%d" % cur)], writes=[res("pW%d" % nxt)])
                        cur = nxt
                    Wc = pW[cur]
                    rWc = res("pW%d" % cur)
                    sc.op("dve", lambda e, Wc=Wc, cc=cc, wnd=wnd: e.scalar_tensor_tensor(
                        mixT[:, cc, :], Wc[:, PADL:PADL + S], 1.0 / wnd, pP[:, PADL:PADL + S], ALU.mult, ALU.subtract),
                        reads=[rWc, res("pP")], writes=[res("mixT")])
                    for (c0, r0) in ((0, 0), (S - 8, 8)):
                        sc.op("dve", lambda e, Wc=Wc, c0=c0, r0=r0, g=g: e.tensor_tensor(
                            ptmp[:, 0:8], Wc[:, PADL + c0:PADL + c0 + 8], rc[:, g, r0:r0 + 8], ALU.mult),
                            reads=[rWc, res("rc")], writes=[res("ptmp")])
                        sc.op("dve", lambda e, c0=c0, cc=cc: e.tensor_tensor(
                            mixT[:, cc, c0:c0 + 8], ptmp[:, 0:8], pP[:, PADL + c0:PADL + c0 + 8], ALU.subtract),
                            reads=[res("ptmp"), res("pP"), res("mixT")], writes=[res("mixT")])
                for dc in range(2):
                    chunk = 2 * g + dc
                    wb, rw = load_win(40 + chunk)
                    for j in range(4):
                        ts = slice(j * 512, (j + 1) * 512)
                        for kc in range(8):
                            sc.op("pe", lambda e, wb=wb, kc=kc, ts=ts: e.matmul(
                                pb[6][:], lhsT=wb[:, kc, :], rhs=hT[:, kc, ts], start=(kc == 0), stop=(kc == 7)),
                                reads=[rw, rhT[2 * j], rhT[2 * j + 1]], writes=[rpb[6]])
                        sc.op("act", lambda e: e.activation(gpT[:], pb[6][:], AF.Sigmoid),
                              reads=[rpb[6]], writes=[res("gpT")])
                        for cc in range(2):
                            sc.op("pe", lambda e, dc=dc, cc=cc, ts=ts: e.matmul(
                                pb[5][:], lhsT=pwS[:, dc, cc, :], rhs=mixT[:, cc, ts], start=(cc == 0), stop=(cc == 1)),
                                reads=[res("pw"), res("mixT")], writes=[rpb[5]])
                        sc.op("dve", lambda e, chunk=chunk: e.scalar_tensor_tensor(
                            ptmp[:], pb[5][:], psc[:, chunk:chunk + 1], gpT[:], ALU.mult, ALU.mult),
                            reads=[rpb[5], res("gpT"), res("psc")], writes=[res("ptmp")])
                        sc.op("dve", lambda e, chunk=chunk, ts=ts: e.tensor_tensor(
                            mT[:, chunk, ts], mT[:, chunk, ts], ptmp[:], ALU.add),
                            reads=[res("ptmp"), rmT[chunk][j]], writes=[rmT[chunk][j]])

            sc.fence()
            sc.op("dve", lambda e, s=s: e.scalar_tensor_tensor(A2[:], modT[:, 32:40, s], 1.0, n2g[:], ALU.add, ALU.mult),
                  reads=[res("modT"), res("n2g")], writes=[res("Acol")])

            def router(j, part):
                for sub in range(CW // 128):
                    gt = s * 16 + (CW // 128) * j + sub
                    Wt = Wtok[gt % 2]
                    rWt = res("Wtok%d" % (gt % 2))
                    pS = posS[gt % 2]
                    rpS = res("posS%d" % (gt % 2))
                    if part == "b":
                        sc.op("pe", lambda e, sub=sub: e.matmul(pb[5][:, 0:NE], lhsT=lsm[:], rhs=selT2[sub][:], start=True, stop=True),
                              reads=[res("lsm"), res("selT%d" % sub)], writes=[rpb[5]])
                        sc.op("dve", lambda e, pS=pS: e.tensor_tensor(pS[:], pb[5][:, 0:NE], base[:], ALU.add),
                              reads=[rpb[5], res("base")], writes=[rpS])
                        sc.op("pe", lambda e, sub=sub: e.matmul(pb[4][:, 0:NE], lhsT=ones[:], rhs=selT2[sub][:], start=True, stop=True),
                              reads=[res("ones"), res("selT%d" % sub)], writes=[rpb[4]])
                        sc.op("dve", lambda e: e.tensor_tensor(base[:], pb[4][:, 0:NE], base[:], ALU.add),
                              reads=[rpb[4], res("base")], writes=[res("base")])
                        sc.dma("sp", "st_pos", pos_d[gt], pS[:], reads=[rpS], writes=[])
                        sc.dma("sp", "st_wt", wt_d[gt], Wt[:], reads=[rWt], writes=[])
                        continue
                    if part == "h":
                        hb = h2tm[gt % 2]
                        rhb = res("h2tm%d" % (gt % 2))
                        tl = slice((CW * j) + sub * 128, (CW * j) + (sub + 1) * 128)
                        pbt = pb[7][:, :].bitcast(BF16)
                        for kc in range(8):
                            sc.op("pe", lambda e, kc=kc, tl=tl, pbt=pbt: e.transpose(pbt[:, kc * 128:(kc + 1) * 128], hT[:, kc, tl], idtb[:]),
                                  reads=[rhT[j], res("idtb")], writes=[rpb[7]])
                        sc.op("act", lambda e, hb=hb, pbt=pbt: e.activation(hb[:], pbt[:, :], AF.Copy),
                              reads=[rpb[7]], writes=[rhb])
                        sc.dma("sp", "st_h2", h2_d[gt * 128:(gt + 1) * 128, :], hb[:], reads=[rhb], writes=[])
                        continue
                    for kc in range(8):
                        sc.op("pe", lambda e, kc=kc, sub=sub: e.matmul(
                            pb[6][:, 0:NE], lhsT=tmpF[:, kc, sub * 128:(sub + 1) * 128], rhs=wr[:, kc, :],
                            start=(kc == 0), stop=(kc == 7)),
                            reads=[rtmpF[kc], res("wr")], writes=[rpb[6]])
                    sc.op("act", lambda e: e.activation(scr[:], pb[6][:, 0:NE], AF.Sigmoid),
                          reads=[rpb[6]], writes=[res("scr")])
                    sc.op("dve", lambda e: e.tensor_tensor(bia[:], scr[:], brt[:], ALU.add),
                          reads=[res("scr"), res("brt")], writes=[res("bia")])
                    for gi in range(8):
                        sc.op("dve", lambda e, gi=gi: e.max(out=m8[:, gi, :], in_=bia[:, gi * 32:(gi + 1) * 32]),
                              reads=[res("bia")], writes=[res("m8")])
                    sc.op("dve", lambda e: e.tensor_tensor(gsc[:], m8[:, :, 0], m8[:, :, 1], ALU.add),
                          reads=[res("m8")], writes=[res("gsc")])
                    sc.op("dve", lambda e: e.max(out=gm8[:], in_=gsc[:]), reads=[res("gsc")], writes=[res("gm8")])
                    sc.op("dve", lambda e: e.tensor_scalar(gmk[:], gsc[:], gm8[:, 3:4], None, ALU.is_ge),
                          reads=[res("gsc"), res("gm8")], writes=[res("gmk")])
                    sc.op("dve", lambda e: e.tensor_scalar(msk[:], bia[:], 2.0, None, ALU.add),
                          reads=[res("bia")], writes=[res("msk")])
                    sc.op("dve", lambda e: e.tensor_tensor(
                        msk[:, :].rearrange("p (g k) -> p g k", g=8), msk[:, :].rearrange("p (g k) -> p g k", g=8),
                        gmk[:, :].unsqueeze(2).to_broadcast([128, 8, 32]), ALU.mult),
                        reads=[res("msk"), res("gmk")], writes=[res("msk")])
                    sc.op("dve", lambda e: e.max(out=t8[:], in_=msk[:]), reads=[res("msk")], writes=[res("t8")])
                    sc.op("dve", lambda e, Wt=Wt: e.scalar_tensor_tensor(
                        Wt[:], msk[:], t8[:, 7:8], scr[:], ALU.is_ge, ALU.mult),
                        reads=[res("msk"), res("t8"), res("scr")], writes=[rWt])
                    sc.op("dve", lambda e, Wt=Wt: e.tensor_reduce(den[:, 0:1], Wt[:], AX.X, ALU.add),
                          reads=[rWt], writes=[res("den")])
                    sc.op("dve", lambda e: e.reciprocal(den[:, 1:2], den[:, 0:1]),
                          reads=[res("den")], writes=[res("den")])
                    sc.op("dve", lambda e, Wt=Wt: e.tensor_scalar(Wt[:], Wt[:], den[:, 1:2], 2.5, ALU.mult, ALU.mult),
                          reads=[rWt, res("den")], writes=[rWt])
                    sc.op("dve", lambda e, Wt=Wt, sub=sub: e.tensor_scalar(selT2[sub][:], Wt[:], 0.0, None, ALU.is_gt),
                          reads=[rWt], writes=[res("selT%d" % sub)])

            for j in range(8):
                ts = slice(j * CW, (j + 1) * CW)
                sc.dma("sp", "ld_x", xch[:], xT_d[s][:, :, ts], writes=[res("xch")])
                for oc in range(8):
                    wb = woS[oc % 2]
                    rw = res("wo%d" % (oc % 2))
                    sc.dma("pool", "ld_wo%d" % (oc % 2), wb[:], wo_d[oc], writes=[rw])
                    for kc in range(8):
                        sc.op("pe", lambda e, wb=wb, kc=kc, ts=ts: e.matmul(
                            pb[4][:, 0:CW], lhsT=wb[:, kc, :], rhs=mT[:, kc, ts], start=(kc == 0), stop=(kc == 7)),
                            reads=[rw], writes=[rpb[4]])
                    sc.op("dve", lambda e, oc=oc, s=s: e.scalar_tensor_tensor(
                        xch[:, oc, :], pb[4][:, 0:CW], modT[:, 16 + oc, s:s + 1], xch[:, oc, :], ALU.mult, ALU.add),
                        reads=[rpb[4], res("modT"), res("xch")], writes=[res("xch")])
                rmsnorm_chunk(A2, 24, s, ts, rhT[j], True)
                router(j, "a")
                router(j, "h")
                for half in range(2):
                    hs = slice(half * 128, (half + 1) * 128)
                    for kc in range(8):
                        sc.op("pe", lambda e, kc=kc, hs=hs, ts=ts, half=half: e.matmul(
                            pb[half][:, 0:CW], lhsT=wsg[:, kc, hs], rhs=hT[:, kc, ts], start=(kc == 0), stop=(kc == 7)),
                            reads=[res("wsg"), rhT[j]], writes=[rpb[half]])
                    for kc in range(8):
                        sc.op("pe", lambda e, kc=kc, hs=hs, ts=ts, half=half: e.matmul(
                            pb[2 + half][:, 0:CW], lhsT=wsu[:, kc, hs], rhs=hT[:, kc, ts], start=(kc == 0), stop=(kc == 7)),
                            reads=[res("wsu"), rhT[j]], writes=[rpb[2 + half]])
                router(j, "b")
                for half in range(2):
                    sc.op("act", lambda e, half=half: e.activation(sil[:, half, 0:CW], pb[half][:, 0:CW], AF.Silu),
                          reads=[rpb[half]], writes=[res("sil%d" % half)])
                    sc.op("dve", lambda e, half=half: e.tensor_tensor(
                        aT[:, half, 0:CW], sil[:, half, 0:CW], pb[2 + half][:, 0:CW], ALU.mult),
                        reads=[res("sil%d" % half), rpb[2 + half]], writes=[res("aT")])
                for dcn in range(8):
                    bi = 5 + (dcn % 2)
                    for cc in range(2):
                        sc.op("pe", lambda e, cc=cc, dcn=dcn, bi=bi: e.matmul(
                            pb[bi][:, 0:CW], lhsT=wsd[:, cc, dcn * 128:(dcn + 1) * 128], rhs=aT[:, cc, 0:CW],
                            start=(cc == 0), stop=(cc == 1)),
                            reads=[res("wsd"), res("aT")], writes=[rpb[bi]])
                    sc.op("dve", lambda e, dcn=dcn, bi=bi, s=s: e.scalar_tensor_tensor(
                        xch[:, dcn, :], pb[bi][:, 0:CW], modT[:, 40 + dcn, s:s + 1], xch[:, dcn, :], ALU.mult, ALU.add),
                        reads=[rpb[bi], res("modT"), res("xch")], writes=[res("xch")])
                sc.dma("sp", "st_xs", xs_d[s][:, :, ts], xch[:], reads=[res("xch")], writes=[])

        sc.fence()
        us = [cvA(0, 256), cvA(256, 256)]
        ui = [cvA(512, 256), cvA(768, 256)]
        iot = cvA(1024, 1024)
        for c in range(2):
            sc.dma("sp", "ld_us", us[c][:], us_d[c], writes=[res("us%d" % c)])
            sc.dma("sp", "ld_ui", ui[c][:], ui_d[c], writes=[res("ui%d" % c)])
        sc.dma("sp", "ld_iot", iot[:], iot_d, writes=[res("iot")])
        sc.op("dve", lambda e: e.tensor_scalar(nbf[:], base[:], 127.0, None, ALU.add), reads=[res("base")], writes=[res("nbf")])
        sc.op("dve", lambda e: e.tensor_copy(out=nbi[:], in_=nbf[:]), reads=[res("nbf")], writes=[res("nbi")])
        sc.op("dve", lambda e: e.tensor_single_scalar(nbi[:], nbi[:], 7, ALU.arith_shift_right),
              reads=[res("nbi")], writes=[res("nbi")])
        sc.op("dve", lambda e: e.tensor_copy(out=nbf[:], in_=nbi[:]), reads=[res("nbi")], writes=[res("nbf")])
        for c in range(2):
            sc.op("pe", lambda e, c=c: e.transpose(pb[c][:, 0:128], nbf[:, c * 128:(c + 1) * 128], idt[:]),
                  reads=[res("nbf"), res("idt")], writes=[rpb[c]])
            sc.op("dve", lambda e, c=c: e.tensor_copy(out=nbT[c][:], in_=pb[c][:, 0:128]), reads=[rpb[c]], writes=[res("nbT%d" % c)])
        for c in range(2):
            sc.op("pe", lambda e, c=c: e.matmul(pb[2][:, 0:NE], lhsT=nbT[c][:], rhs=us[c][:], start=(c == 0), stop=(c == 1)),
                  reads=[res("nbT%d" % c), res("us%d" % c)], writes=[rpb[2]])
        sc.op("dve", lambda e: e.tensor_scalar(sbase[:], pb[2][:, 0:NE], 128.0, None, ALU.mult), reads=[rpb[2]], writes=[res("sbase")])
        for c2 in range(2):
            for c in range(2):
                sc.op("pe", lambda e, c=c, c2=c2: e.matmul(pb[3][:, 0:2], lhsT=ui[c][:, c2 * 128:(c2 + 1) * 128], rhs=nbT[c][:, 0:2],
                                                           start=(c == 0), stop=(c == 1)),
                      reads=[res("nbT%d" % c), res("ui%d" % c)], writes=[rpb[3]])
            sc.op("dve", lambda e, c2=c2: e.tensor_copy(out=bend[:, c2:c2 + 1], in_=pb[3][:, 0:1]), reads=[rpb[3]], writes=[res("bend")])
        for c2 in range(2):
            sc.op("dve", lambda e, c2=c2: e.tensor_scalar(Cm[c2][:], iot[:, 0:NBLK], bend[:, c2:c2 + 1], None, ALU.is_ge),
                  reads=[res("iot"), res("bend")], writes=[res("Cm%d" % c2)])
        for c0 in range(0, NBLK, 512):
            cw = min(512, NBLK - c0)
            for c2 in range(2):
                sc.op("pe", lambda e, c0=c0, cw=cw, c2=c2: e.matmul(pb[4][:, 0:cw], lhsT=ones[:], rhs=Cm[c2][:, c0:c0 + cw],
                                                                    start=(c2 == 0), stop=(c2 == 1)),
                      reads=[res("ones"), res("Cm%d" % c2)], writes=[rpb[4]])
            sc.op("dve", lambda e, c0=c0, cw=cw: e.tensor_scalar(bef[:, c0:c0 + cw], pb[4][:, 0:cw], 128.0, None, ALU.mult),
                  reads=[rpb[4]], writes=[res("bef")])
        sc.op("dve", lambda e: e.tensor_scalar(idxW[:], bef[:], pio8[:, 0:1], None, ALU.add),
              reads=[res("bef"), res("pio8")], writes=[res("idxW")])
        sc.op("dve", lambda e: e.memset(zslot[:], 0.0), writes=[res("zslot")])
        sc.dma("sp", "st_z", slot_d.rearrange("(p a) b -> p (a b)", p=128), zslot[:], reads=[res("zslot")], writes=[res("slot_d")])
        def loadB(gt):
            bp = gt % NPB
            sc.dma("sp", "ld_pos%d" % bp, posL[bp][:], pos_d[gt], reads=[res("pos_d")], writes=[res("posL%d" % bp)])
            sc.dma("sp", "ld_wt%d" % bp, wtL[bp][:], wt_d[gt], reads=[res("wt_d")], writes=[res("wtL%d" % bp)])

        for g0 in range(min(NPB - 1, NT)):
            loadB(g0)
        for gt in range(NT):
            bp, br = gt % NPB, gt % NRB
            if gt + NPB - 1 < NT:
                loadB(gt + NPB - 1)
            sc.op("dve", lambda e, bp=bp: e.scalar_tensor_tensor(dp1[:], posL[bp][:], 1.0, sbase[:], ALU.add, ALU.add),
                  reads=[res("posL%d" % bp), res("sbase")], writes=[res("dp1")])
            sc.op("dve", lambda e, bp=bp: e.scalar_tensor_tensor(keyb[:], wtL[bp][:], 0.0, dp1[:], ALU.is_gt, ALU.mult),
                  reads=[res("wtL%d" % bp), res("dp1")], writes=[res("keyb")])
            sc.op("dve", lambda e: e.max(out=d8[:], in_=keyb[:]), reads=[res("keyb")], writes=[res("d8")])
            rrw = res("rows%d" % br)
            sc.op("dve", lambda e, br=br, gt=gt: e.tensor_scalar(rows[br][:, :, 0], pio8[:], float(gt * 128), None, ALU.add),
                  reads=[res("pio8")], writes=[rrw])
            for k in range(8):
                sc.op("dve", lambda e, bp=bp, br=br, k=k: e.scalar_tensor_tensor(
                    junkb[:], keyb[:], d8[:, k:k + 1], wtL[bp][:], ALU.is_equal, ALU.mult, accum_out=rows[br][:, k, 1:2]),
                    reads=[res("keyb"), res("d8"), res("wtL%d" % bp)], writes=[res("junkb"), rrw])
            sc.op("dve", lambda e, gt=gt: e.tensor_scalar(dstu[:, gt, :], d8[:], -1.0, None, ALU.add),
                  reads=[res("d8")], writes=[res("dstu%d" % gt)])
            for k in range(8):
                sc.idma("sc_slot%d" % br, slot_d, dstu[:, gt, k:k + 1], rows[br][:, k, :], None,
                        reads=[rrw, res("dstu%d" % gt), res("slot_d")], writes=[])

        sc.fence()
        slotv = slot_d.rearrange("(i p) c -> i (p c)", p=128)
        for t6 in range(NBLK // 128):
            b2 = t6 % 2
            sc.dma("sp", "ld_srow%d" % b2, sl6[b2][:], slotv[t6 * 128:(t6 + 1) * 128, :], reads=[res("slot_d")],
                   writes=[res("sl6_%d" % b2)])
            v3 = sl6[b2][:, :].rearrange("i (p c) -> i p c", c=2)
            for c in range(2):
                sc.op("dve", lambda e, c=c, v3=v3: e.tensor_copy(out=sl6c[c][:], in_=v3[:, :, c]),
                      reads=[res("sl6_%d" % b2)], writes=[res("sl6c%d" % c)])
                sc.op("pe", lambda e, c=c: e.transpose(pb[c][:, 0:128], sl6c[c][:], idt[:]),
                      reads=[res("sl6c%d" % c), res("idt")], writes=[rpb[c]])
            sc.op("dve", lambda e, t6=t6: e.tensor_copy(out=tokuA[:, t6 * 128:(t6 + 1) * 128], in_=pb[0][:, 0:128]),
                  reads=[rpb[0]], writes=[res("tokuA")])
            sc.op("act", lambda e, t6=t6: e.activation(wA[:, t6 * 128:(t6 + 1) * 128], pb[1][:, 0:128], AF.Copy),
                  reads=[rpb[1]], writes=[res("srowA")])
        WB = NE * 128 - 1

        def gathers(i):
            b3 = i % 3
            sc.idma("g_x%d" % b3, xgC[b3][:], None, h2_d, tokuA[:, i:i + 1], reads=[res("tokuA"), res("h2_d")],
                    writes=[res("xg%d" % b3)])
            sc.idma("g_wg%d" % b3, wgC[b3][:], None, wg2, idxW[:, i:i + 1], reads=[res("idxW")], writes=[res("wgC%d" % b3)], bound=WB)
            sc.idma("g_wu%d" % b3, wuC[b3][:], None, wu2, idxW[:, i:i + 1], reads=[res("idxW")], writes=[res("wuC%d" % b3)], bound=WB)
            sc.idma("g_wd%d" % b3, wdC[b3][:, :, :].rearrange("p a b -> p (a b)"), None, wd2, idxW[:, i:i + 1],
                    reads=[res("idxW")], writes=[res("wdC%d" % b3)], bound=WB)

        def stage1(i):
            b2, b3 = i % 2, i % 3
            pT_, rT_ = pb[4 * b2], rpb[4 * b2]
            pbt = pT_[:, :].bitcast(BF16)
            for kc in range(8):
                sc.op("pe", lambda e, kc=kc, b3=b3, pbt=pbt: e.transpose(
                    pbt[:, kc * 128:(kc + 1) * 128], xgC[b3][:, kc * 128:(kc + 1) * 128], idtb[:]),
                    reads=[res("xg%d" % b3), res("idtb")], writes=[rT_])
            sc.op("act", lambda e, b2=b2, pbt=pbt: e.activation(xgT[b2][:, :, :].rearrange("p a b -> p (a b)"), pbt[:, :], AF.Copy),
                  reads=[rT_], writes=[res("xgT%d" % b2)])

        def stage2(i):
            b2, b3 = i % 2, i % 3
            pG_, rG_ = pb[4 * b2 + 1], rpb[4 * b2 + 1]
            for gu, wC, rn in ((0, wgC, "wgC%d"), (1, wuC, "wuC%d")):
                for half in range(2):
                    col = (gu * 2 + half) * 128
                    for kc in range(8):
                        sc.op("pe", lambda e, kc=kc, b2=b2, b3=b3, wC=wC, half=half, col=col, pG_=pG_: e.matmul(
                            pG_[:, col:col + 128], lhsT=wC[b3][:, kc * 256 + half * 128:kc * 256 + (half + 1) * 128],
                            rhs=xgT[b2][:, kc, :], start=(kc == 0), stop=(kc == 7)),
                            reads=[res(rn % b3), res("xgT%d" % b2)], writes=[rG_])
            sc.op("act", lambda e, b2=b2, pG_=pG_: e.activation(silC[b2][:], pG_[:, 0:256], AF.Silu),
                  reads=[rG_], writes=[res("silC%d" % b2)])
            sc.op("dve", lambda e, b2=b2, pG_=pG_: e.tensor_tensor(
                aTC[b2][:, :, :].rearrange("p a b -> p (a b)"), silC[b2][:], pG_[:, 256:512], ALU.mult),
                reads=[res("silC%d" % b2), rG_], writes=[res("aTC%d" % b2)])

        def stage3(i):
            b2, b3 = i % 2, i % 3
            pY0, pY1, rY0, rY1 = pb[4 * b2 + 2], pb[4 * b2 + 3], rpb[4 * b2 + 2], rpb[4 * b2 + 3]
            for dh, pY_, rY_ in ((0, pY0, rY0), (1, pY1, rY1)):
                for cc in range(2):
                    sc.op("pe", lambda e, cc=cc, b2=b2, b3=b3, dh=dh, pY_=pY_: e.matmul(
                        pY_[:], lhsT=aTC[b2][:, cc, :], rhs=wdC[b3][:, cc, dh * 512:(dh + 1) * 512],
                        start=(cc == 0), stop=(cc == 1)),
                        reads=[res("aTC%d" % b2), res("wdC%d" % b3)], writes=[rY_])
            sc.op("act", lambda e, b2=b2, pY0=pY0, i=i: e.activation(ysb[b2][:, 0:512], pY0[:], AF.Copy, scale=wA[:, i:i + 1]),
                  reads=[rY0, res("srowA")], writes=[res("ysb%d" % b2)])
            sc.op("dve", lambda e, b2=b2, pY1=pY1, i=i: e.tensor_scalar(ysb[b2][:, 512:1024], pY1[:], wA[:, i:i + 1], None, ALU.mult),
                  reads=[rY1, res("srowA")], writes=[res("ysb%d" % b2)])
            sc.dma("sp", "st_y", y_d[i * 128:(i + 1) * 128, :], ysb[b2][:], reads=[res("ysb%d" % b2)], writes=[])

        gathers(0)
        gathers(1)
        stage1(0)
        for i in range(NBLK):
            if i + 2 < NBLK:
                gathers(i + 2)
            stage2(i)
            if i + 1 < NBLK:
                stage1(i + 1)
            stage3(i)

        sc.fence()
        xstL = [cvB(0, 1024).rearrange("p (a b) -> p a b", a=8), cvB(2048, 1024).rearrange("p (a b) -> p a b", a=8)]
        otlL = [cvB(1024, 1024).rearrange("p (a b) -> p a b", a=8), cvB(3072, 1024).rearrange("p (a b) -> p a b", a=8)]

        def gathD(gt):
            b3 = gt % NYG
            for k in range(8):
                sc.idma("g_y%d" % b3, yg[b3][:, k, :], None, y_d, dstu[:, gt, k:k + 1], reads=[res("dstu"), res("y_d")],
                        writes=[res("yg%d_%d" % (b3, k))])

        for g0 in range(min(NYG - 1, NT)):
            gathD(g0)
        for gt in range(NT):
            b2, b3 = gt % 2, gt % NYG
            if gt + NYG - 1 < NT:
                gathD(gt + NYG - 1)
            s_, t_ = gt // 16, gt % 16
            tt = slice(t_ * 128, (t_ + 1) * 128)
            ryg = res("yg%d" % b3)
            acc_, racc = accD[b2], res("accD%d" % b2)
            xst_, rxst = xstL[b2], res("xst%d" % b2)
            otl_, rotl = otlL[b2], res("otl%d" % b2)
            if gt == 0:
                sc.dma("sp", "ld_xs%d" % b2, xst_[:, :, :], xs_d[s_][:, :, tt], reads=[res("xs_d")], writes=[rxst])
            if gt + 1 < NT:
                g1 = gt + 1
                s1, t1 = g1 // 16, g1 % 16
                sc.dma("sp", "ld_xs%d" % (g1 % 2), xstL[g1 % 2][:, :, :], xs_d[s1][:, :, t1 * 128:(t1 + 1) * 128],
                       reads=[res("xs_d")], writes=[res("xst%d" % (g1 % 2))])
            sc.op("dve", lambda e, b3=b3, acc_=acc_: e.tensor_tensor(acc_[:], yg[b3][:, 0, :], yg[b3][:, 1, :], ALU.add),
                  reads=[res("yg%d_0" % b3), res("yg%d_1" % b3)], writes=[racc])
            for k in range(2, 8):
                sc.op("dve", lambda e, k=k, b3=b3, acc_=acc_: e.tensor_tensor(acc_[:], acc_[:], yg[b3][:, k, :], ALU.add),
                      reads=[res("yg%d_%d" % (b3, k)), racc], writes=[racc])
            for c in range(8):
                bi = 4 * b2 + c // 4
                sc.op("pe", lambda e, c=c, bi=bi, acc_=acc_: e.transpose(pb[bi][:, (c % 4) * 128:(c % 4 + 1) * 128],
                                                                          acc_[:, c * 128:(c + 1) * 128], idt[:]),
                      reads=[racc, res("idt")], writes=[rpb[bi]])
            for c in range(8):
                bi = 4 * b2 + c // 4
                sc.op("dve", lambda e, c=c, bi=bi, s_=s_, otl_=otl_, xst_=xst_: e.scalar_tensor_tensor(
                    otl_[:, c, :], pb[bi][:, (c % 4) * 128:(c % 4 + 1) * 128], modT[:, 40 + c, s_:s_ + 1], xst_[:, c, :],
                    ALU.mult, ALU.add),
                    reads=[rpb[bi], res("modT"), rxst], writes=[rotl])
            tok = sc.dma("sp", "st_out", out_d[s_][:, :, tt], otl_[:, :, :], reads=[rotl], writes=[])
        sc.wait("sp", tok)
        sc.emit()
    return nc


def _t5_bucket_np(rel):
    import jax
    import jax.numpy as jnp
    with jax.default_device(jax.devices("cpu")[0]):
        nb = NB // 2
        max_exact = nb // 2
        rel = jnp.asarray(np.asarray(rel, dtype=np.int32))
        n = jnp.abs(rel)
        large = max_exact + (jnp.log(jnp.maximum(n, 1).astype(jnp.float32) / max_exact)
                             / math.log(128 / max_exact) * (nb - max_exact)).astype(jnp.int32)
        large = jnp.minimum(large, nb - 1)
        return np.asarray(jnp.where(rel > 0, nb, 0) + jnp.where(n < max_exact, n, large))


def _chunk_w(w, ncol_chunk=128):
    K, N = w.shape
    return np.ascontiguousarray(w.reshape(K // 128, 128, N // ncol_chunk, ncol_chunk).transpose(2, 1, 0, 3))


def _prep_shared(inp):
    f = np.float32
    g = {}
    g["w_ada"] = _chunk_w(inp["w_ada"][0])
    g["b_adaT"] = np.ascontiguousarray(inp["b_ada"][0].reshape(48, 128).T)
    g["n1g"] = np.ascontiguousarray(inp["norm1_g"][0].reshape(8, 128).T)
    g["n2g"] = np.ascontiguousarray(inp["norm2_g"][0].reshape(8, 128).T)
    g["w_in"] = _chunk_w(inp["w_in"][0])
    g["qkg"] = np.ascontiguousarray(np.stack([np.tile(inp["q_norm_g"][0], 2), np.tile(inp["k_norm_g"][0], 2)], axis=1))
    lam = np.stack([inp["lambda_q1"][0], inp["lambda_k1"][0], inp["lambda_q2"][0], inp["lambda_k2"][0]], axis=0)
    g["lam_in"] = np.ascontiguousarray(np.broadcast_to(lam[None], (128, 4, 64)))
    g["subln_g"] = np.ascontiguousarray(np.broadcast_to(inp["subln_g"][0][None], (128, 128)))
    pw = inp["pool_w"][0]
    g["pool_w"] = np.ascontiguousarray(pw.reshape(4, 2, 128, 2, 128).transpose(0, 3, 2, 1, 4))
    g["pool_scaleT"] = np.ascontiguousarray(inp["pool_scale"][0].reshape(8, 128).T)
    rc = np.zeros((4, 16), f)
    for gi, w in enumerate((2, 4, 8, 16)):
        for k, pos in enumerate(list(range(8)) + list(range(S - 8, S))):
            lo = min(max(pos - w // 2, 0), S - 1)
            hi = min(max(pos + w // 2 - 1, 0), S - 1)
            rc[gi, k] = 1.0 / float(hi - lo + 1)
    g["pool_rc"] = np.ascontiguousarray(np.broadcast_to(rc[None], (128, 4, 16)))
    g["w_out"] = _chunk_w(inp["w_out"][0])
    g["w_router"] = np.ascontiguousarray(inp["w_router"][0].reshape(8, 128, NE).transpose(1, 0, 2))
    g["b_router"] = np.ascontiguousarray(np.broadcast_to(inp["b_router"][0][None], (128, NE)))
    wg = np.concatenate([inp["w_exp_gate"][0], inp["w_sh_gate"]], axis=0)
    wu = np.concatenate([inp["w_exp_up"][0], inp["w_sh_up"]], axis=0)
    wd = np.concatenate([inp["w_exp_down"][0], inp["w_sh_down"]], axis=0)
    g["w_eg"] = np.ascontiguousarray(wg.reshape(NE + 1, 8, 128, 256).transpose(0, 2, 1, 3))
    g["w_eu"] = np.ascontiguousarray(wu.reshape(NE + 1, 8, 128, 256).transpose(0, 2, 1, 3))
    g["w_ed"] = np.ascontiguousarray(wd.reshape(NE + 1, 2, 128, 1024).transpose(0, 2, 1, 3))
    g["rel_tab"] = np.ascontiguousarray(inp["rel_bias_table"])
    jj = np.arange(FVW)
    bk = _t5_bucket_np(767 - jj)
    oh = np.zeros((NB, FVW), f)
    oh[bk, jj] = 1.0
    g["bias_oh"] = oh
    g["ident"] = np.eye(128, dtype=f)
    g["antiid"] = np.ascontiguousarray(np.eye(128, dtype=f)[::-1])
    bo = np.zeros((128, 128), f)
    bo[:64, :64] = 1.0
    bo[64:, 64:] = 1.0
    g["blockones"] = bo
    ar = np.arange(128)
    g["lstrict"] = (ar[:, None] < ar[None, :]).astype(f)
    ee = np.arange(NE)
    g["ustrict"] = np.stack([((c * 128 + ar)[:, None] < ee[None, :]).astype(f) for c in range(2)])
    g["uincl"] = np.stack([((c * 128 + ar)[:, None] <= ee[None, :]).astype(f) for c in range(2)])
    g["iota_row"] = np.ascontiguousarray(np.broadcast_to(np.arange(1024, dtype=f)[None], (128, 1024)))
    g["piota8"] = np.ascontiguousarray(np.broadcast_to(ar.astype(f)[:, None], (128, 8)))
    return {k: np.asarray(v, dtype=f) for k, v in g.items()}


def _core_inputs(shared, x, c, b0, nseq):
    m = dict(shared)
    xs = x[b0:b0 + nseq]
    m["xT"] = np.ascontiguousarray(xs.reshape(nseq, S, 8, 128).transpose(0, 3, 2, 1))
    m["cT"] = np.ascontiguousarray(c[b0:b0 + nseq].reshape(nseq, 8, 128).transpose(2, 1, 0))
    return m


def _unpack(outT):
    return np.ascontiguousarray(outT.transpose(0, 3, 2, 1).reshape(outT.shape[0], S, D))


def kernel(**inputs):
    inp = {k: np.asarray(v, dtype=np.float32) for k, v in inputs.items()}
    ncores = 8
    nseq = inp["x"].shape[0] // ncores
    shared = _prep_shared(inp)
    nc = build(nseq)
    in_maps = [_core_inputs(shared, inp["x"], inp["c"], i * nseq, nseq) for i in range(ncores)]
    res = run_bass_kernel_spmd(nc, in_maps, core_ids=list(range(ncores)))
    return np.concatenate([_unpack(r["outT"]) for r in res.results], axis=0)
```
